# Optimizing a Trainium2 kernel written in Bass

```python
import math
import jax, jax.numpy as jnp
from jax import lax
import numpy as np

D_MODEL = 2048
BATCH = 4
SEQ = 4096
DEPTH = 2

N_A = DEPTH // 2
N_B = DEPTH - N_A
CONV_WIDTH = 31
D_FF = 5632
FFN_CONV_WIDTH = 3
HEAD_DIM = 128
N_HEADS = D_MODEL // HEAD_DIM
BLOCK = 256
TOP_K = 3
Q_CHUNK = 16
EPS = 1e-6
NEG_INF = -1e30

kernel_name = "yoco_conformer_moba_hybrid"


def rms_norm(x, g):
    xf = x.astype(jnp.float32)
    y = xf * lax.rsqrt(jnp.mean(xf * xf, axis=-1, keepdims=True) + EPS)
    return (y * g.astype(jnp.float32)).astype(x.dtype)


def layer_norm(x, g, b):
    xf = x.astype(jnp.float32)
    mu = jnp.mean(xf, axis=-1, keepdims=True)
    var = jnp.mean(jnp.square(xf - mu), axis=-1, keepdims=True)
    y = (xf - mu) * lax.rsqrt(var + EPS)
    return (y * g.astype(jnp.float32) + b.astype(jnp.float32)).astype(x.dtype)


def causal_dwconv(x, w):
    width = w.shape[0]
    return lax.conv_general_dilated(
        x, w[:, None, :].astype(x.dtype), window_strides=(1,),
        padding=[(width - 1, 0)], dimension_numbers=("NWC", "WIO", "NWC"),
        feature_group_count=x.shape[-1])


def alibi_slopes():
    h = jnp.arange(1, N_HEADS + 1, dtype=jnp.float32)
    return jnp.exp2(-8.0 * h / N_HEADS)


def conformer_conv(h, w_pw1, b_pw1, w_dw, b_dw, ln_g, ln_b, w_pw2, b_pw2):
    u = h @ w_pw1 + b_pw1
    a, gate = jnp.split(u, 2, axis=-1)
    u = a * jax.nn.sigmoid(gate)
    u = causal_dwconv(u, w_dw) + b_dw
    u = jax.nn.silu(layer_norm(u, ln_g, ln_b))
    return u @ w_pw2 + b_pw2


def conv_ffn(h, w_in, w_conv, w_out):
    g, u = jnp.split(h @ w_in, 2, axis=-1)
    g = causal_dwconv(g, w_conv)
    return (jax.nn.silu(g) * u) @ w_out


def to_heads(t):
    b, s, _ = t.shape
    return t.reshape(b, s, N_HEADS, HEAD_DIM).transpose(0, 2, 1, 3)


def shared_kv(x, kv_norm_g, w_kv, k_norm_g):
    b, s, _ = x.shape
    k, v = jnp.split(rms_norm(x, kv_norm_g) @ w_kv, 2, axis=-1)
    k = rms_norm(to_heads(k), k_norm_g)
    v = to_heads(v)
    nb = -(-s // BLOCK)
    pad = nb * BLOCK - s
    k = jnp.pad(k, ((0, 0), (0, 0), (0, pad), (0, 0)))
    v = jnp.pad(v, ((0, 0), (0, 0), (0, pad), (0, 0)))
    kb = k.reshape(b, N_HEADS, nb, BLOCK, HEAD_DIM)
    vb = v.reshape(b, N_HEADS, nb, BLOCK, HEAD_DIM)
    k_mean = jnp.mean(kb.astype(jnp.float32), axis=3)
    return kb, vb, k_mean


def moba_attention(q, kb, vb, k_mean):
    b, h, s, dh = q.shape
    nb = kb.shape[2]
    k_sel = min(TOP_K, nb)
    n_chunks = s // Q_CHUNK
    scale = dh ** -0.5
    slopes = alibi_slopes()
    b_idx = jnp.arange(b)[:, None, None, None]
    h_idx = jnp.arange(h)[None, :, None, None]
    q_chunks = q.reshape(b, h, n_chunks, Q_CHUNK, dh).transpose(2, 0, 1, 3, 4)

    def one_chunk(args):
        qc, c = args
        t = c * Q_CHUNK + jnp.arange(Q_CHUNK)
        cur = (c * Q_CHUNK) // BLOCK
        qf = qc.astype(jnp.float32)
        gate = jnp.einsum("bhqd,bhnd->bhqn", qf, k_mean)
        past = jnp.arange(nb) < cur
        gate = jnp.where(past, gate, NEG_INF)
        _, idx = lax.top_k(gate, k_sel)
        valid = idx < cur
        ks = kb[b_idx, h_idx, idx]
        vs = vb[b_idx, h_idx, idx]
        s_sel = jnp.einsum("bhqd,bhqkjd->bhqkj", qc, ks).astype(jnp.float32) * scale
        pos_sel = (idx[..., None] * BLOCK + jnp.arange(BLOCK)).astype(jnp.float32)
        dist_sel = t.astype(jnp.float32)[None, None, :, None, None] - pos_sel
        s_sel = s_sel - slopes[None, :, None, None, None] * dist_sel
        s_sel = jnp.where(valid[..., None], s_sel, NEG_INF).reshape(b, h, Q_CHUNK, k_sel * BLOCK)
        k_own = lax.dynamic_index_in_dim(kb, cur, axis=2, keepdims=False)
        v_own = lax.dynamic_index_in_dim(vb, cur, axis=2, keepdims=False)
        s_own = jnp.einsum("bhqd,bhjd->bhqj", qc, k_own).astype(jnp.float32) * scale
        dist_own = (t[:, None] - (cur * BLOCK + jnp.arange(BLOCK))[None, :]).astype(jnp.float32)
        s_own = jnp.where(dist_own >= 0, s_own - slopes[None, :, None, None] * dist_own, NEG_INF)
        p = jax.nn.softmax(jnp.concatenate([s_sel, s_own], axis=-1), axis=-1)
        p_sel = p[..., : k_sel * BLOCK].reshape(b, h, Q_CHUNK, k_sel, BLOCK).astype(vs.dtype)
        p_own = p[..., k_sel * BLOCK:].astype(v_own.dtype)
        return (jnp.einsum("bhqkj,bhqkjd->bhqd", p_sel, vs)
                + jnp.einsum("bhqj,bhjd->bhqd", p_own, v_own))

    out = lax.map(one_chunk, (q_chunks, jnp.arange(n_chunks)))
    return out.transpose(1, 2, 0, 3, 4).reshape(b, h, s, dh)


def setup_inputs(seed: int = 0) -> dict:
    key = jax.random.key(seed)
    ks = jax.random.split(key, 24)
    f32 = jnp.float32
    D, F, dh = D_MODEL, D_FF, HEAD_DIM

    def w(k, shape, fan_in):
        return jax.random.normal(k, shape, f32) * fan_in ** -0.5

    def gain(k, shape):
        return 1.0 + 0.02 * jax.random.normal(k, shape, f32)

    def bias(k, shape):
        return 0.02 * jax.random.normal(k, shape, f32)

    return {
        "x": jax.random.normal(ks[0], (BATCH, SEQ, D), f32),
        "conv_norm_g": gain(ks[1], (N_A, D)),
        "conv_w_pw1": w(ks[2], (N_A, D, 2 * D), D),
        "conv_b_pw1": bias(ks[3], (N_A, 2 * D)),
        "conv_w_dw": w(ks[4], (N_A, CONV_WIDTH, D), CONV_WIDTH),
        "conv_b_dw": bias(ks[5], (N_A, D)),
        "conv_ln_g": gain(ks[6], (N_A, D)),
        "conv_ln_b": bias(ks[7], (N_A, D)),
        "conv_w_pw2": w(ks[8], (N_A, D, D), D),
        "conv_b_pw2": bias(ks[9], (N_A, D)),
        "kv_norm_g": gain(ks[10], (D,)),
        "w_kv": w(ks[11], (D, 2 * D), D),
        "k_norm_g": gain(ks[12], (dh,)),
        "attn_norm_g": gain(ks[13], (N_B, D)),
        "w_q": w(ks[14], (N_B, D, D), D),
        "q_norm_g": gain(ks[15], (N_B, dh)),
        "w_o": w(ks[16], (N_B, D, D), D),
        "ffn_norm_g": gain(ks[17], (DEPTH, D)),
        "ffn_w_in": w(ks[18], (DEPTH, D, 2 * F), D),
        "ffn_w_conv": w(ks[19], (DEPTH, FFN_CONV_WIDTH, F), FFN_CONV_WIDTH),
        "ffn_w_out": w(ks[20], (DEPTH, F, D), F),
    }


def reference(x, conv_norm_g, conv_w_pw1, conv_b_pw1, conv_w_dw, conv_b_dw, conv_ln_g, conv_ln_b,
              conv_w_pw2, conv_b_pw2, kv_norm_g, w_kv, k_norm_g, attn_norm_g, w_q, q_norm_g, w_o,
              ffn_norm_g, ffn_w_in, ffn_w_conv, ffn_w_out):
    b, s, d = x.shape
    kv = None
    for layer in range(DEPTH):
        if layer < N_A:
            i = layer
            x = x + conformer_conv(rms_norm(x, conv_norm_g[i]), conv_w_pw1[i], conv_b_pw1[i],
                                   conv_w_dw[i], conv_b_dw[i], conv_ln_g[i], conv_ln_b[i],
                                   conv_w_pw2[i], conv_b_pw2[i])
        else:
            if kv is None:
                kv = shared_kv(x, kv_norm_g, w_kv, k_norm_g)
            kb, vb, k_mean = kv
            j = layer - N_A
            q = rms_norm(to_heads(rms_norm(x, attn_norm_g[j]) @ w_q[j]), q_norm_g[j])
            o = moba_attention(q, kb, vb, k_mean)
            x = x + o.transpose(0, 2, 1, 3).reshape(b, s, d) @ w_o[j]
        x = x + conv_ffn(rms_norm(x, ffn_norm_g[layer]), ffn_w_in[layer], ffn_w_conv[layer],
                         ffn_w_out[layer])
    return x
```

```python
import numpy as np
import ml_dtypes
import concourse.bass as bass
import concourse.mybir as mybir
from concourse.bass_utils import run_bass_kernel_spmd
from contextlib import ExitStack

F32 = mybir.dt.float32
BF16 = mybir.dt.bfloat16
AF = mybir.ActivationFunctionType
ALU = mybir.AluOpType
AX = mybir.AxisListType

D = 2048
KC = 16
TT = 1024
NTILE = 2
TCORE = 2048
HALO = 32
WIN = TT + HALO
OW = TT + 2
FF = 5632
FC = 44
NH = 16
EPS = 1e-6
NBLK = 16
WSLOT = 44 * 128
NWSLOT = 3


class Res:
    __slots__ = ("lw", "rd")

    def __init__(self):
        self.lw = None
        self.rd = {}


class Eng:
    def __init__(self, P, name, nsem_dma=0):
        self.P = P
        self.name = name
        self.sem = P.es.enter_context(P.nc.semaphore("sem_" + name))
        self.n = 0
        self.waited = {}
        self.rec = []
        self.dsems = [P.es.enter_context(P.nc.semaphore("d%s%d" % (name, i))) for i in range(nsem_dma)]
        self.dvals = [0] * nsem_dma
        self.di = 0

    def wait(self, deps):
        for d in deps:
            if d is None:
                continue
            sem, val, key = d
            if self.waited.get(key, 0) >= val:
                continue
            self.rec.append(("w", sem, val))
            self.waited[key] = val

    @staticmethod
    def _deps(r, w):
        d = []
        for x in r:
            if x.lw is not None:
                d.append(x.lw)
        for x in w:
            if x.lw is not None:
                d.append(x.lw)
            d.extend(x.rd.values())
        return d

    @staticmethod
    def _commit(h, r, w):
        for x in r:
            x.rd[h[2]] = h
        for x in w:
            x.lw = h
            x.rd = {}

    def op(self, fn, r=(), w=(), deps=()):
        self.wait(self._deps(r, w))
        self.wait(deps)
        self.n += 1
        self.rec.append(("i", fn, self.sem, 1))
        h = (self.sem, self.n, self.name)
        self._commit(h, r, w)
        return h

    def op_noinc(self, fn, r=(), w=()):
        self.wait(self._deps(r, w))
        self.rec.append(("n", fn))

    def mark(self):
        return (self.sem, self.n, self.name) if self.n else None

    def dma(self, out, in_, r=(), w=(), deps=(), **kw):
        k = self.di
        self.di = (self.di + 1) % len(self.dsems)
        key = "%s_d%d" % (self.name, k)
        if self.dvals[k] > 0:
            self.wait([(self.dsems[k], self.dvals[k], key)])
        self.wait(self._deps(r, w))
        self.wait(deps)
        self.dvals[k] += 16
        self.rec.append(("i", (lambda e, o=out, i=in_, kw=kw: e.dma_start(out=o, in_=i, **kw)), self.dsems[k], 16))
        h = (self.dsems[k], self.dvals[k], key)
        self._commit(h, r, w)
        self.P.dma_handles[key] = h
        return h

    def replay(self, e):
        for r in self.rec:
            if r[0] == "w":
                e.wait_ge(r[1], r[2])
            elif r[0] == "n":
                r[1](e)
            else:
                r[1](e).then_inc(r[2], r[3])


class Prog:
    def __init__(self):
        self.nc = bass.Bass("TRN2", target_bir_lowering=False)
        self.es = ExitStack()
        self.dma_handles = {}
        self.pe = Eng(self, "pe")
        self.act = Eng(self, "act", 10)
        self.dve = Eng(self, "dve")
        self.pool = Eng(self, "pool", 12)
        self.sp = Eng(self, "sp", 24)
        self.engs = [self.pe, self.act, self.dve, self.pool, self.sp]
        self._pr = []
        self._pw = []

    def sb(self, name, shape, dt):
        return self.es.enter_context(self.nc.sbuf_tensor(name, list(shape), dt))

    def ps(self, name, shape, dt=F32):
        return self.es.enter_context(self.nc.psum_tensor(name, list(shape), dt))

    def dram(self, name, shape, dt, kind):
        return self.nc.dram_tensor(name, list(shape), dt, kind=kind).ap()

    def mm(self, out, lhsT, rhs, start, stop, r=(), w=(), last=False):
        pe = self.pe
        fn = (lambda e, o=out, l=lhsT, rr=rhs, s=start, t=stop: e.matmul(o, l, rr, start=s, stop=t))
        self._pr.extend(r)
        self._pw.extend(w)
        if last:
            h = pe.op(fn, r=self._pr, w=self._pw)
            self._pr = []
            self._pw = []
            return h
        pe.op_noinc(fn, r=r, w=w)
        return None

    def fence(self):
        deps = [e.mark() for e in self.engs] + list(self.dma_handles.values())
        for e in self.engs:
            e.wait(deps)

    def finish(self, final_deps):
        self.sp.wait(final_deps)
        self.fence()
        with self.nc.Block() as block:
            @block.tensor
            def _(e):
                self.pe.replay(e)

            @block.scalar
            def _(e):
                self.act.replay(e)

            @block.vector
            def _(e):
                self.dve.replay(e)

            @block.gpsimd
            def _(e):
                self.pool.replay(e)

            @block.sync
            def _(e):
                self.sp.replay(e)
        self.es.close()
        return self.nc


def regions(n):
    out = []
    c = 0
    while c < n:
        m = min(512, n - c)
        out.append((c, m))
        c += m
    return out


VOFF = {}
_o = 0
for _n, _w in [("g1", 16), ("b1", 32), ("bdw", 16), ("lng", 16), ("lnb", 16), ("b2", 16), ("wdw", 496),
               ("gf0", 16), ("gf1", 16), ("wc0", 132), ("wc1", 132), ("gkv", 16), ("gat", 16),
               ("kng", 1), ("qng", 1), ("hflag", 1), ("eps", 1), ("zero", 1)]:
    VOFF[_n] = _o
    _o += _w
NV = _o


def fm(v):
    v = np.asarray(v, np.float32).reshape(-1, 128)
    return np.ascontiguousarray(v.T)


def pack_vecs(inp, hflag):
    V = np.zeros((128, NV), np.float32)

    def put(name, arr):
        V[:, VOFF[name]:VOFF[name] + arr.shape[1]] = arr

    put("g1", fm(inp["conv_norm_g"][0]))
    put("b1", fm(inp["conv_b_pw1"][0]))
    put("bdw", fm(inp["conv_b_dw"][0]))
    put("lng", fm(inp["conv_ln_g"][0]))
    put("lnb", fm(inp["conv_ln_b"][0]))
    put("b2", fm(inp["conv_b_pw2"][0]))
    wdw = np.asarray(inp["conv_w_dw"][0], np.float32)
    put("wdw", np.ascontiguousarray(wdw.reshape(31, 16, 128).transpose(2, 1, 0)).reshape(128, 496))
    put("gf0", fm(inp["ffn_norm_g"][0]))
    put("gf1", fm(inp["ffn_norm_g"][1]))
    for l in range(2):
        wc = np.asarray(inp["ffn_w_conv"][l], np.float32)
        put("wc%d" % l, np.ascontiguousarray(wc.reshape(3, 44, 128).transpose(2, 0, 1)).reshape(128, 132))
    put("gkv", fm(inp["kv_norm_g"]))
    put("gat", fm(inp["attn_norm_g"][0]))
    put("kng", np.asarray(inp["k_norm_g"], np.float32).reshape(128, 1))
    put("qng", np.asarray(inp["q_norm_g"][0], np.float32).reshape(128, 1))
    V[:, VOFF["hflag"]] = hflag
    V[:, VOFF["eps"]] = EPS
    return V


class Ctx:
    pass


def setup_common(P):
    C = Ctx()
    C.P = P
    nc = P.nc
    C.vecs_d = P.dram("vecs", [128, NV], F32, "ExternalInput")
    C.ident_d = P.dram("ident", [128, 128], F32, "ExternalInput")
    C.R1 = P.sb("R1", [128, 16 * WIN + 8 * OW], F32)
    C.bufA = C.R1[:, 0:16 * WIN].rearrange("p (c t) -> p c t", t=WIN)
    C.bufC = C.R1[:, 16 * WIN:16 * WIN + 8 * OW].bitcast(BF16).rearrange("p (c t) -> p c t", t=OW)
    C.mbuf = C.R1[:, 0:FC * TT // 2].bitcast(BF16).rearrange("p (c t) -> p c t", t=TT)
    C.bufB_t = P.sb("bufB", [128, 16 * WIN], BF16)
    C.bufB = C.bufB_t[:, :].rearrange("p (c t) -> p c t", t=WIN)
    C.A = [Res() for _ in range(16)]
    C.B = [Res() for _ in range(16)]
    C.Cc = [Res() for _ in range(16)]
    C.M = [Res() for _ in range(FC)]
    C.wring = P.sb("wring", [128, NWSLOT * WSLOT], BF16)
    C.wres = [Res() for _ in range(NWSLOT)]
    C.wi = 0
    C.S = P.sb("S", [128, 7424], F32)
    C.vecs = P.sb("vecs_sb", [128, NV], F32)
    C.ident = P.sb("ident_sb", [128, 128], F32)
    C.ones = P.sb("ones_sb", [128, 128], BF16)
    C.psum = P.ps("psum", [128, 4096], F32)
    C.bank = [C.psum[:, i * 512:(i + 1) * 512] for i in range(8)]
    C.bres = [Res() for _ in range(8)]
    C.cres = Res()
    P.sp.dma(C.vecs[:], C.vecs_d, w=[C.cres])
    P.sp.dma(C.ident[:], C.ident_d, w=[C.cres])
    P.dve.op(lambda e: e.memset(C.ones[:], 1.0), w=[C.cres])
    P.fence()
    return C


def vcol(C, name, i=0):
    o = VOFF[name] + i
    return C.vecs[:, o:o + 1]


def wget(C, src, kcn, ncols):
    P = C.P
    k = C.wi
    C.wi = (k + 1) % NWSLOT
    view = C.wring[:, k * WSLOT:k * WSLOT + kcn * ncols].rearrange("p (k c) -> p k c", c=ncols)
    P.pool.dma(view, src.rearrange("(k p) c -> p k c", p=128), w=[C.wres[k]])
    return view, C.wres[k]


def scratch(C, off, n, dt=F32):
    if dt == F32:
        return C.S[:, off:off + n]
    return C.S[:, off:off + (n + 1) // 2].bitcast(BF16)[:, 0:n]


def phase_rmsnorm(C, W, gname):
    P = C.P
    sq = [scratch(C, 0, 512, BF16), scratch(C, 256, 512, BF16)]
    sqr = [Res(), Res()]
    rt = scratch(C, 512, W)
    rstd = scratch(C, 512 + WIN, W)
    rr = Res()
    cnt = 0
    for ri, (c0, n) in enumerate(regions(W)):
        bk = C.bank[ri]
        for c in range(16):
            s = cnt % 2
            cnt += 1
            P.act.op(lambda e, s=s, c=c, c0=c0, n=n: e.activation(out=sq[s][:, 0:n], in_=C.bufA[:, c, c0:c0 + n], func=AF.Square),
                     r=[C.A[c]], w=[sqr[s]])
            P.mm(bk[:, 0:n], C.ones[:], sq[s][:, 0:n], start=(c == 0), stop=(c == 15), r=[sqr[s], C.cres], w=[C.bres[ri]], last=True)
        P.act.op(lambda e, bk=bk, c0=c0, n=n: e.activation(out=rt[:, c0:c0 + n], in_=bk[:, 0:n], func=AF.Sqrt, bias=vcol(C, "eps"), scale=1.0 / D),
                 r=[C.bres[ri], C.cres], w=[rr])
        P.dve.op(lambda e, c0=c0, n=n: e.reciprocal(out=rstd[:, c0:c0 + n], in_=rt[:, c0:c0 + n]), r=[rr], w=[rr])
    for c in range(16):
        P.dve.op(lambda e, c=c: e.scalar_tensor_tensor(out=C.bufB[:, c, 0:W], in0=C.bufA[:, c, 0:W], scalar=vcol(C, gname, c),
                                                       in1=rstd[:, 0:W], op0=ALU.mult, op1=ALU.mult),
                 r=[C.A[c], rr, C.cres], w=[C.B[c]])


def load_window(C, src, col0, W):
    P = C.P
    for c in range(16):
        P.sp.dma(C.bufA[:, c, 0:W], src[:, c, col0:col0 + W], w=[C.A[c]])


def phase_conformer(C, tt, xT, X1A, hf):
    P = C.P
    w1 = C.w_pw1
    w2 = C.w_pw2
    col0 = tt * TT
    load_window(C, xT, col0, WIN)
    phase_rmsnorm(C, WIN, "g1")
    P.fence()
    sg = scratch(C, 0, WIN)
    u = [scratch(C, 1056, WIN, BF16), scratch(C, 1056 + 528, WIN, BF16)]
    dg = [scratch(C, 2112, 31 * 128, BF16).rearrange("p (k c) -> p k c", c=128),
          scratch(C, 2112 + 1984, 31 * 128, BF16).rearrange("p (k c) -> p k c", c=128)]
    vh = scratch(C, 6080, 4)
    junk = scratch(C, 6088, 32)
    sgr = Res()
    ur = [Res(), Res()]
    dgr = [Res(), Res()]
    vhr = Res()
    regs = regions(WIN)
    wdwv = C.vecs[:, VOFF["wdw"]:VOFF["wdw"] + 496].rearrange("p (j k) -> p j k", k=31)

    TD = 9
    cacc = scratch(C, 6120, TT)
    caccr = Res()

    def conv(j):
        s = j % 2
        for ro in range(2):
            bk = C.bank[6 + ro]
            for k in range(TD, 31):
                o0 = 2 + ro * 512 + k
                P.mm(bk[:, :], dg[s][:, k, :], u[s][:, o0:o0 + 512], start=(k == TD), stop=(k == 30),
                     r=[dgr[s], ur[s]], w=[C.bres[6 + ro]], last=(k == 30))
        for k in range(TD):
            if k == 0:
                P.dve.op(lambda e, s=s, j=j, k=k: e.tensor_scalar(out=cacc[:, :], in0=u[s][:, 2 + k:2 + k + TT], scalar1=vcol(C, "wdw", j * 31 + k),
                                                                scalar2=None, op0=ALU.mult), r=[ur[s], C.cres], w=[caccr])
            else:
                P.dve.op(lambda e, s=s, j=j, k=k: e.scalar_tensor_tensor(out=cacc[:, :], in0=u[s][:, 2 + k:2 + k + TT], scalar=vcol(C, "wdw", j * 31 + k),
                                                                       in1=cacc[:, :], op0=ALU.mult, op1=ALU.add), r=[ur[s], C.cres], w=[caccr])
        P.dve.op(lambda e, j=j: e.scalar_tensor_tensor(out=C.bufA[:, j, 2:2 + TT], in0=C.psum[:, 6 * 512:8 * 512], scalar=vcol(C, "bdw", j), in1=cacc[:, :],
                                                      op0=ALU.add, op1=ALU.add), r=[C.bres[6], C.bres[7], caccr, C.cres], w=[C.A[j]])
        for o in range(2):
            P.dve.op(lambda e, o=o, s=s, j=j: e.tensor_tensor(out=junk[:, 0:31], in0=u[s][:, o:o + 31], in1=wdwv[:, j, :], op=ALU.mult),
                     r=[ur[s], C.cres], w=[vhr])
            P.dve.op(lambda e, o=o: e.reduce_sum(out=vh[:, o:o + 1], in_=junk[:, 0:31], axis=AX.X), r=[vhr], w=[vhr])
        P.dve.op(lambda e, j=j: e.tensor_scalar(out=C.bufA[:, j, 0:2], in0=vh[:, 0:2], scalar1=vcol(C, "bdw", j), scalar2=None, op0=ALU.add),
                 r=[vhr, C.cres], w=[C.A[j]])

    for j in range(16):
        s = j % 2
        if j % 2 == 0:
            wa, war = wget(C, w1[:, j * 128:(j + 2) * 128], 16, 256)
            wg, wgr = wget(C, w1[:, D + j * 128:D + (j + 2) * 128], 16, 256)
        jc = (j % 2) * 128
        for kc in range(16):
            for ri, (c0, n) in enumerate(regs):
                P.mm(C.bank[ri][:, 0:n], wg[:, kc, jc:jc + 128], C.bufB[:, kc, c0:c0 + n], start=(kc == 0), stop=(kc == 15),
                     r=[wgr, C.B[kc]], w=[C.bres[ri]], last=(kc == 15))
        for ri, (c0, n) in enumerate(regs):
            P.act.op(lambda e, ri=ri, c0=c0, n=n, j=j: e.activation(out=sg[:, c0:c0 + n], in_=C.bank[ri][:, 0:n], func=AF.Sigmoid,
                                                                  bias=vcol(C, "b1", 16 + j), scale=1.0),
                     r=[C.bres[ri], C.cres], w=[sgr])
        for kc in range(16):
            for ri, (c0, n) in enumerate(regs):
                P.mm(C.bank[3 + ri][:, 0:n], wa[:, kc, jc:jc + 128], C.bufB[:, kc, c0:c0 + n], start=(kc == 0), stop=(kc == 15),
                     r=[war, C.B[kc]], w=[C.bres[3 + ri]], last=(kc == 15))
        for ri, (c0, n) in enumerate(regs):
            P.dve.op(lambda e, ri=ri, c0=c0, n=n, j=j, s=s: e.scalar_tensor_tensor(out=u[s][:, c0:c0 + n], in0=C.bank[3 + ri][:, 0:n],
                                                                               scalar=vcol(C, "b1", j), in1=sg[:, c0:c0 + n],
                                                                               op0=ALU.add, op1=ALU.mult),
                     r=[C.bres[3 + ri], sgr, C.cres], w=[ur[s]])
        if hf is not None:
            P.dve.op(lambda e, s=s: e.tensor_scalar(out=u[s][:, 0:HALO], in0=u[s][:, 0:HALO], scalar1=vcol(C, hf), scalar2=None, op0=ALU.mult),
                     r=[C.cres], w=[ur[s]])
        for k in range(TD, 31):
            P.act.op(lambda e, s=s, k=k, j=j: e.activation(out=dg[s][:, k, :], in_=C.ident[:], func=AF.Identity, scale=vcol(C, "wdw", j * 31 + k)),
                     r=[C.cres], w=[dgr[s]])
        if j >= 1:
            conv(j - 1)
    conv(15)
    P.fence()
    sq = [scratch(C, 0, OW, BF16), scratch(C, 520, OW, BF16)]
    vb = [scratch(C, 1040, OW, BF16), scratch(C, 1560, OW, BF16)]
    mean = scratch(C, 2080, OW)
    rstd = scratch(C, 2080 + OW, OW)
    t1 = [scratch(C, 2080 + 2 * OW, OW), scratch(C, 2080 + 3 * OW, OW)]
    sqr = [Res(), Res()]
    vbr = [Res(), Res()]
    mr = Res()
    t1r = [Res(), Res()]
    oregs = regions(OW)
    for c in range(16):
        s = c % 2
        P.act.op(lambda e, s=s, c=c: e.activation(out=sq[s][:, 0:OW], in_=C.bufA[:, c, 0:OW], func=AF.Square), r=[C.A[c]], w=[sqr[s]])
        P.dve.op(lambda e, s=s, c=c: e.tensor_copy(out=vb[s][:, 0:OW], in_=C.bufA[:, c, 0:OW]), r=[C.A[c]], w=[vbr[s]])
        for ri, (c0, n) in enumerate(oregs):
            P.mm(C.bank[ri][:, 0:n], C.ones[:], vb[s][:, c0:c0 + n], start=(c == 0), stop=(c == 15), r=[vbr[s], C.cres], w=[C.bres[ri]], last=True)
            P.mm(C.bank[3 + ri][:, 0:n], C.ones[:], sq[s][:, c0:c0 + n], start=(c == 0), stop=(c == 15), r=[sqr[s], C.cres], w=[C.bres[3 + ri]], last=True)
    for ri, (c0, n) in enumerate(oregs):
        P.act.op(lambda e, ri=ri, c0=c0, n=n: e.activation(out=mean[:, c0:c0 + n], in_=C.bank[ri][:, 0:n], func=AF.Identity, scale=1.0 / D),
                 r=[C.bres[ri]], w=[mr])
        P.dve.op(lambda e, c0=c0, n=n: e.tensor_tensor(out=t1[0][:, c0:c0 + n], in0=mean[:, c0:c0 + n], in1=mean[:, c0:c0 + n], op=ALU.mult),
                 r=[mr], w=[t1r[0]])
        P.dve.op(lambda e, ri=ri, c0=c0, n=n: e.scalar_tensor_tensor(out=t1[1][:, c0:c0 + n], in0=C.bank[3 + ri][:, 0:n], scalar=1.0 / D,
                                                                    in1=t1[0][:, c0:c0 + n], op0=ALU.mult, op1=ALU.subtract),
                 r=[C.bres[3 + ri], t1r[0]], w=[t1r[1]])
        P.act.op(lambda e, c0=c0, n=n: e.activation(out=t1[0][:, c0:c0 + n], in_=t1[1][:, c0:c0 + n], func=AF.Sqrt, bias=vcol(C, "eps"), scale=1.0),
                 r=[t1r[1], C.cres], w=[t1r[0]])
        P.dve.op(lambda e, c0=c0, n=n: e.reciprocal(out=rstd[:, c0:c0 + n], in_=t1[0][:, c0:c0 + n]), r=[t1r[0]], w=[mr])
    for c in range(16):
        s = c % 2
        P.dve.op(lambda e, s=s, c=c: e.tensor_tensor(out=t1[s][:, 0:OW], in0=C.bufA[:, c, 0:OW], in1=mean[:, 0:OW], op=ALU.subtract),
                 r=[C.A[c], mr], w=[t1r[s]])
        P.dve.op(lambda e, s=s: e.tensor_tensor(out=t1[s][:, 0:OW], in0=t1[s][:, 0:OW], in1=rstd[:, 0:OW], op=ALU.mult),
                 r=[mr], w=[t1r[s]])
        P.act.op(lambda e, s=s, c=c: e.activation(out=C.bufC[:, c, 0:OW], in_=t1[s][:, 0:OW], func=AF.Silu, bias=vcol(C, "lnb", c), scale=vcol(C, "lng", c)),
                 r=[t1r[s], C.cres], w=[C.Cc[c]])
    P.fence()
    xr = [scratch(C, 0, OW), scratch(C, OW, OW)]
    xrr = [Res(), Res()]
    for oc in range(16):
        s = oc % 2
        if oc % 2 == 0:
            wv, wr = wget(C, w2[:, oc * 128:(oc + 2) * 128], 16, 256)
        jc = (oc % 2) * 128
        bs = 3 * s
        P.sp.dma(xr[s][:, 0:OW], xT[:, oc, col0 + 30:col0 + 30 + OW], w=[xrr[s]])
        for kc in range(16):
            for ri, (c0, n) in enumerate(oregs):
                P.mm(C.bank[bs + ri][:, 0:n], wv[:, kc, jc:jc + 128], C.bufC[:, kc, c0:c0 + n], start=(kc == 0), stop=(kc == 15),
                     r=[wr, C.Cc[kc]], w=[C.bres[bs + ri]], last=(kc == 15))
        for ri, (c0, n) in enumerate(oregs):
            P.dve.op(lambda e, bs=bs, ri=ri, c0=c0, n=n, oc=oc, s=s: e.scalar_tensor_tensor(out=C.bufA[:, oc, c0:c0 + n], in0=C.bank[bs + ri][:, 0:n],
                                                                                       scalar=vcol(C, "b2", oc), in1=xr[s][:, c0:c0 + n],
                                                                                       op0=ALU.add, op1=ALU.add),
                     r=[C.bres[bs + ri], xrr[s], C.cres], w=[C.A[oc]])
        if tt == 0:
            P.act.dma(X1A[:, oc, 0:OW], C.bufA[:, oc, 0:OW], r=[C.A[oc]])
        else:
            P.act.dma(X1A[:, oc, 2 + tt * TT:2 + (tt + 1) * TT], C.bufA[:, oc, 2:OW], r=[C.A[oc]])
    P.fence()


def phase_ffn(C, tt, l, XRES, XOUT, res_halo, out_halo, hf):
    P = C.P
    w_in = C.w_in[l]
    w_out = C.w_out[l]
    phase_rmsnorm(C, OW, "gf%d" % l)
    P.fence()
    y = [scratch(C, 0, TT), scratch(C, TT, TT)]
    sl = [scratch(C, 2 * TT, TT), scratch(C, 3 * TT, TT)]
    yr = [Res(), Res()]
    sr = [Res(), Res()]
    wcn = "wc%d" % l
    for c in range(FC):
        s = c % 2
        if c % 2 == 0:
            wg, wgr = wget(C, w_in[:, c * 128:(c + 2) * 128], 16, 256)
            wu, wur = wget(C, w_in[:, FF + c * 128:FF + (c + 2) * 128], 16, 256)
        jc = (c % 2) * 128
        gb = 0 if s == 0 else 5
        gps = C.psum[:, gb * 512:gb * 512 + OW]
        for kc in range(16):
            for ri, (c0, n) in enumerate(regions(OW)):
                P.mm(C.bank[gb + ri][:, 0:n], wg[:, kc, jc:jc + 128], C.bufB[:, kc, c0:c0 + n], start=(kc == 0), stop=(kc == 15),
                     r=[wgr, C.B[kc]], w=[C.bres[gb + ri]], last=(kc == 15))
        gres = [C.bres[gb], C.bres[gb + 1], C.bres[gb + 2]]
        if hf is not None:
            P.dve.op(lambda e, gps=gps: e.tensor_scalar(out=gps[:, 0:2], in0=gps[:, 0:2], scalar1=vcol(C, hf), scalar2=None, op0=ALU.mult),
                     r=[C.cres], w=gres)
        P.act.op(lambda e, gps=gps, s=s, c=c: e.activation(out=y[s][:, :], in_=gps[:, 2:OW], func=AF.Identity, scale=vcol(C, wcn, 2 * FC + c)),
                 r=gres + [C.cres], w=[yr[s]])
        P.dve.op(lambda e, gps=gps, s=s, c=c: e.scalar_tensor_tensor(out=y[s][:, :], in0=gps[:, 1:OW - 1], scalar=vcol(C, wcn, FC + c), in1=y[s][:, :],
                                                                  op0=ALU.mult, op1=ALU.add), r=gres + [C.cres], w=[yr[s]])
        P.dve.op(lambda e, gps=gps, s=s, c=c: e.scalar_tensor_tensor(out=y[s][:, :], in0=gps[:, 0:OW - 2], scalar=vcol(C, wcn, c), in1=y[s][:, :],
                                                                  op0=ALU.mult, op1=ALU.add), r=gres + [C.cres], w=[yr[s]])
        P.act.op(lambda e, s=s: e.activation(out=sl[s][:, :], in_=y[s][:, :], func=AF.Silu), r=[yr[s]], w=[sr[s]])
        for kc in range(16):
            for ri in range(2):
                P.mm(C.bank[3 + ri][:, :], wu[:, kc, jc:jc + 128], C.bufB[:, kc, 2 + ri * 512:2 + ri * 512 + 512], start=(kc == 0), stop=(kc == 15),
                     r=[wur, C.B[kc]], w=[C.bres[3 + ri]], last=(kc == 15))
        P.dve.op(lambda e, s=s, c=c: e.tensor_tensor(out=C.mbuf[:, c, :], in0=C.psum[:, 3 * 512:5 * 512], in1=sl[s][:, :], op=ALU.mult),
                 r=[C.bres[3], C.bres[4], sr[s]], w=[C.M[c]])
    P.fence()
    xr = [scratch(C, 0, TT), scratch(C, TT, TT)]
    xo = [scratch(C, 2 * TT, TT), scratch(C, 3 * TT, TT)]
    xrr = [Res(), Res()]
    xor_ = [Res(), Res()]
    for oc in range(16):
        s = oc % 2
        wv, wr = wget(C, w_out[:, oc * 128:(oc + 1) * 128], FC, 128)
        P.sp.dma(xr[s][:, :], XRES[:, oc, res_halo + tt * TT:res_halo + (tt + 1) * TT], w=[xrr[s]])
        bs = 2 * s
        for c in range(FC):
            for ri in range(2):
                P.mm(C.bank[bs + ri][:, :], wv[:, c, :], C.mbuf[:, c, ri * 512:(ri + 1) * 512], start=(c == 0), stop=(c == FC - 1),
                     r=[wr, C.M[c]], w=[C.bres[bs + ri]], last=(c == FC - 1))
        P.dve.op(lambda e, bs=bs, s=s: e.tensor_tensor(out=xo[s][:, :], in0=C.psum[:, bs * 512:(bs + 2) * 512], in1=xr[s][:, :], op=ALU.add),
                 r=[C.bres[bs], C.bres[bs + 1], xrr[s]], w=[xor_[s]])
        P.act.dma(XOUT[:, oc, out_halo + tt * TT:out_halo + (tt + 1) * TT], xo[s][:, :], r=[xor_[s]])
    P.fence()


_CACHE = {}


def _ident():
    return np.eye(128, dtype=np.float32)


def phase_proj_hn(C, w, colbase, gname, scale, dst, dst_col0, km=None, km_col0=0, ncols=TT):
    P = C.P
    sqk = [scratch(C, 0, TT, BF16), scratch(C, 5300, TT, BF16)]
    rt = scratch(C, 512, TT)
    rs = scratch(C, 1536, TT)
    kf = scratch(C, 2560, TT)
    kb = [scratch(C, 3584, TT, BF16), scratch(C, 4096, TT, BF16)]
    kms = scratch(C, 4608, 4)
    r_sq = [Res(), Res()]
    r_rt, r_kf, r_km = Res(), Res(), Res()
    r_kb = [Res(), Res()]
    regs = regions(ncols)
    nr = len(regs)
    wcur = {}

    def proj(hc):
        s = hc % 2
        if hc % 2 == 0:
            wcur["w"] = wget(C, w[:, colbase + hc * 128:colbase + (hc + 2) * 128], 16, 256)
        wv, wr = wcur["w"]
        jc = (hc % 2) * 128
        bs = 2 * s
        for kc in range(16):
            for ri, (c0, n) in enumerate(regs):
                P.mm(C.bank[bs + ri][:, 0:n], wv[:, kc, jc:jc + 128], C.bufB[:, kc, c0:c0 + n], start=(kc == 0), stop=(kc == 15),
                     r=[wr, C.B[kc]], w=[C.bres[bs + ri]], last=(kc == 15))
        yps = C.psum[:, bs * 512:bs * 512 + ncols]
        yres = [C.bres[bs + ri] for ri in range(nr)]
        P.act.op(lambda e, yps=yps: e.activation(out=sqk[s][:, 0:ncols], in_=yps, func=AF.Square), r=yres, w=[r_sq[s]])

    def epi(hc):
        s = hc % 2
        bs = 2 * s
        yps = C.psum[:, bs * 512:bs * 512 + ncols]
        yres = [C.bres[bs + ri] for ri in range(nr)]
        for ri, (c0, n) in enumerate(regs):
            P.mm(C.bank[4 + ri][:, 0:n], C.ones[:], sqk[s][:, c0:c0 + n], start=True, stop=True, r=[r_sq[s], C.cres], w=[C.bres[4 + ri]], last=True)
        P.act.op(lambda e: e.activation(out=rt[:, 0:ncols], in_=C.psum[:, 4 * 512:4 * 512 + ncols], func=AF.Sqrt, bias=vcol(C, "eps"), scale=1.0 / 128),
                 r=[C.bres[4 + ri] for ri in range(nr)] + [C.cres], w=[r_rt])
        P.dve.op(lambda e: e.reciprocal(out=rs[:, 0:ncols], in_=rt[:, 0:ncols]), r=[r_rt], w=[r_rt])
        P.dve.op(lambda e, yps=yps: e.scalar_tensor_tensor(out=kf[:, 0:ncols], in0=yps, scalar=vcol(C, gname), in1=rs[:, 0:ncols], op0=ALU.mult, op1=ALU.mult),
                 r=yres + [r_rt, C.cres], w=[r_kf])
        P.act.op(lambda e, s=s: e.activation(out=kb[s][:, 0:ncols], in_=kf[:, 0:ncols], func=AF.Identity, scale=float(scale)), r=[r_kf], w=[r_kb[s]])
        P.act.dma(dst[:, hc, dst_col0:dst_col0 + ncols], kb[s][:, 0:ncols], r=[r_kb[s]])
        if km is not None:
            P.dve.op(lambda e: e.tensor_reduce(out=kms[:, 0:4], in_=kf[:, :].rearrange("p (n j) -> p n j", j=256), axis=AX.X, op=ALU.add),
                     r=[r_kf], w=[r_km])
            P.act.op(lambda e, hc=hc: e.activation(out=km[:, hc, km_col0:km_col0 + 4], in_=kms[:, 0:4], func=AF.Identity, scale=1.0 / 256),
                     r=[r_km], w=[C.kmres])

    for hc in range(16):
        proj(hc)
        if hc >= 1:
            epi(hc - 1)
    epi(15)


def phase_v(C, w, colbase, V_d, tok0):
    P = C.P
    vst = [scratch(C, 5000, 256, BF16), scratch(C, 5128, 256, BF16)]
    vr = [Res(), Res()]
    for vb in range(8):
        wv, wr = wget(C, w[:, colbase + vb * 256:colbase + (vb + 1) * 256], 16, 256)
        for tk in range(8):
            s = tk % 2
            bk = 6 + s
            for kc in range(16):
                P.mm(C.bank[bk][:, 0:256], C.bufB[:, kc, tk * 128:(tk + 1) * 128], wv[:, kc, :], start=(kc == 0), stop=(kc == 15),
                     r=[wr, C.B[kc]], w=[C.bres[bk]], last=(kc == 15))
            P.act.op(lambda e, s=s, bk=bk: e.activation(out=vst[s][:, :], in_=C.bank[bk][:, 0:256], func=AF.Identity), r=[C.bres[bk]], w=[vr[s]])
            P.act.dma(V_d[tok0 + tk * 128:tok0 + (tk + 1) * 128, vb * 256:(vb + 1) * 256], vst[s][:, :], r=[vr[s]])


def r1v(C, off, n, dt=F32):
    if dt == F32:
        return C.R1[:, off:off + n]
    return C.R1[:, off:off + (n + 1) // 2].bitcast(BF16)[:, 0:n]


def attention_setup(C, D_):
    P = C.P
    A = Ctx()
    A.Qb = [r1v(C, 0, 8192, BF16).rearrange("p (h q) -> p h q", q=512), C.bufB_t[:, 0:8192].rearrange("p (h q) -> p h q", q=512)]
    A.KTh = [r1v(C, 4096, 4096, BF16), r1v(C, 6144, 4096, BF16)]
    A.Vh = [r1v(C, 8192, 4096, BF16).rearrange("p (c d) -> p c d", d=128), r1v(C, 10240, 4096, BF16).rearrange("p (c d) -> p c d", d=128)]
    A.Rt = [r1v(C, 12288, 8192, BF16).rearrange("p (h q) -> p h q", q=512), C.bufB_t[:, 8192:16384].rearrange("p (h q) -> p h q", q=512)]
    A.pt = [r1v(C, 16384 + i * 256, 512, BF16) for i in range(4)]
    A.ssb = [r1v(C, 17408, 512), r1v(C, 17920, 512)]
    A.acc = [r1v(C, 18432, 512), r1v(C, 18944, 512)]
    A.rl = [r1v(C, 19456, 512), r1v(C, 19968, 512)]
    A.ot = [r1v(C, 20480, 512, BF16), r1v(C, 20736, 512, BF16)]
    A.gm = r1v(C, 20992, 256)
    A.sel = r1v(C, 21248, 256)
    A.rbq = r1v(C, 21504, 256, BF16)
    A.mx = r1v(C, 21632, 128).rearrange("p (h k) -> p h k", k=8)
    A.thr = r1v(C, 21760, 16)
    A.Em = r1v(C, 21776, 2048, BF16).rearrange("p (n j) -> p n j", j=128)
    A.cmask = r1v(C, 22800, 1024).rearrange("p (t q) -> p t q", t=2)
    A.kmb = r1v(C, 23824, 256, BF16).rearrange("p (h n) -> p h n", n=16)
    A.identb = r1v(C, 23952, 128, BF16)
    A.ones32 = r1v(C, 24016, 128)
    A.pastm = C.S[:, 0:2304].rearrange("p (b x) -> p b x", x=256)
    A.ownm = C.S[:, 2304:4608].rearrange("p (b x) -> p b x", x=256)
    A.r_const = Res()
    A.r_Qb = [Res(), Res()]
    A.r_Rt = [Res(), Res()]
    A.r_KT = [Res(), Res()]
    A.r_V = [Res(), Res()]
    A.r_pt = [Res() for _ in range(4)]
    A.r_ssb = [Res(), Res()]
    A.r_acc = [Res(), Res()]
    A.r_g = Res()
    A.r_rl = [Res(), Res()]
    A.r_ot = [Res(), Res()]
    w = [A.r_const]
    for bf in range(2):
        P.dve.op(lambda e, bf=bf: e.memset(A.Rt[bf][:, :, :], 0.0), w=[A.r_Rt[bf]])
    P.dve.op(lambda e: e.memset(A.ones32[:, :], 1.0), w=w)
    P.sp.dma(A.Em[0:34, :, :], D_["emat"], w=w)
    P.sp.dma(A.cmask[:, :, :], D_["cmask"].rearrange("p (t q) -> p t q", t=2), w=w)
    P.sp.dma(A.pastm[:, :, :], D_["pastm"].rearrange("p (b x) -> p b x", x=256), w=w)
    P.sp.dma(A.ownm[:, :, :], D_["ownm"].rearrange("p (b x) -> p b x", x=256), w=w)
    P.act.op(lambda e: e.activation(out=A.kmb[:, :, :], in_=C.kmT[:, :].rearrange("p (h n) -> p h n", n=16), func=AF.Identity), r=[C.kmres], w=w)
    P.act.op(lambda e: e.activation(out=A.identb[:, :], in_=C.ident[:], func=AF.Identity), r=[C.cres], w=w)
    return A


def attention_all(C, A, units, QT_d, KT_d, V_d, OT_d, qal_d):
    P = C.P
    cst = [A.r_const]
    GB = 3
    gps = C.bank[GB]
    tps = C.psum[:, GB * 512 + 256:(GB + 1) * 512].bitcast(BF16)

    def gating_steps(ui):
        i0, nqb = units[ui]
        bf = ui % 2
        NQc = nqb * 256
        q0 = i0 * 256
        Qb, Rt = A.Qb[bf], A.Rt[bf]
        steps = []

        def load():
            P.sp.dma(Qb[:, :, 0:NQc], QT_d[:, :, q0:q0 + NQc], w=[A.r_Qb[bf]])
            P.sp.dma(Rt[32:34, :, 0:NQc], qal_d[:, :, q0:q0 + NQc], w=[A.r_Rt[bf]])
        steps.append(load)
        for qt in range(NQc // 128):
            qi = i0 + qt // 2

            def stepA(qt=qt, qi=qi):
                for h in range(16):
                    P.mm(gps[:, h * 16:(h + 1) * 16], Qb[:, h, qt * 128:(qt + 1) * 128], A.kmb[:, h, :], start=True, stop=True,
                         r=[A.r_Qb[bf]] + cst, w=[C.bres[GB]], last=(h == 15))
                P.dve.op(lambda e: e.tensor_tensor(out=A.gm[:, :], in0=gps[:, 0:256], in1=A.pastm[:, qi, :], op=ALU.add),
                         r=[C.bres[GB]] + cst, w=[A.r_g])
                for h in range(16):
                    P.dve.op(lambda e, h=h: e.max(out=A.mx[:, h, :], in_=A.gm[:, h * 16:(h + 1) * 16]), r=[A.r_g], w=[A.r_g])
                P.dve.op(lambda e: e.tensor_scalar(out=A.thr[:, :], in0=A.mx[:, :, 2], scalar1=-1e29, scalar2=None, op0=ALU.max), r=[A.r_g], w=[A.r_g])
                for h in range(16):
                    P.dve.op(lambda e, h=h: e.tensor_scalar(out=A.sel[:, h * 16:(h + 1) * 16], in0=A.gm[:, h * 16:(h + 1) * 16], scalar1=A.thr[:, h:h + 1],
                                                            scalar2=None, op0=ALU.is_ge), r=[A.r_g], w=[A.r_g])
                P.dve.op(lambda e: e.tensor_tensor(out=A.sel[:, :], in0=A.sel[:, :], in1=A.ownm[:, qi, :], op=ALU.max), r=[A.r_g] + cst, w=[A.r_g])
                P.dve.op(lambda e: e.tensor_scalar(out=A.rbq[:, :], in0=A.sel[:, :], scalar1=-1.0, scalar2=30000.0, op0=ALU.add, op1=ALU.mult),
                         r=[A.r_g], w=[A.r_g])

            def stepB(qt=qt):
                for hg in range(4):
                    for hh in range(4):
                        h = hg * 4 + hh
                        P.pe.op(lambda e, h=h, hh=hh: e.transpose(tps[0:16, hh * 128:(hh + 1) * 128], A.rbq[:, h * 16:(h + 1) * 16], A.identb[:, :]),
                                r=[A.r_g] + cst, w=[C.bres[GB]])
                    P.act.op(lambda e, hg=hg: e.activation(out=Rt[0:16, hg * 4:(hg + 1) * 4, qt * 128:(qt + 1) * 128],
                                                           in_=tps[0:16, :].rearrange("p (h q) -> p h q", q=128), func=AF.Identity),
                             r=[C.bres[GB]], w=[A.r_Rt[bf]])
            steps.append(stepA)
            steps.append(stepB)
        return steps

    def unit_tiles(ui):
        i0, nqb = units[ui]
        NQc = nqb * 256
        nkb = 8 + i0 + nqb - 1
        tiles = []
        for h in range(16):
            lst = []
            for n in range(nkb):
                for jt in range(2):
                    if n <= 7 + i0:
                        lst.append((n, jt, 0, NQc))
                    else:
                        lst.append((n, jt, 256, 256))
            for ist, (n, jt, c0, ncl) in enumerate(lst):
                tiles.append((h, n, jt, c0, ncl, ist, len(lst)))
        return tiles

    cnt = {"k": 0}
    for st in gating_steps(0):
        st()
    for ui, (i0, nqb) in enumerate(units):
        bf = ui % 2
        NQc = nqb * 256
        nkb = 8 + i0 + nqb - 1
        nk = nkb * 256
        q0 = i0 * 256
        Qb, Rt = A.Qb[bf], A.Rt[bf]
        tiles = unit_tiles(ui)
        nxt = gating_steps(ui + 1) if ui + 1 < len(units) else []

        def load_head(h):
            s = h % 2
            P.sp.dma(A.KTh[s][:, 0:nk], KT_d[:, h, 0:nk], w=[A.r_KT[s]])
            P.sp.dma(A.Vh[s][:, 0:nkb * 2, :], V_d[0:nk, h * 128:(h + 1) * 128].rearrange("(c p) d -> p c d", p=128), w=[A.r_V[s]])

        def score(k, kg):
            h, n, jt, c0, ncl, ist, nst = tiles[k]
            s = h % 2
            if ist == 0:
                load_head(h)
            sb_ = C.bank[kg % 3]
            sr = C.bres[kg % 3]
            P.mm(sb_[:, 0:ncl], A.KTh[s][:, n * 256 + jt * 128:n * 256 + (jt + 1) * 128], Qb[:, h, c0:c0 + ncl], start=True, stop=False,
                 r=[A.r_KT[s], A.r_Qb[bf]], w=[sr])
            P.mm(sb_[:, 0:ncl], A.Em[0:34, n, :], Rt[0:34, h, c0:c0 + ncl], start=False, stop=True, r=[A.r_Rt[bf]] + cst, w=[sr], last=True)
            p = kg % 4
            bias = C.kalf[:, h, n * 2 + jt:n * 2 + jt + 1]
            if n >= 7 + i0:
                x = kg % 2
                P.dve.op(lambda e: e.tensor_tensor(out=A.ssb[x][:, 0:ncl], in0=sb_[:, 0:ncl], in1=A.cmask[:, jt, 0:ncl], op=ALU.add),
                         r=[sr] + cst, w=[A.r_ssb[x]])
                P.act.op(lambda e: e.activation(out=A.pt[p][:, 0:ncl], in_=A.ssb[x][:, 0:ncl], func=AF.Exp, bias=bias, scale=1.0),
                         r=[A.r_ssb[x]] + cst, w=[A.r_pt[p]])
            else:
                P.act.op(lambda e: e.activation(out=A.pt[p][:, 0:ncl], in_=sb_[:, 0:ncl], func=AF.Exp, bias=bias, scale=1.0),
                         r=[sr] + cst, w=[A.r_pt[p]])
            if ist == 0:
                P.dve.op(lambda e, NQc=NQc: e.tensor_copy(out=A.acc[s][:, 0:NQc], in_=A.pt[p][:, 0:NQc]), r=[A.r_pt[p]], w=[A.r_acc[s]])
            else:
                P.dve.op(lambda e: e.tensor_tensor(out=A.acc[s][:, c0:c0 + ncl], in0=A.acc[s][:, c0:c0 + ncl], in1=A.pt[p][:, 0:ncl], op=ALU.add),
                         r=[A.r_pt[p]], w=[A.r_acc[s]])

        def pvl(k, kg):
            h, n, jt, c0, ncl, ist, nst = tiles[k]
            s = h % 2
            p = kg % 4
            obi = 4 + 2 * s
            ob = C.bank[obi]
            lb = C.bank[obi + 1]
            lastst = (ist == nst - 1)
            P.mm(ob[:, c0:c0 + ncl], A.Vh[s][:, n * 2 + jt, :], A.pt[p][:, 0:ncl], start=(ist == 0), stop=lastst, r=[A.r_V[s], A.r_pt[p]], w=[C.bres[obi]], last=True)
            if lastst:
                P.pe.op(lambda e, NQc=NQc: e.matmul(lb[:, 0:NQc], A.ones32[:, :], A.acc[s][:, 0:NQc], start=True, stop=True),
                        r=[A.r_acc[s]] + cst, w=[C.bres[obi + 1]])
                P.dve.op(lambda e, NQc=NQc: e.reciprocal(out=A.rl[s][:, 0:NQc], in_=lb[:, 0:NQc]), r=[C.bres[obi + 1]], w=[A.r_rl[s]])
                P.dve.op(lambda e, NQc=NQc: e.tensor_tensor(out=A.ot[s][:, 0:NQc], in0=ob[:, 0:NQc], in1=A.rl[s][:, 0:NQc], op=ALU.mult),
                         r=[C.bres[obi], A.r_rl[s]], w=[A.r_ot[s]])
                P.act.dma(OT_d[:, h, q0:q0 + NQc], A.ot[s][:, 0:NQc], r=[A.r_ot[s]])

        LA = 2
        nt = len(tiles)
        sched = {}
        for si, st in enumerate(nxt):
            sched.setdefault(((si + 1) * nt) // (len(nxt) + 1), []).append(st)
        base = cnt["k"]
        for k in range(nt + LA):
            if k < nt:
                score(k, base + k)
            if k - LA >= 0:
                pvl(k - LA, base + k - LA)
            for st in sched.get(k, []):
                st()
        cnt["k"] = base + nt


LSEQ = 4096
QPRE = 1792
NQ = LSEQ - QPRE
QTILES = [(1792, 256), (2048, 1024), (3072, 1024)]


def build_F():
    P = Prog()
    C = setup_common(P)
    xT = P.dram("xT", [128, 16, HALO + LSEQ], F32, "ExternalInput")
    C.w_pw1 = P.dram("w_pw1", [D, 2 * D], F32, "ExternalInput")
    C.w_pw2 = P.dram("w_pw2", [D, D], F32, "ExternalInput")
    C.w_in = [P.dram("w_in0", [D, 2 * FF], F32, "ExternalInput"), P.dram("w_in1", [D, 2 * FF], F32, "ExternalInput")]
    C.w_out = [P.dram("w_out0", [FF, D], F32, "ExternalInput"), P.dram("w_out1", [FF, D], F32, "ExternalInput")]
    w_kv = P.dram("w_kv", [D, 2 * D], F32, "ExternalInput")
    w_q = P.dram("w_q", [D, D], F32, "ExternalInput")
    w_o = P.dram("w_o", [D, D], F32, "ExternalInput")
    D_ = {"emat": P.dram("emat", [34, 16, 128], BF16, "ExternalInput"),
          "kal": P.dram("kal", [128, 32], F32, "ExternalInput"),
          "cmask": P.dram("cmask", [128, 1024], F32, "ExternalInput"),
          "pastm": P.dram("pastm", [128, 2304], F32, "ExternalInput"),
          "ownm": P.dram("ownm", [128, 2304], F32, "ExternalInput")}
    kalf_d = P.dram("kalf", [128, 16 * 32], F32, "ExternalInput")
    qal_d = P.dram("qal", [2, 16, NQ], BF16, "ExternalInput")
    OUT = P.dram("outT", [128, 16, TCORE], F32, "ExternalOutput")
    X1A = P.dram("X1A", [128, 16, 2 + LSEQ], F32, "Internal")
    X1 = P.dram("X1", [128, 16, 2 + LSEQ], F32, "Internal")
    X2 = P.dram("X2", [128, 16, 256 + TCORE], F32, "Internal")
    KT_d = P.dram("KT_d", [128, 16, LSEQ], BF16, "Internal")
    V_d = P.dram("V_d", [LSEQ, D], BF16, "Internal")
    QT_d = P.dram("QT_d", [128, 16, NQ], BF16, "Internal")
    OT_d = P.dram("OT_d", [128, 16, NQ], BF16, "Internal")
    C.kmT = P.sb("kmT", [128, 256], F32)
    C.kmres = Res()
    kalf_t = P.sb("kalf_sb", [128, 512], F32)
    C.kalf = kalf_t[:, :].rearrange("p (h c) -> p h c", c=32)
    P.sp.dma(kalf_t[:, :], kalf_d, w=[C.cres])
    kmv = C.kmT[:, :].rearrange("p (h n) -> p h n", n=16)
    for tt in range(4):
        hf = "zero" if tt == 0 else ("hflag" if tt == 2 else None)
        phase_conformer(C, tt, xT, X1A, hf)
        phase_ffn(C, tt, 0, X1A, X1, 2, 2, hf)
    for t4 in range(4):
        load_window(C, X1, 2 + t4 * TT, TT)
        phase_rmsnorm(C, TT, "gkv")
        P.fence()
        phase_proj_hn(C, w_kv, 0, "kng", 1.0, KT_d, t4 * TT, km=kmv, km_col0=t4 * 4)
        phase_v(C, w_kv, D, V_d, t4 * TT)
        P.fence()
    for (st, n) in QTILES:
        load_window(C, X1, 2 + st, n)
        phase_rmsnorm(C, n, "gat")
        P.fence()
        phase_proj_hn(C, w_q, 0, "qng", 128.0 ** -0.5, QT_d, st - QPRE, ncols=n)
        P.fence()
    A = attention_setup(C, D_)
    P.fence()
    attention_all(C, A, [(0, 1), (1, 2), (3, 2), (5, 2), (7, 2)], QT_d, KT_d, V_d, OT_d, qal_d)
    P.fence()
    xr = [scratch(C, 0, TT), scratch(C, TT, TT)]
    xo = [scratch(C, 2 * TT, TT), scratch(C, 3 * TT, TT)]
    xrr = [Res(), Res()]
    xor_ = [Res(), Res()]
    for (st, n) in QTILES:
        regs = regions(n)
        for c in range(16):
            P.sp.dma(C.bufB[:, c, 0:n], OT_d[:, c, st - QPRE:st - QPRE + n], w=[C.B[c]])
        for oc in range(16):
            s = oc % 2
            if oc % 2 == 0:
                wv, wr = wget(C, w_o[:, oc * 128:(oc + 2) * 128], 16, 256)
            jc = (oc % 2) * 128
            bs = 2 * s
            P.sp.dma(xr[s][:, 0:n], X1[:, oc, 2 + st:2 + st + n], w=[xrr[s]])
            for kc in range(16):
                for ri, (c0, m) in enumerate(regs):
                    P.mm(C.bank[bs + ri][:, 0:m], wv[:, kc, jc:jc + 128], C.bufB[:, kc, c0:c0 + m], start=(kc == 0), stop=(kc == 15),
                         r=[wr, C.B[kc]], w=[C.bres[bs + ri]], last=(kc == 15))
            P.dve.op(lambda e, bs=bs, s=s, n=n: e.tensor_tensor(out=xo[s][:, 0:n], in0=C.psum[:, bs * 512:bs * 512 + n], in1=xr[s][:, 0:n], op=ALU.add),
                     r=[C.bres[bs + ri] for ri in range(len(regs))] + [xrr[s]], w=[xor_[s]])
            P.act.dma(X2[:, oc, st - QPRE:st - QPRE + n], xo[s][:, 0:n], r=[xor_[s]])
        P.fence()
    for t in range(NTILE):
        load_window(C, X2, 254 + t * TT, OW)
        phase_ffn(C, t, 1, X2, OUT, 256, 0, "hflag" if t == 0 else None)
    return P.finish([])


SLOPES = np.exp2(-8.0 * np.arange(1, NH + 1, dtype=np.float64) / NH)


def attn_tables(half):
    T = {}
    em = np.zeros((34, 16, 128), np.float32)
    for n in range(16):
        em[n, n, :] = 1.0
    em[32:34, :, :] = 1.0
    T["emat"] = em.astype(ml_dtypes.bfloat16)
    T["kal"] = np.zeros((128, 32), np.float32)
    pos = (np.arange(32)[None, :] * 128 + np.arange(128)[:, None]).astype(np.float64)
    T["kalf"] = (SLOPES[None, :, None] * pos[:, None, :]).astype(np.float32).reshape(128, 512)
    tq = QPRE + np.arange(NQ, dtype=np.float64)
    v = -(SLOPES[:, None] * tq[None, :])
    hi = v.astype(np.float32).astype(ml_dtypes.bfloat16)
    lo = (v - hi.astype(np.float64)).astype(np.float32).astype(ml_dtypes.bfloat16)
    T["qal"] = np.stack([hi, lo], 0)
    kp = np.arange(2)[None, :, None] * 128 + np.arange(128)[:, None, None]
    cm = np.zeros((128, 2, 512), np.float32)
    cm[:, :, 0:256] = np.where(kp > np.arange(256)[None, None, :], -30000.0, 0.0)
    T["cmask"] = cm.reshape(128, 1024)
    pm = np.full((9, 16, 16), -1e30, np.float32)
    om = np.zeros((9, 16, 16), np.float32)
    nlo = 0 if half == 1 else 8
    for i in range(9):
        cur = 7 + i
        pm[i, :, nlo:cur] = 0.0
        om[i, :, cur] = 1.0
    T["pastm"] = np.broadcast_to(pm.reshape(1, 2304), (128, 2304)).copy()
    T["ownm"] = np.broadcast_to(om.reshape(1, 2304), (128, 2304)).copy()
    return T


_CACHE = {}


def kernel(**inputs):
    inp = {k: np.asarray(v) for k, v in inputs.items()}
    cores = list(range(8))
    if "F" not in _CACHE:
        _CACHE["F"] = build_F()
    x = inp["x"].astype(np.float32, copy=False)
    ws = {"w_pw1": np.ascontiguousarray(inp["conv_w_pw1"][0]), "w_pw2": np.ascontiguousarray(inp["conv_w_pw2"][0]),
          "w_in0": np.ascontiguousarray(inp["ffn_w_in"][0]), "w_out0": np.ascontiguousarray(inp["ffn_w_out"][0]),
          "w_in1": np.ascontiguousarray(inp["ffn_w_in"][1]), "w_out1": np.ascontiguousarray(inp["ffn_w_out"][1]),
          "w_kv": np.ascontiguousarray(inp["w_kv"]), "w_q": np.ascontiguousarray(inp["w_q"][0]), "w_o": np.ascontiguousarray(inp["w_o"][0])}
    tabs = [attn_tables(0), attn_tables(1)]
    ident = np.eye(128, dtype=np.float32)
    maps = []
    for c in cores:
        b, h = c // 2, c % 2
        xt = np.ascontiguousarray(x[b].T).reshape(16, 128, 4096).transpose(1, 0, 2)
        buf = np.zeros((128, 16, HALO + LSEQ), np.float32)
        if h == 1:
            buf[:, :, HALO:] = xt
        else:
            buf[:, :, HALO + TCORE:] = xt[:, :, 0:TCORE]
        maps.append(dict(xT=buf, vecs=pack_vecs(inp, float(h)), ident=ident, **ws, **tabs[h]))
    res = run_bass_kernel_spmd(_CACHE["F"], maps, core_ids=cores)
    out = np.zeros((4, 4096, D), np.float32)
    for c in cores:
        b, h = c // 2, c % 2
        o = res.results[c]["outT"]
        out[b, h * TCORE:(h + 1) * TCORE, :] = o.transpose(2, 1, 0).reshape(TCORE, D)
    return out
```

```python
import numpy as np
import ml_dtypes
import concourse.bass as bass
import concourse.mybir as mybir
from concourse.bass_utils import run_bass_kernel_spmd
from contextlib import ExitStack

F32 = mybir.dt.float32
BF16 = mybir.dt.bfloat16
AF = mybir.ActivationFunctionType
ALU = mybir.AluOpType
AX = mybir.AxisListType

D = 2048
KC = 16
TT = 1024
NTILE = 2
TCORE = 2048
HALO = 32
WIN = TT + HALO
OW = TT + 2
FF = 5632
FC = 44
NH = 16
EPS = 1e-6
NBLK = 16
WSLOT = 44 * 128
NWSLOT = 3


class Res:
    __slots__ = ("lw", "rd")

    def __init__(self):
        self.lw = None
        self.rd = {}


class Eng:
    def __init__(self, P, name, nsem_dma=0):
        self.P = P
        self.name = name
        self.sem = P.es.enter_context(P.nc.semaphore("sem_" + name))
        self.n = 0
        self.waited = {}
        self.rec = []
        self.dsems = [P.es.enter_context(P.nc.semaphore("d%s%d" % (name, i))) for i in range(nsem_dma)]
        self.dvals = [0] * nsem_dma
        self.di = 0

    def wait(self, deps):
        for d in deps:
            if d is None:
                continue
            sem, val, key = d
            if self.waited.get(key, 0) >= val:
                continue
            self.rec.append(("w", sem, val))
            self.waited[key] = val

    @staticmethod
    def _deps(r, w):
        d = []
        for x in r:
            if x.lw is not None:
                d.append(x.lw)
        for x in w:
            if x.lw is not None:
                d.append(x.lw)
            d.extend(x.rd.values())
        return d

    @staticmethod
    def _commit(h, r, w):
        for x in r:
            x.rd[h[2]] = h
        for x in w:
            x.lw = h
            x.rd = {}

    def op(self, fn, r=(), w=(), deps=()):
        self.wait(self._deps(r, w))
        self.wait(deps)
        self.n += 1
        self.rec.append(("i", fn, self.sem, 1))
        h = (self.sem, self.n, self.name)
        self._commit(h, r, w)
        return h

    def op_noinc(self, fn, r=(), w=()):
        self.wait(self._deps(r, w))
        self.rec.append(("n", fn))

    def mark(self):
        return (self.sem, self.n, self.name) if self.n else None

    def dma(self, out, in_, r=(), w=(), deps=(), **kw):
        k = self.di
        self.di = (self.di + 1) % len(self.dsems)
        key = "%s_d%d" % (self.name, k)
        if self.dvals[k] > 0:
            self.wait([(self.dsems[k], self.dvals[k], key)])
        self.wait(self._deps(r, w))
        self.wait(deps)
        self.dvals[k] += 16
        self.rec.append(("i", (lambda e, o=out, i=in_, kw=kw: e.dma_start(out=o, in_=i, **kw)), self.dsems[k], 16))
        h = (self.dsems[k], self.dvals[k], key)
        self._commit(h, r, w)
        self.P.dma_handles[key] = h
        return h

    def replay(self, e):
        for r in self.rec:
            if r[0] == "w":
                e.wait_ge(r[1], r[2])
            elif r[0] == "n":
                r[1](e)
            else:
                r[1](e).then_inc(r[2], r[3])


class Prog:
    def __init__(self):
        self.nc = bass.Bass("TRN2", target_bir_lowering=False)
        self.es = ExitStack()
        self.dma_handles = {}
        self.pe = Eng(self, "pe")
        self.act = Eng(self, "act", 10)
        self.dve = Eng(self, "dve")
        self.pool = Eng(self, "pool", 12)
        self.sp = Eng(self, "sp", 24)
        self.engs = [self.pe, self.act, self.dve, self.pool, self.sp]
        self._pr = []
        self._pw = []

    def sb(self, name, shape, dt):
        return self.es.enter_context(self.nc.sbuf_tensor(name, list(shape), dt))

    def ps(self, name, shape, dt=F32):
        return self.es.enter_context(self.nc.psum_tensor(name, list(shape), dt))

    def dram(self, name, shape, dt, kind):
        return self.nc.dram_tensor(name, list(shape), dt, kind=kind).ap()

    def mm(self, out, lhsT, rhs, start, stop, r=(), w=(), last=False):
        pe = self.pe
        fn = (lambda e, o=out, l=lhsT, rr=rhs, s=start, t=stop: e.matmul(o, l, rr, start=s, stop=t))
        self._pr.extend(r)
        self._pw.extend(w)
        if last:
            h = pe.op(fn, r=self._pr, w=self._pw)
            self._pr = []
            self._pw = []
            return h
        pe.op_noinc(fn, r=r, w=w)
        return None

    def fence(self):
        deps = [e.mark() for e in self.engs] + list(self.dma_handles.values())
        for e in self.engs:
            e.wait(deps)

    def finish(self, final_deps):
        self.sp.wait(final_deps)
        self.fence()
        with self.nc.Block() as block:
            @block.tensor
            def _(e):
                self.pe.replay(e)

            @block.scalar
            def _(e):
                self.act.replay(e)

            @block.vector
            def _(e):
                self.dve.replay(e)

            @block.gpsimd
            def _(e):
                self.pool.replay(e)

            @block.sync
            def _(e):
                self.sp.replay(e)
        self.es.close()
        return self.nc


def regions(n):
    out = []
    c = 0
    while c < n:
        m = min(512, n - c)
        out.append((c, m))
        c += m
    return out


VOFF = {}
_o = 0
for _n, _w in [("g1", 16), ("b1", 32), ("bdw", 16), ("lng", 16), ("lnb", 16), ("b2", 16), ("wdw", 496),
               ("gf0", 16), ("gf1", 16), ("wc0", 132), ("wc1", 132), ("gkv", 16), ("gat", 16),
               ("kng", 1), ("qng", 1), ("hflag", 1), ("eps", 1), ("zero", 1)]:
    VOFF[_n] = _o
    _o += _w
NV = _o


def fm(v):
    v = np.asarray(v, np.float32).reshape(-1, 128)
    return np.ascontiguousarray(v.T)


def pack_vecs(inp, hflag):
    V = np.zeros((128, NV), np.float32)

    def put(name, arr):
        V[:, VOFF[name]:VOFF[name] + arr.shape[1]] = arr

    put("g1", fm(inp["conv_norm_g"][0]))
    put("b1", fm(inp["conv_b_pw1"][0]))
    put("bdw", fm(inp["conv_b_dw"][0]))
    put("lng", fm(inp["conv_ln_g"][0]))
    put("lnb", fm(inp["conv_ln_b"][0]))
    put("b2", fm(inp["conv_b_pw2"][0]))
    wdw = np.asarray(inp["conv_w_dw"][0], np.float32)
    put("wdw", np.ascontiguousarray(wdw.reshape(31, 16, 128).transpose(2, 1, 0)).reshape(128, 496))
    put("gf0", fm(inp["ffn_norm_g"][0]))
    put("gf1", fm(inp["ffn_norm_g"][1]))
    for l in range(2):
        wc = np.asarray(inp["ffn_w_conv"][l], np.float32)
        put("wc%d" % l, np.ascontiguousarray(wc.reshape(3, 44, 128).transpose(2, 0, 1)).reshape(128, 132))
    put("gkv", fm(inp["kv_norm_g"]))
    put("gat", fm(inp["attn_norm_g"][0]))
    put("kng", np.asarray(inp["k_norm_g"], np.float32).reshape(128, 1))
    put("qng", np.asarray(inp["q_norm_g"][0], np.float32).reshape(128, 1))
    V[:, VOFF["hflag"]] = hflag
    V[:, VOFF["eps"]] = EPS
    return V


class Ctx:
    pass


def setup_common(P):
    C = Ctx()
    C.P = P
    nc = P.nc
    C.vecs_d = P.dram("vecs", [128, NV], F32, "ExternalInput")
    C.ident_d = P.dram("ident", [128, 128], F32, "ExternalInput")
    C.R1 = P.sb("R1", [128, 16 * WIN + 8 * OW], F32)
    C.bufA = C.R1[:, 0:16 * WIN].rearrange("p (c t) -> p c t", t=WIN)
    C.bufC = C.R1[:, 16 * WIN:16 * WIN + 8 * OW].bitcast(BF16).rearrange("p (c t) -> p c t", t=OW)
    C.mbuf = C.R1[:, 0:FC * TT // 2].bitcast(BF16).rearrange("p (c t) -> p c t", t=TT)
    C.bufB_t = P.sb("bufB", [128, 16 * WIN], BF16)
    C.bufB = C.bufB_t[:, :].rearrange("p (c t) -> p c t", t=WIN)
    C.A = [Res() for _ in range(16)]
    C.B = [Res() for _ in range(16)]
    C.Cc = [Res() for _ in range(16)]
    C.M = [Res() for _ in range(FC)]
    C.wring = P.sb("wring", [128, NWSLOT * WSLOT], BF16)
    C.wres = [Res() for _ in range(NWSLOT)]
    C.wi = 0
    C.S = P.sb("S", [128, 7424], F32)
    C.vecs = P.sb("vecs_sb", [128, NV], F32)
    C.ident = P.sb("ident_sb", [128, 128], F32)
    C.ones = P.sb("ones_sb", [128, 128], BF16)
    C.psum = P.ps("psum", [128, 4096], F32)
    C.bank = [C.psum[:, i * 512:(i + 1) * 512] for i in range(8)]
    C.bres = [Res() for _ in range(8)]
    C.cres = Res()
    P.sp.dma(C.vecs[:], C.vecs_d, w=[C.cres])
    P.sp.dma(C.ident[:], C.ident_d, w=[C.cres])
    P.dve.op(lambda e: e.memset(C.ones[:], 1.0), w=[C.cres])
    P.fence()
    return C


def vcol(C, name, i=0):
    o = VOFF[name] + i
    return C.vecs[:, o:o + 1]


def wget(C, src, kcn, ncols):
    P = C.P
    k = C.wi
    C.wi = (k + 1) % NWSLOT
    view = C.wring[:, k * WSLOT:k * WSLOT + kcn * ncols].rearrange("p (k c) -> p k c", c=ncols)
    P.pool.dma(view, src.rearrange("(k p) c -> p k c", p=128), w=[C.wres[k]])
    return view, C.wres[k]


def scratch(C, off, n, dt=F32):
    if dt == F32:
        return C.S[:, off:off + n]
    return C.S[:, off:off + (n + 1) // 2].bitcast(BF16)[:, 0:n]


def phase_rmsnorm(C, W, gname):
    P = C.P
    sq = [scratch(C, 0, 512, BF16), scratch(C, 256, 512, BF16)]
    sqr = [Res(), Res()]
    rt = scratch(C, 512, W)
    rstd = scratch(C, 512 + WIN, W)
    rr = Res()
    cnt = 0
    for ri, (c0, n) in enumerate(regions(W)):
        bk = C.bank[ri]
        for c in range(16):
            s = cnt % 2
            cnt += 1
            P.act.op(lambda e, s=s, c=c, c0=c0, n=n: e.activation(out=sq[s][:, 0:n], in_=C.bufA[:, c, c0:c0 + n], func=AF.Square),
                     r=[C.A[c]], w=[sqr[s]])
            P.mm(bk[:, 0:n], C.ones[:], sq[s][:, 0:n], start=(c == 0), stop=(c == 15), r=[sqr[s], C.cres], w=[C.bres[ri]], last=True)
        P.act.op(lambda e, bk=bk, c0=c0, n=n: e.activation(out=rt[:, c0:c0 + n], in_=bk[:, 0:n], func=AF.Sqrt, bias=vcol(C, "eps"), scale=1.0 / D),
                 r=[C.bres[ri], C.cres], w=[rr])
        P.dve.op(lambda e, c0=c0, n=n: e.reciprocal(out=rstd[:, c0:c0 + n], in_=rt[:, c0:c0 + n]), r=[rr], w=[rr])
    for c in range(16):
        P.dve.op(lambda e, c=c: e.scalar_tensor_tensor(out=C.bufB[:, c, 0:W], in0=C.bufA[:, c, 0:W], scalar=vcol(C, gname, c),
                                                       in1=rstd[:, 0:W], op0=ALU.mult, op1=ALU.mult),
                 r=[C.A[c], rr, C.cres], w=[C.B[c]])


def load_window(C, src, col0, W):
    P = C.P
    for c in range(16):
        P.sp.dma(C.bufA[:, c, 0:W], src[:, c, col0:col0 + W], w=[C.A[c]])


def phase_conformer(C, tt, xT, X1A, hf):
    P = C.P
    w1 = C.w_pw1
    w2 = C.w_pw2
    col0 = tt * TT
    load_window(C, xT, col0, WIN)
    phase_rmsnorm(C, WIN, "g1")
    P.fence()
    sg = scratch(C, 0, WIN)
    u = [scratch(C, 1056, WIN, BF16), scratch(C, 1056 + 528, WIN, BF16)]
    dg = [scratch(C, 2112, 31 * 128, BF16).rearrange("p (k c) -> p k c", c=128),
          scratch(C, 2112 + 1984, 31 * 128, BF16).rearrange("p (k c) -> p k c", c=128)]
    vh = scratch(C, 6080, 4)
    junk = scratch(C, 6088, 32)
    sgr = Res()
    ur = [Res(), Res()]
    dgr = [Res(), Res()]
    vhr = Res()
    regs = regions(WIN)
    wdwv = C.vecs[:, VOFF["wdw"]:VOFF["wdw"] + 496].rearrange("p (j k) -> p j k", k=31)

    TD = 9
    cacc = scratch(C, 6120, TT)
    caccr = Res()

    def conv(j):
        s = j % 2
        for ro in range(2):
            bk = C.bank[6 + ro]
            for k in range(TD, 31):
                o0 = 2 + ro * 512 + k
                P.mm(bk[:, :], dg[s][:, k, :], u[s][:, o0:o0 + 512], start=(k == TD), stop=(k == 30),
                     r=[dgr[s], ur[s]], w=[C.bres[6 + ro]], last=(k == 30))
        for k in range(TD):
            if k == 0:
                P.dve.op(lambda e, s=s, j=j, k=k: e.tensor_scalar(out=cacc[:, :], in0=u[s][:, 2 + k:2 + k + TT], scalar1=vcol(C, "wdw", j * 31 + k),
                                                                scalar2=None, op0=ALU.mult), r=[ur[s], C.cres], w=[caccr])
            else:
                P.dve.op(lambda e, s=s, j=j, k=k: e.scalar_tensor_tensor(out=cacc[:, :], in0=u[s][:, 2 + k:2 + k + TT], scalar=vcol(C, "wdw", j * 31 + k),
                                                                       in1=cacc[:, :], op0=ALU.mult, op1=ALU.add), r=[ur[s], C.cres], w=[caccr])
        P.dve.op(lambda e, j=j: e.scalar_tensor_tensor(out=C.bufA[:, j, 2:2 + TT], in0=C.psum[:, 6 * 512:8 * 512], scalar=vcol(C, "bdw", j), in1=cacc[:, :],
                                                      op0=ALU.add, op1=ALU.add), r=[C.bres[6], C.bres[7], caccr, C.cres], w=[C.A[j]])
        for o in range(2):
            P.dve.op(lambda e, o=o, s=s, j=j: e.tensor_tensor(out=junk[:, 0:31], in0=u[s][:, o:o + 31], in1=wdwv[:, j, :], op=ALU.mult),
                     r=[ur[s], C.cres], w=[vhr])
            P.dve.op(lambda e, o=o: e.reduce_sum(out=vh[:, o:o + 1], in_=junk[:, 0:31], axis=AX.X), r=[vhr], w=[vhr])
        P.dve.op(lambda e, j=j: e.tensor_scalar(out=C.bufA[:, j, 0:2], in0=vh[:, 0:2], scalar1=vcol(C, "bdw", j), scalar2=None, op0=ALU.add),
                 r=[vhr, C.cres], w=[C.A[j]])

    for j in range(16):
        s = j % 2
        if j % 2 == 0:
            wa, war = wget(C, w1[:, j * 128:(j + 2) * 128], 16, 256)
            wg, wgr = wget(C, w1[:, D + j * 128:D + (j + 2) * 128], 16, 256)
        jc = (j % 2) * 128
        for kc in range(16):
            for ri, (c0, n) in enumerate(regs):
                P.mm(C.bank[ri][:, 0:n], wg[:, kc, jc:jc + 128], C.bufB[:, kc, c0:c0 + n], start=(kc == 0), stop=(kc == 15),
                     r=[wgr, C.B[kc]], w=[C.bres[ri]], last=(kc == 15))
        for ri, (c0, n) in enumerate(regs):
            P.act.op(lambda e, ri=ri, c0=c0, n=n, j=j: e.activation(out=sg[:, c0:c0 + n], in_=C.bank[ri][:, 0:n], func=AF.Sigmoid,
                                                                  bias=vcol(C, "b1", 16 + j), scale=1.0),
                     r=[C.bres[ri], C.cres], w=[sgr])
        for kc in range(16):
            for ri, (c0, n) in enumerate(regs):
                P.mm(C.bank[3 + ri][:, 0:n], wa[:, kc, jc:jc + 128], C.bufB[:, kc, c0:c0 + n], start=(kc == 0), stop=(kc == 15),
                     r=[war, C.B[kc]], w=[C.bres[3 + ri]], last=(kc == 15))
        for ri, (c0, n) in enumerate(regs):
            P.dve.op(lambda e, ri=ri, c0=c0, n=n, j=j, s=s: e.scalar_tensor_tensor(out=u[s][:, c0:c0 + n], in0=C.bank[3 + ri][:, 0:n],
                                                                               scalar=vcol(C, "b1", j), in1=sg[:, c0:c0 + n],
                                                                               op0=ALU.add, op1=ALU.mult),
                     r=[C.bres[3 + ri], sgr, C.cres], w=[ur[s]])
        if hf is not None:
            P.dve.op(lambda e, s=s: e.tensor_scalar(out=u[s][:, 0:HALO], in0=u[s][:, 0:HALO], scalar1=vcol(C, hf), scalar2=None, op0=ALU.mult),
                     r=[C.cres], w=[ur[s]])
        for k in range(TD, 31):
            P.act.op(lambda e, s=s, k=k, j=j: e.activation(out=dg[s][:, k, :], in_=C.ident[:], func=AF.Identity, scale=vcol(C, "wdw", j * 31 + k)),
                     r=[C.cres], w=[dgr[s]])
        if j >= 1:
            conv(j - 1)
    conv(15)
    P.fence()
    sq = [scratch(C, 0, OW, BF16), scratch(C, 520, OW, BF16)]
    vb = [scratch(C, 1040, OW, BF16), scratch(C, 1560, OW, BF16)]
    mean = scratch(C, 2080, OW)
    rstd = scratch(C, 2080 + OW, OW)
    t1 = [scratch(C, 2080 + 2 * OW, OW), scratch(C, 2080 + 3 * OW, OW)]
    sqr = [Res(), Res()]
    vbr = [Res(), Res()]
    mr = Res()
    t1r = [Res(), Res()]
    oregs = regions(OW)
    for c in range(16):
        s = c % 2
        P.act.op(lambda e, s=s, c=c: e.activation(out=sq[s][:, 0:OW], in_=C.bufA[:, c, 0:OW], func=AF.Square), r=[C.A[c]], w=[sqr[s]])
        P.dve.op(lambda e, s=s, c=c: e.tensor_copy(out=vb[s][:, 0:OW], in_=C.bufA[:, c, 0:OW]), r=[C.A[c]], w=[vbr[s]])
        for ri, (c0, n) in enumerate(oregs):
            P.mm(C.bank[ri][:, 0:n], C.ones[:], vb[s][:, c0:c0 + n], start=(c == 0), stop=(c == 15), r=[vbr[s], C.cres], w=[C.bres[ri]], last=True)
            P.mm(C.bank[3 + ri][:, 0:n], C.ones[:], sq[s][:, c0:c0 + n], start=(c == 0), stop=(c == 15), r=[sqr[s], C.cres], w=[C.bres[3 + ri]], last=True)
    for ri, (c0, n) in enumerate(oregs):
        P.act.op(lambda e, ri=ri, c0=c0, n=n: e.activation(out=mean[:, c0:c0 + n], in_=C.bank[ri][:, 0:n], func=AF.Identity, scale=1.0 / D),
                 r=[C.bres[ri]], w=[mr])
        P.dve.op(lambda e, c0=c0, n=n: e.tensor_tensor(out=t1[0][:, c0:c0 + n], in0=mean[:, c0:c0 + n], in1=mean[:, c0:c0 + n], op=ALU.mult),
                 r=[mr], w=[t1r[0]])
        P.dve.op(lambda e, ri=ri, c0=c0, n=n: e.scalar_tensor_tensor(out=t1[1][:, c0:c0 + n], in0=C.bank[3 + ri][:, 0:n], scalar=1.0 / D,
                                                                    in1=t1[0][:, c0:c0 + n], op0=ALU.mult, op1=ALU.subtract),
                 r=[C.bres[3 + ri], t1r[0]], w=[t1r[1]])
        P.act.op(lambda e, c0=c0, n=n: e.activation(out=t1[0][:, c0:c0 + n], in_=t1[1][:, c0:c0 + n], func=AF.Sqrt, bias=vcol(C, "eps"), scale=1.0),
                 r=[t1r[1], C.cres], w=[t1r[0]])
        P.dve.op(lambda e, c0=c0, n=n: e.reciprocal(out=rstd[:, c0:c0 + n], in_=t1[0][:, c0:c0 + n]), r=[t1r[0]], w=[mr])
    for c in range(16):
        s = c % 2
        P.dve.op(lambda e, s=s, c=c: e.tensor_tensor(out=t1[s][:, 0:OW], in0=C.bufA[:, c, 0:OW], in1=mean[:, 0:OW], op=ALU.subtract),
                 r=[C.A[c], mr], w=[t1r[s]])
        P.dve.op(lambda e, s=s: e.tensor_tensor(out=t1[s][:, 0:OW], in0=t1[s][:, 0:OW], in1=rstd[:, 0:OW], op=ALU.mult),
                 r=[mr], w=[t1r[s]])
        P.act.op(lambda e, s=s, c=c: e.activation(out=C.bufC[:, c, 0:OW], in_=t1[s][:, 0:OW], func=AF.Silu, bias=vcol(C, "lnb", c), scale=vcol(C, "lng", c)),
                 r=[t1r[s], C.cres], w=[C.Cc[c]])
    P.fence()
    xr = [scratch(C, 0, OW), scratch(C, OW, OW)]
    xrr = [Res(), Res()]
    for oc in range(16):
        s = oc % 2
        if oc % 2 == 0:
            wv, wr = wget(C, w2[:, oc * 128:(oc + 2) * 128], 16, 256)
        jc = (oc % 2) * 128
        bs = 3 * s
        P.sp.dma(xr[s][:, 0:OW], xT[:, oc, col0 + 30:col0 + 30 + OW], w=[xrr[s]])
        for kc in range(16):
            for ri, (c0, n) in enumerate(oregs):
                P.mm(C.bank[bs + ri][:, 0:n], wv[:, kc, jc:jc + 128], C.bufC[:, kc, c0:c0 + n], start=(kc == 0), stop=(kc == 15),
                     r=[wr, C.Cc[kc]], w=[C.bres[bs + ri]], last=(kc == 15))
        for ri, (c0, n) in enumerate(oregs):
            P.dve.op(lambda e, bs=bs, ri=ri, c0=c0, n=n, oc=oc, s=s: e.scalar_tensor_tensor(out=C.bufA[:, oc, c0:c0 + n], in0=C.bank[bs + ri][:, 0:n],
                                                                                       scalar=vcol(C, "b2", oc), in1=xr[s][:, c0:c0 + n],
                                                                                       op0=ALU.add, op1=ALU.add),
                     r=[C.bres[bs + ri], xrr[s], C.cres], w=[C.A[oc]])
        if tt == 0:
            P.act.dma(X1A[:, oc, 0:OW], C.bufA[:, oc, 0:OW], r=[C.A[oc]])
        else:
            P.act.dma(X1A[:, oc, 2 + tt * TT:2 + (tt + 1) * TT], C.bufA[:, oc, 2:OW], r=[C.A[oc]])
    P.fence()


def phase_ffn(C, tt, l, XRES, XOUT, res_halo, out_halo, hf):
    P = C.P
    w_in = C.w_in[l]
    w_out = C.w_out[l]
    phase_rmsnorm(C, OW, "gf%d" % l)
    P.fence()
    y = [scratch(C, 0, TT), scratch(C, TT, TT)]
    sl = [scratch(C, 2 * TT, TT), scratch(C, 3 * TT, TT)]
    yr = [Res(), Res()]
    sr = [Res(), Res()]
    wcn = "wc%d" % l
    for c in range(FC):
        s = c % 2
        if c % 2 == 0:
            wg, wgr = wget(C, w_in[:, c * 128:(c + 2) * 128], 16, 256)
            wu, wur = wget(C, w_in[:, FF + c * 128:FF + (c + 2) * 128], 16, 256)
        jc = (c % 2) * 128
        gb = 0 if s == 0 else 5
        gps = C.psum[:, gb * 512:gb * 512 + OW]
        for kc in range(16):
            for ri, (c0, n) in enumerate(regions(OW)):
                P.mm(C.bank[gb + ri][:, 0:n], wg[:, kc, jc:jc + 128], C.bufB[:, kc, c0:c0 + n], start=(kc == 0), stop=(kc == 15),
                     r=[wgr, C.B[kc]], w=[C.bres[gb + ri]], last=(kc == 15))
        gres = [C.bres[gb], C.bres[gb + 1], C.bres[gb + 2]]
        if hf is not None:
            P.dve.op(lambda e, gps=gps: e.tensor_scalar(out=gps[:, 0:2], in0=gps[:, 0:2], scalar1=vcol(C, hf), scalar2=None, op0=ALU.mult),
                     r=[C.cres], w=gres)
        P.act.op(lambda e, gps=gps, s=s, c=c: e.activation(out=y[s][:, :], in_=gps[:, 2:OW], func=AF.Identity, scale=vcol(C, wcn, 2 * FC + c)),
                 r=gres + [C.cres], w=[yr[s]])
        P.dve.op(lambda e, gps=gps, s=s, c=c: e.scalar_tensor_tensor(out=y[s][:, :], in0=gps[:, 1:OW - 1], scalar=vcol(C, wcn, FC + c), in1=y[s][:, :],
                                                                  op0=ALU.mult, op1=ALU.add), r=gres + [C.cres], w=[yr[s]])
        P.dve.op(lambda e, gps=gps, s=s, c=c: e.scalar_tensor_tensor(out=y[s][:, :], in0=gps[:, 0:OW - 2], scalar=vcol(C, wcn, c), in1=y[s][:, :],
                                                                  op0=ALU.mult, op1=ALU.add), r=gres + [C.cres], w=[yr[s]])
        P.act.op(lambda e, s=s: e.activation(out=sl[s][:, :], in_=y[s][:, :], func=AF.Silu), r=[yr[s]], w=[sr[s]])
        for kc in range(16):
            for ri in range(2):
                P.mm(C.bank[3 + ri][:, :], wu[:, kc, jc:jc + 128], C.bufB[:, kc, 2 + ri * 512:2 + ri * 512 + 512], start=(kc == 0), stop=(kc == 15),
                     r=[wur, C.B[kc]], w=[C.bres[3 + ri]], last=(kc == 15))
        P.dve.op(lambda e, s=s, c=c: e.tensor_tensor(out=C.mbuf[:, c, :], in0=C.psum[:, 3 * 512:5 * 512], in1=sl[s][:, :], op=ALU.mult),
                 r=[C.bres[3], C.bres[4], sr[s]], w=[C.M[c]])
    P.fence()
    xr = [scratch(C, 0, TT), scratch(C, TT, TT)]
    xo = [scratch(C, 2 * TT, TT), scratch(C, 3 * TT, TT)]
    xrr = [Res(), Res()]
    xor_ = [Res(), Res()]
    for oc in range(16):
        s = oc % 2
        wv, wr = wget(C, w_out[:, oc * 128:(oc + 1) * 128], FC, 128)
        P.sp.dma(xr[s][:, :], XRES[:, oc, res_halo + tt * TT:res_halo + (tt + 1) * TT], w=[xrr[s]])
        bs = 2 * s
        for c in range(FC):
            for ri in range(2):
                P.mm(C.bank[bs + ri][:, :], wv[:, c, :], C.mbuf[:, c, ri * 512:(ri + 1) * 512], start=(c == 0), stop=(c == FC - 1),
                     r=[wr, C.M[c]], w=[C.bres[bs + ri]], last=(c == FC - 1))
        P.dve.op(lambda e, bs=bs, s=s: e.tensor_tensor(out=xo[s][:, :], in0=C.psum[:, bs * 512:(bs + 2) * 512], in1=xr[s][:, :], op=ALU.add),
                 r=[C.bres[bs], C.bres[bs + 1], xrr[s]], w=[xor_[s]])
        P.act.dma(XOUT[:, oc, out_halo + tt * TT:out_halo + (tt + 1) * TT], xo[s][:, :], r=[xor_[s]])
    P.fence()


_CACHE = {}


def _ident():
    return np.eye(128, dtype=np.float32)


def phase_proj_hn(C, w, colbase, gname, scale, dst, dst_col0, km=None, km_col0=0, ncols=TT):
    P = C.P
    sqk = [scratch(C, 0, TT, BF16), scratch(C, 5300, TT, BF16)]
    rt = scratch(C, 512, TT)
    rs = scratch(C, 1536, TT)
    kf = scratch(C, 2560, TT)
    kb = [scratch(C, 3584, TT, BF16), scratch(C, 4096, TT, BF16)]
    kms = scratch(C, 4608, 4)
    r_sq = [Res(), Res()]
    r_rt, r_kf, r_km = Res(), Res(), Res()
    r_kb = [Res(), Res()]
    regs = regions(ncols)
    nr = len(regs)
    wcur = {}

    def proj(hc):
        s = hc % 2
        if hc % 2 == 0:
            wcur["w"] = wget(C, w[:, colbase + hc * 128:colbase + (hc + 2) * 128], 16, 256)
        wv, wr = wcur["w"]
        jc = (hc % 2) * 128
        bs = (0, 2, 6)[hc % 3]
        for kc in range(16):
            for ri, (c0, n) in enumerate(regs):
                P.mm(C.bank[bs + ri][:, 0:n], wv[:, kc, jc:jc + 128], C.bufB[:, kc, c0:c0 + n], start=(kc == 0), stop=(kc == 15),
                     r=[wr, C.B[kc]], w=[C.bres[bs + ri]], last=(kc == 15))
        yps = C.psum[:, bs * 512:bs * 512 + ncols]
        yres = [C.bres[bs + ri] for ri in range(nr)]
        P.act.op(lambda e, yps=yps: e.activation(out=sqk[s][:, 0:ncols], in_=yps, func=AF.Square), r=yres, w=[r_sq[s]])

    def epi(hc):
        s = hc % 2
        bs = (0, 2, 6)[hc % 3]
        yps = C.psum[:, bs * 512:bs * 512 + ncols]
        yres = [C.bres[bs + ri] for ri in range(nr)]
        for ri, (c0, n) in enumerate(regs):
            P.mm(C.bank[4 + ri][:, 0:n], C.ones[:], sqk[s][:, c0:c0 + n], start=True, stop=True, r=[r_sq[s], C.cres], w=[C.bres[4 + ri]], last=True)
        P.act.op(lambda e: e.activation(out=rt[:, 0:ncols], in_=C.psum[:, 4 * 512:4 * 512 + ncols], func=AF.Sqrt, bias=vcol(C, "eps"), scale=1.0 / 128),
                 r=[C.bres[4 + ri] for ri in range(nr)] + [C.cres], w=[r_rt])
        P.dve.op(lambda e: e.reciprocal(out=rs[:, 0:ncols], in_=rt[:, 0:ncols]), r=[r_rt], w=[r_rt])
        P.dve.op(lambda e, yps=yps: e.scalar_tensor_tensor(out=kf[:, 0:ncols], in0=yps, scalar=vcol(C, gname), in1=rs[:, 0:ncols], op0=ALU.mult, op1=ALU.mult),
                 r=yres + [r_rt, C.cres], w=[r_kf])
        P.act.op(lambda e, s=s: e.activation(out=kb[s][:, 0:ncols], in_=kf[:, 0:ncols], func=AF.Identity, scale=float(scale)), r=[r_kf], w=[r_kb[s]])
        P.act.dma(dst[:, hc, dst_col0:dst_col0 + ncols], kb[s][:, 0:ncols], r=[r_kb[s]])
        if km is not None:
            P.dve.op(lambda e: e.tensor_reduce(out=kms[:, 0:4], in_=kf[:, :].rearrange("p (n j) -> p n j", j=256), axis=AX.X, op=ALU.add),
                     r=[r_kf], w=[r_km])
            P.act.op(lambda e, hc=hc: e.activation(out=km[:, hc, km_col0:km_col0 + 4], in_=kms[:, 0:4], func=AF.Identity, scale=1.0 / 256),
                     r=[r_km], w=[C.kmres])

    for hc in range(16):
        proj(hc)
        if hc >= 1:
            epi(hc - 1)
    epi(15)


def phase_v(C, w, colbase, V_d, tok0):
    P = C.P
    vst = [scratch(C, 5000, 256, BF16), scratch(C, 5128, 256, BF16)]
    vr = [Res(), Res()]
    for vb in range(8):
        wv, wr = wget(C, w[:, colbase + vb * 256:colbase + (vb + 1) * 256], 16, 256)
        for tk in range(8):
            s = tk % 2
            bk = 6 + s
            for kc in range(16):
                P.mm(C.bank[bk][:, 0:256], C.bufB[:, kc, tk * 128:(tk + 1) * 128], wv[:, kc, :], start=(kc == 0), stop=(kc == 15),
                     r=[wr, C.B[kc]], w=[C.bres[bk]], last=(kc == 15))
            P.act.op(lambda e, s=s, bk=bk: e.activation(out=vst[s][:, :], in_=C.bank[bk][:, 0:256], func=AF.Identity), r=[C.bres[bk]], w=[vr[s]])
            P.act.dma(V_d[tok0 + tk * 128:tok0 + (tk + 1) * 128, vb * 256:(vb + 1) * 256], vst[s][:, :], r=[vr[s]])


def r1v(C, off, n, dt=F32):
    if dt == F32:
        return C.R1[:, off:off + n]
    return C.R1[:, off:off + (n + 1) // 2].bitcast(BF16)[:, 0:n]


def attention_setup(C, D_):
    P = C.P
    A = Ctx()
    A.Qb = [r1v(C, 0, 8192, BF16).rearrange("p (h q) -> p h q", q=512), C.bufB_t[:, 0:8192].rearrange("p (h q) -> p h q", q=512)]
    A.KTh = [r1v(C, 4096, 4096, BF16), r1v(C, 6144, 4096, BF16)]
    A.Vh = [r1v(C, 8192, 4096, BF16).rearrange("p (c d) -> p c d", d=128), r1v(C, 10240, 4096, BF16).rearrange("p (c d) -> p c d", d=128)]
    A.Rt = [r1v(C, 12288, 8192, BF16).rearrange("p (h q) -> p h q", q=512), C.bufB_t[:, 8192:16384].rearrange("p (h q) -> p h q", q=512)]
    A.pt = [r1v(C, 16384 + i * 256, 512, BF16) for i in range(4)]
    A.ssb = [r1v(C, 17408, 512), r1v(C, 17920, 512)]
    A.acc = [r1v(C, 18432, 512), r1v(C, 18944, 512)]
    A.rl = [r1v(C, 19456, 512), r1v(C, 19968, 512)]
    A.ot = [r1v(C, 20480, 512, BF16), r1v(C, 20736, 512, BF16)]
    A.gm = r1v(C, 20992, 256)
    A.sel = r1v(C, 21248, 256)
    A.rbq = r1v(C, 21504, 256, BF16)
    A.mx = r1v(C, 21632, 128).rearrange("p (h k) -> p h k", k=8)
    A.thr = r1v(C, 21760, 16)
    A.Em = r1v(C, 21776, 2048, BF16).rearrange("p (n j) -> p n j", j=128)
    A.cmask = r1v(C, 22800, 1024).rearrange("p (t q) -> p t q", t=2)
    A.kmb = r1v(C, 23824, 256, BF16).rearrange("p (h n) -> p h n", n=16)
    A.identb = r1v(C, 23952, 128, BF16)
    A.ones32 = r1v(C, 24016, 128)
    A.pastm = C.S[:, 0:2304].rearrange("p (b x) -> p b x", x=256)
    A.ownm = C.S[:, 2304:4608].rearrange("p (b x) -> p b x", x=256)
    A.r_const = Res()
    A.r_Qb = [Res(), Res()]
    A.r_Rt = [Res(), Res()]
    A.r_KT = [Res(), Res()]
    A.r_V = [Res(), Res()]
    A.r_pt = [Res() for _ in range(4)]
    A.r_ssb = [Res(), Res()]
    A.r_acc = [Res(), Res()]
    A.r_g = Res()
    A.r_rl = [Res(), Res()]
    A.r_ot = [Res(), Res()]
    w = [A.r_const]
    for bf in range(2):
        P.dve.op(lambda e, bf=bf: e.memset(A.Rt[bf][:, :, :], 0.0), w=[A.r_Rt[bf]])
    P.dve.op(lambda e: e.memset(A.ones32[:, :], 1.0), w=w)
    P.sp.dma(A.Em[0:34, :, :], D_["emat"], w=w)
    P.sp.dma(A.cmask[:, :, :], D_["cmask"].rearrange("p (t q) -> p t q", t=2), w=w)
    P.sp.dma(A.pastm[:, :, :], D_["pastm"].rearrange("p (b x) -> p b x", x=256), w=w)
    P.sp.dma(A.ownm[:, :, :], D_["ownm"].rearrange("p (b x) -> p b x", x=256), w=w)
    P.act.op(lambda e: e.activation(out=A.kmb[:, :, :], in_=C.kmT[:, :].rearrange("p (h n) -> p h n", n=16), func=AF.Identity), r=[C.kmres], w=w)
    P.act.op(lambda e: e.activation(out=A.identb[:, :], in_=C.ident[:], func=AF.Identity), r=[C.cres], w=w)
    return A


def attention_all(C, A, units, QT_d, KT_d, V_d, OT_d, qal_d):
    P = C.P
    cst = [A.r_const]
    GB = 3
    gps = C.bank[GB]
    tps = C.psum[:, GB * 512 + 256:(GB + 1) * 512].bitcast(BF16)

    def gating_steps(ui):
        i0, nqb = units[ui]
        bf = ui % 2
        NQc = nqb * 256
        q0 = i0 * 256
        Qb, Rt = A.Qb[bf], A.Rt[bf]
        steps = []

        def load():
            P.sp.dma(Qb[:, :, 0:NQc], QT_d[:, :, q0:q0 + NQc], w=[A.r_Qb[bf]])
            P.sp.dma(Rt[32:34, :, 0:NQc], qal_d[:, :, q0:q0 + NQc], w=[A.r_Rt[bf]])
        steps.append(load)
        for qt in range(NQc // 128):
            qi = i0 + qt // 2

            def stepA(qt=qt, qi=qi):
                for h in range(16):
                    P.mm(gps[:, h * 16:(h + 1) * 16], Qb[:, h, qt * 128:(qt + 1) * 128], A.kmb[:, h, :], start=True, stop=True,
                         r=[A.r_Qb[bf]] + cst, w=[C.bres[GB]], last=(h == 15))
                P.dve.op(lambda e: e.tensor_tensor(out=A.gm[:, :], in0=gps[:, 0:256], in1=A.pastm[:, qi, :], op=ALU.add),
                         r=[C.bres[GB]] + cst, w=[A.r_g])
                for h in range(16):
                    P.dve.op(lambda e, h=h: e.max(out=A.mx[:, h, :], in_=A.gm[:, h * 16:(h + 1) * 16]), r=[A.r_g], w=[A.r_g])
                P.dve.op(lambda e: e.tensor_scalar(out=A.thr[:, :], in0=A.mx[:, :, 2], scalar1=-1e29, scalar2=None, op0=ALU.max), r=[A.r_g], w=[A.r_g])
                for h in range(16):
                    P.dve.op(lambda e, h=h: e.tensor_scalar(out=A.sel[:, h * 16:(h + 1) * 16], in0=A.gm[:, h * 16:(h + 1) * 16], scalar1=A.thr[:, h:h + 1],
                                                            scalar2=None, op0=ALU.is_ge), r=[A.r_g], w=[A.r_g])
                P.dve.op(lambda e: e.tensor_tensor(out=A.sel[:, :], in0=A.sel[:, :], in1=A.ownm[:, qi, :], op=ALU.max), r=[A.r_g] + cst, w=[A.r_g])
                P.dve.op(lambda e: e.tensor_scalar(out=A.rbq[:, :], in0=A.sel[:, :], scalar1=-1.0, scalar2=30000.0, op0=ALU.add, op1=ALU.mult),
                         r=[A.r_g], w=[A.r_g])

            def stepB(qt=qt):
                for hg in range(4):
                    for hh in range(4):
                        h = hg * 4 + hh
                        P.pe.op(lambda e, h=h, hh=hh: e.transpose(tps[0:16, hh * 128:(hh + 1) * 128], A.rbq[:, h * 16:(h + 1) * 16], A.identb[:, :]),
                                r=[A.r_g] + cst, w=[C.bres[GB]])
                    P.act.op(lambda e, hg=hg: e.activation(out=Rt[0:16, hg * 4:(hg + 1) * 4, qt * 128:(qt + 1) * 128],
                                                           in_=tps[0:16, :].rearrange("p (h q) -> p h q", q=128), func=AF.Identity),
                             r=[C.bres[GB]], w=[A.r_Rt[bf]])
            steps.append(stepA)
            steps.append(stepB)
        return steps

    def unit_tiles(ui):
        i0, nqb = units[ui]
        NQc = nqb * 256
        nkb = 8 + i0 + nqb - 1
        tiles = []
        for h in range(16):
            lst = []
            for n in range(nkb):
                for jt in range(2):
                    if n <= 7 + i0:
                        lst.append((n, jt, 0, NQc))
                    else:
                        lst.append((n, jt, 256, 256))
            for ist, (n, jt, c0, ncl) in enumerate(lst):
                tiles.append((h, n, jt, c0, ncl, ist, len(lst)))
        return tiles

    cnt = {"k": 0}
    for st in gating_steps(0):
        st()
    for ui, (i0, nqb) in enumerate(units):
        bf = ui % 2
        NQc = nqb * 256
        nkb = 8 + i0 + nqb - 1
        nk = nkb * 256
        q0 = i0 * 256
        Qb, Rt = A.Qb[bf], A.Rt[bf]
        tiles = unit_tiles(ui)
        nxt = gating_steps(ui + 1) if ui + 1 < len(units) else []

        def load_head(h):
            s = h % 2
            P.sp.dma(A.KTh[s][:, 0:nk], KT_d[:, h, 0:nk], w=[A.r_KT[s]])
            P.sp.dma(A.Vh[s][:, 0:nkb * 2, :], V_d[0:nk, h * 128:(h + 1) * 128].rearrange("(c p) d -> p c d", p=128), w=[A.r_V[s]])

        def score(k, kg):
            h, n, jt, c0, ncl, ist, nst = tiles[k]
            s = h % 2
            if ist == 0:
                load_head(h)
            sb_ = C.bank[kg % 3]
            sr = C.bres[kg % 3]
            P.mm(sb_[:, 0:ncl], A.KTh[s][:, n * 256 + jt * 128:n * 256 + (jt + 1) * 128], Qb[:, h, c0:c0 + ncl], start=True, stop=False,
                 r=[A.r_KT[s], A.r_Qb[bf]], w=[sr])
            P.mm(sb_[:, 0:ncl], A.Em[0:34, n, :], Rt[0:34, h, c0:c0 + ncl], start=False, stop=True, r=[A.r_Rt[bf]] + cst, w=[sr], last=True)
            p = kg % 4
            bias = C.kalf[:, h, n * 2 + jt:n * 2 + jt + 1]
            if n >= 7 + i0:
                x = kg % 2
                P.dve.op(lambda e: e.tensor_tensor(out=A.ssb[x][:, 0:ncl], in0=sb_[:, 0:ncl], in1=A.cmask[:, jt, 0:ncl], op=ALU.add),
                         r=[sr] + cst, w=[A.r_ssb[x]])
                P.act.op(lambda e: e.activation(out=A.pt[p][:, 0:ncl], in_=A.ssb[x][:, 0:ncl], func=AF.Exp, bias=bias, scale=1.0),
                         r=[A.r_ssb[x]] + cst, w=[A.r_pt[p]])
            else:
                P.act.op(lambda e: e.activation(out=A.pt[p][:, 0:ncl], in_=sb_[:, 0:ncl], func=AF.Exp, bias=bias, scale=1.0),
                         r=[sr] + cst, w=[A.r_pt[p]])
            if ist == 0:
                P.dve.op(lambda e, NQc=NQc: e.tensor_copy(out=A.acc[s][:, 0:NQc], in_=A.pt[p][:, 0:NQc]), r=[A.r_pt[p]], w=[A.r_acc[s]])
            else:
                P.dve.op(lambda e: e.tensor_tensor(out=A.acc[s][:, c0:c0 + ncl], in0=A.acc[s][:, c0:c0 + ncl], in1=A.pt[p][:, 0:ncl], op=ALU.add),
                         r=[A.r_pt[p]], w=[A.r_acc[s]])

        def pvl(k, kg):
            h, n, jt, c0, ncl, ist, nst = tiles[k]
            s = h % 2
            p = kg % 4
            obi = 4 + 2 * s
            ob = C.bank[obi]
            lb = C.bank[obi + 1]
            lastst = (ist == nst - 1)
            P.mm(ob[:, c0:c0 + ncl], A.Vh[s][:, n * 2 + jt, :], A.pt[p][:, 0:ncl], start=(ist == 0), stop=lastst, r=[A.r_V[s], A.r_pt[p]], w=[C.bres[obi]], last=True)
            if lastst:
                P.pe.op(lambda e, NQc=NQc: e.matmul(lb[:, 0:NQc], A.ones32[:, :], A.acc[s][:, 0:NQc], start=True, stop=True),
                        r=[A.r_acc[s]] + cst, w=[C.bres[obi + 1]])
                P.dve.op(lambda e, NQc=NQc: e.reciprocal(out=A.rl[s][:, 0:NQc], in_=lb[:, 0:NQc]), r=[C.bres[obi + 1]], w=[A.r_rl[s]])
                P.dve.op(lambda e, NQc=NQc: e.tensor_tensor(out=A.ot[s][:, 0:NQc], in0=ob[:, 0:NQc], in1=A.rl[s][:, 0:NQc], op=ALU.mult),
                         r=[C.bres[obi], A.r_rl[s]], w=[A.r_ot[s]])
                P.act.dma(OT_d[:, h, q0:q0 + NQc], A.ot[s][:, 0:NQc], r=[A.r_ot[s]])

        LA = 2
        nt = len(tiles)
        sched = {}
        for si, st in enumerate(nxt):
            sched.setdefault(((si + 1) * nt) // (len(nxt) + 1), []).append(st)
        base = cnt["k"]
        for k in range(nt + LA):
            if k < nt:
                score(k, base + k)
            if k - LA >= 0:
                pvl(k - LA, base + k - LA)
            for st in sched.get(k, []):
                st()
        cnt["k"] = base + nt


LSEQ = 4096
QPRE = 1792
NQ = LSEQ - QPRE
QTILES = [(1792, 256), (2048, 1024), (3072, 1024)]


def build_F():
    P = Prog()
    C = setup_common(P)
    xT = P.dram("xT", [128, 16, HALO + LSEQ], F32, "ExternalInput")
    C.w_pw1 = P.dram("w_pw1", [D, 2 * D], F32, "ExternalInput")
    C.w_pw2 = P.dram("w_pw2", [D, D], F32, "ExternalInput")
    C.w_in = [P.dram("w_in0", [D, 2 * FF], F32, "ExternalInput"), P.dram("w_in1", [D, 2 * FF], F32, "ExternalInput")]
    C.w_out = [P.dram("w_out0", [FF, D], F32, "ExternalInput"), P.dram("w_out1", [FF, D], F32, "ExternalInput")]
    w_kv = P.dram("w_kv", [D, 2 * D], F32, "ExternalInput")
    w_q = P.dram("w_q", [D, D], F32, "ExternalInput")
    w_o = P.dram("w_o", [D, D], F32, "ExternalInput")
    D_ = {"emat": P.dram("emat", [34, 16, 128], BF16, "ExternalInput"),
          "kal": P.dram("kal", [128, 32], F32, "ExternalInput"),
          "cmask": P.dram("cmask", [128, 1024], F32, "ExternalInput"),
          "pastm": P.dram("pastm", [128, 2304], F32, "ExternalInput"),
          "ownm": P.dram("ownm", [128, 2304], F32, "ExternalInput")}
    kalf_d = P.dram("kalf", [128, 16 * 32], F32, "ExternalInput")
    qal_d = P.dram("qal", [2, 16, NQ], BF16, "ExternalInput")
    OUT = P.dram("outT", [128, 16, TCORE], F32, "ExternalOutput")
    X1A = P.dram("X1A", [128, 16, 2 + LSEQ], F32, "Internal")
    X1 = P.dram("X1", [128, 16, 2 + LSEQ], F32, "Internal")
    X2 = P.dram("X2", [128, 16, 256 + TCORE], F32, "Internal")
    KT_d = P.dram("KT_d", [128, 16, LSEQ], BF16, "Internal")
    V_d = P.dram("V_d", [LSEQ, D], BF16, "Internal")
    QT_d = P.dram("QT_d", [128, 16, NQ], BF16, "Internal")
    OT_d = P.dram("OT_d", [128, 16, NQ], BF16, "Internal")
    C.kmT = P.sb("kmT", [128, 256], F32)
    C.kmres = Res()
    kalf_t = P.sb("kalf_sb", [128, 512], F32)
    C.kalf = kalf_t[:, :].rearrange("p (h c) -> p h c", c=32)
    P.sp.dma(kalf_t[:, :], kalf_d, w=[C.cres])
    kmv = C.kmT[:, :].rearrange("p (h n) -> p h n", n=16)
    for tt in range(4):
        hf = "zero" if tt == 0 else ("hflag" if tt == 2 else None)
        phase_conformer(C, tt, xT, X1A, hf)
        phase_ffn(C, tt, 0, X1A, X1, 2, 2, hf)
    for t4 in range(4):
        load_window(C, X1, 2 + t4 * TT, TT)
        phase_rmsnorm(C, TT, "gkv")
        P.fence()
        phase_proj_hn(C, w_kv, 0, "kng", 1.0, KT_d, t4 * TT, km=kmv, km_col0=t4 * 4)
        phase_v(C, w_kv, D, V_d, t4 * TT)
        P.fence()
    for (st, n) in QTILES:
        load_window(C, X1, 2 + st, n)
        phase_rmsnorm(C, n, "gat")
        P.fence()
        phase_proj_hn(C, w_q, 0, "qng", 128.0 ** -0.5, QT_d, st - QPRE, ncols=n)
        P.fence()
    A = attention_setup(C, D_)
    P.fence()
    attention_all(C, A, [(0, 1), (1, 2), (3, 2), (5, 2), (7, 2)], QT_d, KT_d, V_d, OT_d, qal_d)
    P.fence()
    xr = [scratch(C, 0, TT), scratch(C, TT, TT)]
    xo = [scratch(C, 2 * TT, TT), scratch(C, 3 * TT, TT)]
    xrr = [Res(), Res()]
    xor_ = [Res(), Res()]
    for (st, n) in QTILES:
        regs = regions(n)
        for c in range(16):
            P.sp.dma(C.bufB[:, c, 0:n], OT_d[:, c, st - QPRE:st - QPRE + n], w=[C.B[c]])
        for oc in range(16):
            s = oc % 2
            if oc % 2 == 0:
                wv, wr = wget(C, w_o[:, oc * 128:(oc + 2) * 128], 16, 256)
            jc = (oc % 2) * 128
            bs = 2 * s
            P.sp.dma(xr[s][:, 0:n], X1[:, oc, 2 + st:2 + st + n], w=[xrr[s]])
            for kc in range(16):
                for ri, (c0, m) in enumerate(regs):
                    P.mm(C.bank[bs + ri][:, 0:m], wv[:, kc, jc:jc + 128], C.bufB[:, kc, c0:c0 + m], start=(kc == 0), stop=(kc == 15),
                         r=[wr, C.B[kc]], w=[C.bres[bs + ri]], last=(kc == 15))
            P.dve.op(lambda e, bs=bs, s=s, n=n: e.tensor_tensor(out=xo[s][:, 0:n], in0=C.psum[:, bs * 512:bs * 512 + n], in1=xr[s][:, 0:n], op=ALU.add),
                     r=[C.bres[bs + ri] for ri in range(len(regs))] + [xrr[s]], w=[xor_[s]])
            P.act.dma(X2[:, oc, st - QPRE:st - QPRE + n], xo[s][:, 0:n], r=[xor_[s]])
        P.fence()
    for t in range(NTILE):
        load_window(C, X2, 254 + t * TT, OW)
        phase_ffn(C, t, 1, X2, OUT, 256, 0, "hflag" if t == 0 else None)
    return P.finish([])


SLOPES = np.exp2(-8.0 * np.arange(1, NH + 1, dtype=np.float64) / NH)


def attn_tables(half):
    T = {}
    em = np.zeros((34, 16, 128), np.float32)
    for n in range(16):
        em[n, n, :] = 1.0
    em[32:34, :, :] = 1.0
    T["emat"] = em.astype(ml_dtypes.bfloat16)
    T["kal"] = np.zeros((128, 32), np.float32)
    pos = (np.arange(32)[None, :] * 128 + np.arange(128)[:, None]).astype(np.float64)
    T["kalf"] = (SLOPES[None, :, None] * pos[:, None, :]).astype(np.float32).reshape(128, 512)
    tq = QPRE + np.arange(NQ, dtype=np.float64)
    v = -(SLOPES[:, None] * tq[None, :])
    hi = v.astype(np.float32).astype(ml_dtypes.bfloat16)
    lo = (v - hi.astype(np.float64)).astype(np.float32).astype(ml_dtypes.bfloat16)
    T["qal"] = np.stack([hi, lo], 0)
    kp = np.arange(2)[None, :, None] * 128 + np.arange(128)[:, None, None]
    cm = np.zeros((128, 2, 512), np.float32)
    cm[:, :, 0:256] = np.where(kp > np.arange(256)[None, None, :], -30000.0, 0.0)
    T["cmask"] = cm.reshape(128, 1024)
    pm = np.full((9, 16, 16), -1e30, np.float32)
    om = np.zeros((9, 16, 16), np.float32)
    nlo = 0 if half == 1 else 8
    for i in range(9):
        cur = 7 + i
        pm[i, :, nlo:cur] = 0.0
        om[i, :, cur] = 1.0
    T["pastm"] = np.broadcast_to(pm.reshape(1, 2304), (128, 2304)).copy()
    T["ownm"] = np.broadcast_to(om.reshape(1, 2304), (128, 2304)).copy()
    return T


_CACHE = {}


def kernel(**inputs):
    inp = {k: np.asarray(v) for k, v in inputs.items()}
    cores = list(range(8))
    if "F" not in _CACHE:
        _CACHE["F"] = build_F()
    x = inp["x"].astype(np.float32, copy=False)
    ws = {"w_pw1": np.ascontiguousarray(inp["conv_w_pw1"][0]), "w_pw2": np.ascontiguousarray(inp["conv_w_pw2"][0]),
          "w_in0": np.ascontiguousarray(inp["ffn_w_in"][0]), "w_out0": np.ascontiguousarray(inp["ffn_w_out"][0]),
          "w_in1": np.ascontiguousarray(inp["ffn_w_in"][1]), "w_out1": np.ascontiguousarray(inp["ffn_w_out"][1]),
          "w_kv": np.ascontiguousarray(inp["w_kv"]), "w_q": np.ascontiguousarray(inp["w_q"][0]), "w_o": np.ascontiguousarray(inp["w_o"][0])}
    tabs = [attn_tables(0), attn_tables(1)]
    ident = np.eye(128, dtype=np.float32)
    maps = []
    for c in cores:
        b, h = c // 2, c % 2
        xt = np.ascontiguousarray(x[b].T).reshape(16, 128, 4096).transpose(1, 0, 2)
        buf = np.zeros((128, 16, HALO + LSEQ), np.float32)
        if h == 1:
            buf[:, :, HALO:] = xt
        else:
            buf[:, :, HALO + TCORE:] = xt[:, :, 0:TCORE]
        maps.append(dict(xT=buf, vecs=pack_vecs(inp, float(h)), ident=ident, **ws, **tabs[h]))
    res = run_bass_kernel_spmd(_CACHE["F"], maps, core_ids=cores)
    out = np.zeros((4, 4096, D), np.float32)
    for c in cores:
        b, h = c // 2, c % 2
        o = res.results[c]["outT"]
        out[b, h * TCORE:(h + 1) * TCORE, :] = o.transpose(2, 1, 0).reshape(TCORE, D)
    return out
```

```python
import numpy as np
import ml_dtypes
import concourse.bass as bass
import concourse.mybir as mybir
from concourse.bass_utils import run_bass_kernel_spmd
from contextlib import ExitStack

F32 = mybir.dt.float32
BF16 = mybir.dt.bfloat16
AF = mybir.ActivationFunctionType
ALU = mybir.AluOpType
AX = mybir.AxisListType

D = 2048
KC = 16
TT = 1024
NTILE = 2
TCORE = 2048
HALO = 32
WIN = TT + HALO
OW = TT + 2
FF = 5632
FC = 44
NH = 16
EPS = 1e-6
NBLK = 16
WSLOT = 44 * 128
NWSLOT = 3


class Res:
    __slots__ = ("lw", "rd")

    def __init__(self):
        self.lw = None
        self.rd = {}


class Eng:
    def __init__(self, P, name, nsem_dma=0):
        self.P = P
        self.name = name
        self.sem = P.es.enter_context(P.nc.semaphore("sem_" + name))
        self.n = 0
        self.waited = {}
        self.rec = []
        self.dsems = [P.es.enter_context(P.nc.semaphore("d%s%d" % (name, i))) for i in range(nsem_dma)]
        self.dvals = [0] * nsem_dma
        self.di = 0

    def wait(self, deps):
        for d in deps:
            if d is None:
                continue
            sem, val, key = d
            if self.waited.get(key, 0) >= val:
                continue
            self.rec.append(("w", sem, val))
            self.waited[key] = val

    @staticmethod
    def _deps(r, w):
        d = []
        for x in r:
            if x.lw is not None:
                d.append(x.lw)
        for x in w:
            if x.lw is not None:
                d.append(x.lw)
            d.extend(x.rd.values())
        return d

    @staticmethod
    def _commit(h, r, w):
        for x in r:
            x.rd[h[2]] = h
        for x in w:
            x.lw = h
            x.rd = {}

    def op(self, fn, r=(), w=(), deps=()):
        self.wait(self._deps(r, w))
        self.wait(deps)
        self.n += 1
        self.rec.append(("i", fn, self.sem, 1))
        h = (self.sem, self.n, self.name)
        self._commit(h, r, w)
        return h

    def op_noinc(self, fn, r=(), w=()):
        self.wait(self._deps(r, w))
        self.rec.append(("n", fn))

    def mark(self):
        return (self.sem, self.n, self.name) if self.n else None

    def dma(self, out, in_, r=(), w=(), deps=(), **kw):
        k = self.di
        self.di = (self.di + 1) % len(self.dsems)
        key = "%s_d%d" % (self.name, k)
        if self.dvals[k] > 0:
            self.wait([(self.dsems[k], self.dvals[k], key)])
        self.wait(self._deps(r, w))
        self.wait(deps)
        self.dvals[k] += 16
        self.rec.append(("i", (lambda e, o=out, i=in_, kw=kw: e.dma_start(out=o, in_=i, **kw)), self.dsems[k], 16))
        h = (self.dsems[k], self.dvals[k], key)
        self._commit(h, r, w)
        self.P.dma_handles[key] = h
        return h

    def replay(self, e):
        for r in self.rec:
            if r[0] == "w":
                e.wait_ge(r[1], r[2])
            elif r[0] == "n":
                r[1](e)
            else:
                r[1](e).then_inc(r[2], r[3])


class Prog:
    def __init__(self):
        self.nc = bass.Bass("TRN2", target_bir_lowering=False)
        self.es = ExitStack()
        self.dma_handles = {}
        self.pe = Eng(self, "pe")
        self.act = Eng(self, "act", 10)
        self.dve = Eng(self, "dve")
        self.pool = Eng(self, "pool", 12)
        self.sp = Eng(self, "sp", 24)
        self.engs = [self.pe, self.act, self.dve, self.pool, self.sp]
        self._pr = []
        self._pw = []

    def sb(self, name, shape, dt):
        return self.es.enter_context(self.nc.sbuf_tensor(name, list(shape), dt))

    def ps(self, name, shape, dt=F32):
        return self.es.enter_context(self.nc.psum_tensor(name, list(shape), dt))

    def dram(self, name, shape, dt, kind):
        return self.nc.dram_tensor(name, list(shape), dt, kind=kind).ap()

    def mm(self, out, lhsT, rhs, start, stop, r=(), w=(), last=False):
        pe = self.pe
        fn = (lambda e, o=out, l=lhsT, rr=rhs, s=start, t=stop: e.matmul(o, l, rr, start=s, stop=t))
        self._pr.extend(r)
        self._pw.extend(w)
        if last:
            h = pe.op(fn, r=self._pr, w=self._pw)
            self._pr = []
            self._pw = []
            return h
        pe.op_noinc(fn, r=r, w=w)
        return None

    def fence(self):
        deps = [e.mark() for e in self.engs] + list(self.dma_handles.values())
        for e in self.engs:
            e.wait(deps)

    def finish(self, final_deps):
        self.sp.wait(final_deps)
        self.fence()
        with self.nc.Block() as block:
            @block.tensor
            def _(e):
                self.pe.replay(e)

            @block.scalar
            def _(e):
                self.act.replay(e)

            @block.vector
            def _(e):
                self.dve.replay(e)

            @block.gpsimd
            def _(e):
                self.pool.replay(e)

            @block.sync
            def _(e):
                self.sp.replay(e)
        self.es.close()
        return self.nc


def regions(n):
    out = []
    c = 0
    while c < n:
        m = min(512, n - c)
        out.append((c, m))
        c += m
    return out


VOFF = {}
_o = 0
for _n, _w in [("g1", 16), ("b1", 32), ("bdw", 16), ("lng", 16), ("lnb", 16), ("b2", 16), ("wdw", 496),
               ("gf0", 16), ("gf1", 16), ("wc0", 132), ("wc1", 132), ("gkv", 16), ("gat", 16),
               ("kng", 1), ("qng", 1), ("hflag", 1), ("eps", 1), ("zero", 1)]:
    VOFF[_n] = _o
    _o += _w
NV = _o


def fm(v):
    v = np.asarray(v, np.float32).reshape(-1, 128)
    return np.ascontiguousarray(v.T)


def pack_vecs(inp, hflag):
    V = np.zeros((128, NV), np.float32)

    def put(name, arr):
        V[:, VOFF[name]:VOFF[name] + arr.shape[1]] = arr

    put("g1", fm(inp["conv_norm_g"][0]))
    put("b1", fm(inp["conv_b_pw1"][0]))
    put("bdw", fm(inp["conv_b_dw"][0]))
    put("lng", fm(inp["conv_ln_g"][0]))
    put("lnb", fm(inp["conv_ln_b"][0]))
    put("b2", fm(inp["conv_b_pw2"][0]))
    wdw = np.asarray(inp["conv_w_dw"][0], np.float32)
    put("wdw", np.ascontiguousarray(wdw.reshape(31, 16, 128).transpose(2, 1, 0)).reshape(128, 496))
    put("gf0", fm(inp["ffn_norm_g"][0]))
    put("gf1", fm(inp["ffn_norm_g"][1]))
    for l in range(2):
        wc = np.asarray(inp["ffn_w_conv"][l], np.float32)
        put("wc%d" % l, np.ascontiguousarray(wc.reshape(3, 44, 128).transpose(2, 0, 1)).reshape(128, 132))
    put("gkv", fm(inp["kv_norm_g"]))
    put("gat", fm(inp["attn_norm_g"][0]))
    put("kng", np.asarray(inp["k_norm_g"], np.float32).reshape(128, 1))
    put("qng", np.asarray(inp["q_norm_g"][0], np.float32).reshape(128, 1))
    V[:, VOFF["hflag"]] = hflag
    V[:, VOFF["eps"]] = EPS
    return V


class Ctx:
    pass


def setup_common(P):
    C = Ctx()
    C.P = P
    nc = P.nc
    C.vecs_d = P.dram("vecs", [128, NV], F32, "ExternalInput")
    C.ident_d = P.dram("ident", [128, 128], F32, "ExternalInput")
    C.R1 = P.sb("R1", [128, 16 * WIN + 8 * OW], F32)
    C.bufA = C.R1[:, 0:16 * WIN].rearrange("p (c t) -> p c t", t=WIN)
    C.bufC = C.R1[:, 16 * WIN:16 * WIN + 8 * OW].bitcast(BF16).rearrange("p (c t) -> p c t", t=OW)
    C.mbuf = C.R1[:, 0:FC * TT // 2].bitcast(BF16).rearrange("p (c t) -> p c t", t=TT)
    C.bufB_t = P.sb("bufB", [128, 16 * WIN], BF16)
    C.bufB = C.bufB_t[:, :].rearrange("p (c t) -> p c t", t=WIN)
    C.A = [Res() for _ in range(16)]
    C.B = [Res() for _ in range(16)]
    C.Cc = [Res() for _ in range(16)]
    C.M = [Res() for _ in range(FC)]
    C.wring = P.sb("wring", [128, NWSLOT * WSLOT], BF16)
    C.wres = [Res() for _ in range(NWSLOT)]
    C.wi = 0
    C.S = P.sb("S", [128, 7424], F32)
    C.vecs = P.sb("vecs_sb", [128, NV], F32)
    C.ident = P.sb("ident_sb", [128, 128], F32)
    C.ones = P.sb("ones_sb", [128, 128], BF16)
    C.psum = P.ps("psum", [128, 4096], F32)
    C.bank = [C.psum[:, i * 512:(i + 1) * 512] for i in range(8)]
    C.bres = [Res() for _ in range(8)]
    C.cres = Res()
    P.sp.dma(C.vecs[:], C.vecs_d, w=[C.cres])
    P.sp.dma(C.ident[:], C.ident_d, w=[C.cres])
    P.dve.op(lambda e: e.memset(C.ones[:], 1.0), w=[C.cres])
    P.fence()
    return C


def vcol(C, name, i=0):
    o = VOFF[name] + i
    return C.vecs[:, o:o + 1]


def wget(C, src, kcn, ncols):
    P = C.P
    k = C.wi
    C.wi = (k + 1) % NWSLOT
    view = C.wring[:, k * WSLOT:k * WSLOT + kcn * ncols].rearrange("p (k c) -> p k c", c=ncols)
    P.pool.dma(view, src.rearrange("(k p) c -> p k c", p=128), w=[C.wres[k]])
    return view, C.wres[k]


def scratch(C, off, n, dt=F32):
    if dt == F32:
        return C.S[:, off:off + n]
    return C.S[:, off:off + (n + 1) // 2].bitcast(BF16)[:, 0:n]


def phase_rmsnorm(C, W, gname):
    P = C.P
    sq = [scratch(C, 0, 512, BF16), scratch(C, 256, 512, BF16)]
    sqr = [Res(), Res()]
    rt = scratch(C, 512, W)
    rstd = scratch(C, 512 + WIN, W)
    rr = Res()
    cnt = 0
    for ri, (c0, n) in enumerate(regions(W)):
        bk = C.bank[ri]
        for c in range(16):
            s = cnt % 2
            cnt += 1
            P.act.op(lambda e, s=s, c=c, c0=c0, n=n: e.activation(out=sq[s][:, 0:n], in_=C.bufA[:, c, c0:c0 + n], func=AF.Square),
                     r=[C.A[c]], w=[sqr[s]])
            P.mm(bk[:, 0:n], C.ones[:], sq[s][:, 0:n], start=(c == 0), stop=(c == 15), r=[sqr[s], C.cres], w=[C.bres[ri]], last=True)
        P.act.op(lambda e, bk=bk, c0=c0, n=n: e.activation(out=rt[:, c0:c0 + n], in_=bk[:, 0:n], func=AF.Sqrt, bias=vcol(C, "eps"), scale=1.0 / D),
                 r=[C.bres[ri], C.cres], w=[rr])
        P.dve.op(lambda e, c0=c0, n=n: e.reciprocal(out=rstd[:, c0:c0 + n], in_=rt[:, c0:c0 + n]), r=[rr], w=[rr])
    for c in range(16):
        P.dve.op(lambda e, c=c: e.scalar_tensor_tensor(out=C.bufB[:, c, 0:W], in0=C.bufA[:, c, 0:W], scalar=vcol(C, gname, c),
                                                       in1=rstd[:, 0:W], op0=ALU.mult, op1=ALU.mult),
                 r=[C.A[c], rr, C.cres], w=[C.B[c]])


def load_window(C, src, col0, W):
    P = C.P
    for c in range(16):
        P.sp.dma(C.bufA[:, c, 0:W], src[:, c, col0:col0 + W], w=[C.A[c]])


def phase_conformer(C, tt, xT, X1A, hf):
    P = C.P
    w1 = C.w_pw1
    w2 = C.w_pw2
    col0 = tt * TT
    load_window(C, xT, col0, WIN)
    phase_rmsnorm(C, WIN, "g1")
    P.fence()
    sg = scratch(C, 0, WIN)
    u = [scratch(C, 1056, WIN, BF16), scratch(C, 1056 + 528, WIN, BF16)]
    dg = [scratch(C, 2112, 31 * 128, BF16).rearrange("p (k c) -> p k c", c=128),
          scratch(C, 2112 + 1984, 31 * 128, BF16).rearrange("p (k c) -> p k c", c=128)]
    vh = scratch(C, 6080, 4)
    junk = scratch(C, 6088, 32)
    sgr = Res()
    ur = [Res(), Res()]
    dgr = [Res(), Res()]
    vhr = Res()
    regs = regions(WIN)
    wdwv = C.vecs[:, VOFF["wdw"]:VOFF["wdw"] + 496].rearrange("p (j k) -> p j k", k=31)

    TD = 9
    cacc = scratch(C, 6120, TT)
    caccr = Res()

    def conv(j):
        s = j % 2
        for ro in range(2):
            bk = C.bank[6 + ro]
            for k in range(TD, 31):
                o0 = 2 + ro * 512 + k
                P.mm(bk[:, :], dg[s][:, k, :], u[s][:, o0:o0 + 512], start=(k == TD), stop=(k == 30),
                     r=[dgr[s], ur[s]], w=[C.bres[6 + ro]], last=(k == 30))
        for k in range(TD):
            if k == 0:
                P.dve.op(lambda e, s=s, j=j, k=k: e.tensor_scalar(out=cacc[:, :], in0=u[s][:, 2 + k:2 + k + TT], scalar1=vcol(C, "wdw", j * 31 + k),
                                                                scalar2=None, op0=ALU.mult), r=[ur[s], C.cres], w=[caccr])
            else:
                P.dve.op(lambda e, s=s, j=j, k=k: e.scalar_tensor_tensor(out=cacc[:, :], in0=u[s][:, 2 + k:2 + k + TT], scalar=vcol(C, "wdw", j * 31 + k),
                                                                       in1=cacc[:, :], op0=ALU.mult, op1=ALU.add), r=[ur[s], C.cres], w=[caccr])
        P.dve.op(lambda e, j=j: e.scalar_tensor_tensor(out=C.bufA[:, j, 2:2 + TT], in0=C.psum[:, 6 * 512:8 * 512], scalar=vcol(C, "bdw", j), in1=cacc[:, :],
                                                      op0=ALU.add, op1=ALU.add), r=[C.bres[6], C.bres[7], caccr, C.cres], w=[C.A[j]])
        for o in range(2):
            P.dve.op(lambda e, o=o, s=s, j=j: e.tensor_tensor(out=junk[:, 0:31], in0=u[s][:, o:o + 31], in1=wdwv[:, j, :], op=ALU.mult),
                     r=[ur[s], C.cres], w=[vhr])
            P.dve.op(lambda e, o=o: e.reduce_sum(out=vh[:, o:o + 1], in_=junk[:, 0:31], axis=AX.X), r=[vhr], w=[vhr])
        P.dve.op(lambda e, j=j: e.tensor_scalar(out=C.bufA[:, j, 0:2], in0=vh[:, 0:2], scalar1=vcol(C, "bdw", j), scalar2=None, op0=ALU.add),
                 r=[vhr, C.cres], w=[C.A[j]])

    for j in range(16):
        s = j % 2
        if j % 2 == 0:
            wa, war = wget(C, w1[:, j * 128:(j + 2) * 128], 16, 256)
            wg, wgr = wget(C, w1[:, D + j * 128:D + (j + 2) * 128], 16, 256)
        jc = (j % 2) * 128
        for kc in range(16):
            for ri, (c0, n) in enumerate(regs):
                P.mm(C.bank[ri][:, 0:n], wg[:, kc, jc:jc + 128], C.bufB[:, kc, c0:c0 + n], start=(kc == 0), stop=(kc == 15),
                     r=[wgr, C.B[kc]], w=[C.bres[ri]], last=(kc == 15))
        for ri, (c0, n) in enumerate(regs):
            P.act.op(lambda e, ri=ri, c0=c0, n=n, j=j: e.activation(out=sg[:, c0:c0 + n], in_=C.bank[ri][:, 0:n], func=AF.Sigmoid,
                                                                  bias=vcol(C, "b1", 16 + j), scale=1.0),
                     r=[C.bres[ri], C.cres], w=[sgr])
        for kc in range(16):
            for ri, (c0, n) in enumerate(regs):
                P.mm(C.bank[3 + ri][:, 0:n], wa[:, kc, jc:jc + 128], C.bufB[:, kc, c0:c0 + n], start=(kc == 0), stop=(kc == 15),
                     r=[war, C.B[kc]], w=[C.bres[3 + ri]], last=(kc == 15))
        for ri, (c0, n) in enumerate(regs):
            P.dve.op(lambda e, ri=ri, c0=c0, n=n, j=j, s=s: e.scalar_tensor_tensor(out=u[s][:, c0:c0 + n], in0=C.bank[3 + ri][:, 0:n],
                                                                               scalar=vcol(C, "b1", j), in1=sg[:, c0:c0 + n],
                                                                               op0=ALU.add, op1=ALU.mult),
                     r=[C.bres[3 + ri], sgr, C.cres], w=[ur[s]])
        if hf is not None:
            P.dve.op(lambda e, s=s: e.tensor_scalar(out=u[s][:, 0:HALO], in0=u[s][:, 0:HALO], scalar1=vcol(C, hf), scalar2=None, op0=ALU.mult),
                     r=[C.cres], w=[ur[s]])
        for k in range(TD, 31):
            P.act.op(lambda e, s=s, k=k, j=j: e.activation(out=dg[s][:, k, :], in_=C.ident[:], func=AF.Identity, scale=vcol(C, "wdw", j * 31 + k)),
                     r=[C.cres], w=[dgr[s]])
        if j >= 1:
            conv(j - 1)
    conv(15)
    P.fence()
    sq = [scratch(C, 0, OW, BF16), scratch(C, 520, OW, BF16)]
    vb = [scratch(C, 1040, OW, BF16), scratch(C, 1560, OW, BF16)]
    mean = scratch(C, 2080, OW)
    rstd = scratch(C, 2080 + OW, OW)
    t1 = [scratch(C, 2080 + 2 * OW, OW), scratch(C, 2080 + 3 * OW, OW)]
    sqr = [Res(), Res()]
    vbr = [Res(), Res()]
    mr = Res()
    t1r = [Res(), Res()]
    oregs = regions(OW)
    for c in range(16):
        s = c % 2
        P.act.op(lambda e, s=s, c=c: e.activation(out=sq[s][:, 0:OW], in_=C.bufA[:, c, 0:OW], func=AF.Square), r=[C.A[c]], w=[sqr[s]])
        P.dve.op(lambda e, s=s, c=c: e.tensor_copy(out=vb[s][:, 0:OW], in_=C.bufA[:, c, 0:OW]), r=[C.A[c]], w=[vbr[s]])
        for ri, (c0, n) in enumerate(oregs):
            P.mm(C.bank[ri][:, 0:n], C.ones[:], vb[s][:, c0:c0 + n], start=(c == 0), stop=(c == 15), r=[vbr[s], C.cres], w=[C.bres[ri]], last=True)
            P.mm(C.bank[3 + ri][:, 0:n], C.ones[:], sq[s][:, c0:c0 + n], start=(c == 0), stop=(c == 15), r=[sqr[s], C.cres], w=[C.bres[3 + ri]], last=True)
    for ri, (c0, n) in enumerate(oregs):
        P.act.op(lambda e, ri=ri, c0=c0, n=n: e.activation(out=mean[:, c0:c0 + n], in_=C.bank[ri][:, 0:n], func=AF.Identity, scale=1.0 / D),
                 r=[C.bres[ri]], w=[mr])
        P.dve.op(lambda e, c0=c0, n=n: e.tensor_tensor(out=t1[0][:, c0:c0 + n], in0=mean[:, c0:c0 + n], in1=mean[:, c0:c0 + n], op=ALU.mult),
                 r=[mr], w=[t1r[0]])
        P.dve.op(lambda e, ri=ri, c0=c0, n=n: e.scalar_tensor_tensor(out=t1[1][:, c0:c0 + n], in0=C.bank[3 + ri][:, 0:n], scalar=1.0 / D,
                                                                    in1=t1[0][:, c0:c0 + n], op0=ALU.mult, op1=ALU.subtract),
                 r=[C.bres[3 + ri], t1r[0]], w=[t1r[1]])
        P.act.op(lambda e, c0=c0, n=n: e.activation(out=t1[0][:, c0:c0 + n], in_=t1[1][:, c0:c0 + n], func=AF.Sqrt, bias=vcol(C, "eps"), scale=1.0),
                 r=[t1r[1], C.cres], w=[t1r[0]])
        P.dve.op(lambda e, c0=c0, n=n: e.reciprocal(out=rstd[:, c0:c0 + n], in_=t1[0][:, c0:c0 + n]), r=[t1r[0]], w=[mr])
    for c in range(16):
        s = c % 2
        P.dve.op(lambda e, s=s, c=c: e.tensor_tensor(out=t1[s][:, 0:OW], in0=C.bufA[:, c, 0:OW], in1=mean[:, 0:OW], op=ALU.subtract),
                 r=[C.A[c], mr], w=[t1r[s]])
        P.dve.op(lambda e, s=s: e.tensor_tensor(out=t1[s][:, 0:OW], in0=t1[s][:, 0:OW], in1=rstd[:, 0:OW], op=ALU.mult),
                 r=[mr], w=[t1r[s]])
        P.act.op(lambda e, s=s, c=c: e.activation(out=C.bufC[:, c, 0:OW], in_=t1[s][:, 0:OW], func=AF.Silu, bias=vcol(C, "lnb", c), scale=vcol(C, "lng", c)),
                 r=[t1r[s], C.cres], w=[C.Cc[c]])
    P.fence()
    xr = [scratch(C, 0, OW), scratch(C, OW, OW)]
    xrr = [Res(), Res()]
    for oc in range(16):
        s = oc % 2
        if oc % 2 == 0:
            wv, wr = wget(C, w2[:, oc * 128:(oc + 2) * 128], 16, 256)
        jc = (oc % 2) * 128
        bs = 3 * s
        P.sp.dma(xr[s][:, 0:OW], xT[:, oc, col0 + 30:col0 + 30 + OW], w=[xrr[s]])
        for kc in range(16):
            for ri, (c0, n) in enumerate(oregs):
                P.mm(C.bank[bs + ri][:, 0:n], wv[:, kc, jc:jc + 128], C.bufC[:, kc, c0:c0 + n], start=(kc == 0), stop=(kc == 15),
                     r=[wr, C.Cc[kc]], w=[C.bres[bs + ri]], last=(kc == 15))
        for ri, (c0, n) in enumerate(oregs):
            P.dve.op(lambda e, bs=bs, ri=ri, c0=c0, n=n, oc=oc, s=s: e.scalar_tensor_tensor(out=C.bufA[:, oc, c0:c0 + n], in0=C.bank[bs + ri][:, 0:n],
                                                                                       scalar=vcol(C, "b2", oc), in1=xr[s][:, c0:c0 + n],
                                                                                       op0=ALU.add, op1=ALU.add),
                     r=[C.bres[bs + ri], xrr[s], C.cres], w=[C.A[oc]])
        if tt == 0:
            P.act.dma(X1A[:, oc, 0:OW], C.bufA[:, oc, 0:OW], r=[C.A[oc]])
        else:
            P.act.dma(X1A[:, oc, 2 + tt * TT:2 + (tt + 1) * TT], C.bufA[:, oc, 2:OW], r=[C.A[oc]])
    P.fence()


def phase_ffn(C, tt, l, XRES, XOUT, res_halo, out_halo, hf):
    P = C.P
    w_in = C.w_in[l]
    w_out = C.w_out[l]
    phase_rmsnorm(C, OW, "gf%d" % l)
    P.fence()
    y = [scratch(C, 0, TT), scratch(C, TT, TT)]
    sl = [scratch(C, 2 * TT, TT), scratch(C, 3 * TT, TT)]
    yr = [Res(), Res()]
    sr = [Res(), Res()]
    wcn = "wc%d" % l
    for c in range(FC):
        s = c % 2
        if c % 2 == 0:
            wg, wgr = wget(C, w_in[:, c * 128:(c + 2) * 128], 16, 256)
            wu, wur = wget(C, w_in[:, FF + c * 128:FF + (c + 2) * 128], 16, 256)
        jc = (c % 2) * 128
        gb = 0 if s == 0 else 5
        gps = C.psum[:, gb * 512:gb * 512 + OW]
        for kc in range(16):
            for ri, (c0, n) in enumerate(regions(OW)):
                P.mm(C.bank[gb + ri][:, 0:n], wg[:, kc, jc:jc + 128], C.bufB[:, kc, c0:c0 + n], start=(kc == 0), stop=(kc == 15),
                     r=[wgr, C.B[kc]], w=[C.bres[gb + ri]], last=(kc == 15))
        gres = [C.bres[gb], C.bres[gb + 1], C.bres[gb + 2]]
        if hf is not None:
            P.dve.op(lambda e, gps=gps: e.tensor_scalar(out=gps[:, 0:2], in0=gps[:, 0:2], scalar1=vcol(C, hf), scalar2=None, op0=ALU.mult),
                     r=[C.cres], w=gres)
        P.act.op(lambda e, gps=gps, s=s, c=c: e.activation(out=y[s][:, :], in_=gps[:, 2:OW], func=AF.Identity, scale=vcol(C, wcn, 2 * FC + c)),
                 r=gres + [C.cres], w=[yr[s]])
        P.dve.op(lambda e, gps=gps, s=s, c=c: e.scalar_tensor_tensor(out=y[s][:, :], in0=gps[:, 1:OW - 1], scalar=vcol(C, wcn, FC + c), in1=y[s][:, :],
                                                                  op0=ALU.mult, op1=ALU.add), r=gres + [C.cres], w=[yr[s]])
        P.dve.op(lambda e, gps=gps, s=s, c=c: e.scalar_tensor_tensor(out=y[s][:, :], in0=gps[:, 0:OW - 2], scalar=vcol(C, wcn, c), in1=y[s][:, :],
                                                                  op0=ALU.mult, op1=ALU.add), r=gres + [C.cres], w=[yr[s]])
        P.act.op(lambda e, s=s: e.activation(out=sl[s][:, :], in_=y[s][:, :], func=AF.Silu), r=[yr[s]], w=[sr[s]])
        for kc in range(16):
            for ri in range(2):
                P.mm(C.bank[3 + ri][:, :], wu[:, kc, jc:jc + 128], C.bufB[:, kc, 2 + ri * 512:2 + ri * 512 + 512], start=(kc == 0), stop=(kc == 15),
                     r=[wur, C.B[kc]], w=[C.bres[3 + ri]], last=(kc == 15))
        P.dve.op(lambda e, s=s, c=c: e.tensor_tensor(out=C.mbuf[:, c, :], in0=C.psum[:, 3 * 512:5 * 512], in1=sl[s][:, :], op=ALU.mult),
                 r=[C.bres[3], C.bres[4], sr[s]], w=[C.M[c]])
    P.fence()
    xr = [scratch(C, 0, TT), scratch(C, TT, TT)]
    xo = [scratch(C, 2 * TT, TT), scratch(C, 3 * TT, TT)]
    xrr = [Res(), Res()]
    xor_ = [Res(), Res()]
    for oc in range(16):
        s = oc % 2
        wv, wr = wget(C, w_out[:, oc * 128:(oc + 1) * 128], FC, 128)
        P.sp.dma(xr[s][:, :], XRES[:, oc, res_halo + tt * TT:res_halo + (tt + 1) * TT], w=[xrr[s]])
        bs = 2 * s
        for c in range(FC):
            for ri in range(2):
                P.mm(C.bank[bs + ri][:, :], wv[:, c, :], C.mbuf[:, c, ri * 512:(ri + 1) * 512], start=(c == 0), stop=(c == FC - 1),
                     r=[wr, C.M[c]], w=[C.bres[bs + ri]], last=(c == FC - 1))
        P.dve.op(lambda e, bs=bs, s=s: e.tensor_tensor(out=xo[s][:, :], in0=C.psum[:, bs * 512:(bs + 2) * 512], in1=xr[s][:, :], op=ALU.add),
                 r=[C.bres[bs], C.bres[bs + 1], xrr[s]], w=[xor_[s]])
        P.act.dma(XOUT[:, oc, out_halo + tt * TT:out_halo + (tt + 1) * TT], xo[s][:, :], r=[xor_[s]])
    P.fence()


_CACHE = {}


def _ident():
    return np.eye(128, dtype=np.float32)


def phase_proj_hn(C, w, colbase, gname, scale, dst, dst_col0, km=None, km_col0=0, ncols=TT):
    P = C.P
    sqk = [scratch(C, 0, TT, BF16), scratch(C, 5300, TT, BF16)]
    rt = scratch(C, 512, TT)
    rs = scratch(C, 1536, TT)
    kf = scratch(C, 2560, TT)
    kb = [scratch(C, 3584, TT, BF16), scratch(C, 4096, TT, BF16)]
    kms = scratch(C, 4608, 4)
    r_sq = [Res(), Res()]
    r_rt, r_kf, r_km = Res(), Res(), Res()
    r_kb = [Res(), Res()]
    regs = regions(ncols)
    nr = len(regs)
    wcur = {}

    def proj(hc):
        s = hc % 2
        if hc % 2 == 0:
            wcur["w"] = wget(C, w[:, colbase + hc * 128:colbase + (hc + 2) * 128], 16, 256)
        wv, wr = wcur["w"]
        jc = (hc % 2) * 128
        bs = (0, 2, 6)[hc % 3]
        for kc in range(16):
            for ri, (c0, n) in enumerate(regs):
                P.mm(C.bank[bs + ri][:, 0:n], wv[:, kc, jc:jc + 128], C.bufB[:, kc, c0:c0 + n], start=(kc == 0), stop=(kc == 15),
                     r=[wr, C.B[kc]], w=[C.bres[bs + ri]], last=(kc == 15))
        yps = C.psum[:, bs * 512:bs * 512 + ncols]
        yres = [C.bres[bs + ri] for ri in range(nr)]
        P.act.op(lambda e, yps=yps: e.activation(out=sqk[s][:, 0:ncols], in_=yps, func=AF.Square), r=yres, w=[r_sq[s]])

    def epi(hc):
        s = hc % 2
        bs = (0, 2, 6)[hc % 3]
        yps = C.psum[:, bs * 512:bs * 512 + ncols]
        yres = [C.bres[bs + ri] for ri in range(nr)]
        for ri, (c0, n) in enumerate(regs):
            P.mm(C.bank[4 + ri][:, 0:n], C.ones[:], sqk[s][:, c0:c0 + n], start=True, stop=True, r=[r_sq[s], C.cres], w=[C.bres[4 + ri]], last=True)
        P.act.op(lambda e: e.activation(out=rt[:, 0:ncols], in_=C.psum[:, 4 * 512:4 * 512 + ncols], func=AF.Sqrt, bias=vcol(C, "eps"), scale=1.0 / 128),
                 r=[C.bres[4 + ri] for ri in range(nr)] + [C.cres], w=[r_rt])
        P.dve.op(lambda e: e.reciprocal(out=rs[:, 0:ncols], in_=rt[:, 0:ncols]), r=[r_rt], w=[r_rt])
        P.dve.op(lambda e, yps=yps: e.scalar_tensor_tensor(out=kf[:, 0:ncols], in0=yps, scalar=vcol(C, gname), in1=rs[:, 0:ncols], op0=ALU.mult, op1=ALU.mult),
                 r=yres + [r_rt, C.cres], w=[r_kf])
        P.act.op(lambda e, s=s: e.activation(out=kb[s][:, 0:ncols], in_=kf[:, 0:ncols], func=AF.Identity, scale=float(scale)), r=[r_kf], w=[r_kb[s]])
        P.act.dma(dst[:, hc, dst_col0:dst_col0 + ncols], kb[s][:, 0:ncols], r=[r_kb[s]])
        if km is not None:
            P.dve.op(lambda e: e.tensor_reduce(out=kms[:, 0:4], in_=kf[:, :].rearrange("p (n j) -> p n j", j=256), axis=AX.X, op=ALU.add),
                     r=[r_kf], w=[r_km])
            P.act.op(lambda e, hc=hc: e.activation(out=km[:, hc, km_col0:km_col0 + 4], in_=kms[:, 0:4], func=AF.Identity, scale=1.0 / 256),
                     r=[r_km], w=[C.kmres])

    for hc in range(16):
        proj(hc)
        if hc >= 1:
            epi(hc - 1)
    epi(15)


def phase_v(C, w, colbase, V_d, tok0):
    P = C.P
    vst = [scratch(C, 5812, 512, BF16), scratch(C, 6068, 512, BF16)]
    vr = [Res(), Res()]
    base = 16 * WIN
    vw = [C.R1[:, base + i * 4096:base + (i + 1) * 4096].bitcast(BF16).rearrange("p (k c) -> p k c", c=512) for i in range(2)]
    if not hasattr(C, "vwr"):
        C.vwr = [Res(), Res()]
        C.vcnt = 0
    for vb in range(4):
        i = vb % 2
        P.pool.dma(vw[i], w[:, colbase + vb * 512:colbase + (vb + 1) * 512].rearrange("(k p) c -> p k c", p=128), w=[C.vwr[i]])
        for tk in range(8):
            s = C.vcnt % 2
            C.vcnt += 1
            bk = 6 + s
            for kc in range(16):
                P.mm(C.bank[bk][:, :], C.bufB[:, kc, tk * 128:(tk + 1) * 128], vw[i][:, kc, :], start=(kc == 0), stop=(kc == 15),
                     r=[C.vwr[i], C.B[kc]], w=[C.bres[bk]], last=(kc == 15))
            P.act.op(lambda e, s=s, bk=bk: e.activation(out=vst[s][:, :], in_=C.bank[bk][:, :], func=AF.Identity), r=[C.bres[bk]], w=[vr[s]])
            P.act.dma(V_d[tok0 + tk * 128:tok0 + (tk + 1) * 128, vb * 512:(vb + 1) * 512], vst[s][:, :], r=[vr[s]])


def r1v(C, off, n, dt=F32):
    if dt == F32:
        return C.R1[:, off:off + n]
    return C.R1[:, off:off + (n + 1) // 2].bitcast(BF16)[:, 0:n]


def attention_setup(C, D_):
    P = C.P
    A = Ctx()
    A.Qb = [r1v(C, 0, 8192, BF16).rearrange("p (h q) -> p h q", q=512), C.bufB_t[:, 0:8192].rearrange("p (h q) -> p h q", q=512)]
    A.KTh = [r1v(C, 4096, 4096, BF16), r1v(C, 6144, 4096, BF16)]
    A.Vh = [r1v(C, 8192, 4096, BF16).rearrange("p (c d) -> p c d", d=128), r1v(C, 10240, 4096, BF16).rearrange("p (c d) -> p c d", d=128)]
    A.Rt = [r1v(C, 12288, 8192, BF16).rearrange("p (h q) -> p h q", q=512), C.bufB_t[:, 8192:16384].rearrange("p (h q) -> p h q", q=512)]
    A.pt = [r1v(C, 16384 + i * 256, 512, BF16) for i in range(4)]
    A.ssb = [r1v(C, 17408, 512), r1v(C, 17920, 512)]
    A.acc = [r1v(C, 18432, 512), r1v(C, 18944, 512)]
    A.rl = [r1v(C, 19456, 512), r1v(C, 19968, 512)]
    A.ot = [r1v(C, 20480, 512, BF16), r1v(C, 20736, 512, BF16)]
    A.gm = r1v(C, 20992, 256)
    A.sel = r1v(C, 21248, 256)
    A.rbq = r1v(C, 21504, 256, BF16)
    A.mx = r1v(C, 21632, 128).rearrange("p (h k) -> p h k", k=8)
    A.thr = r1v(C, 21760, 16)
    A.Em = r1v(C, 21776, 2048, BF16).rearrange("p (n j) -> p n j", j=128)
    A.cmask = r1v(C, 22800, 1024).rearrange("p (t q) -> p t q", t=2)
    A.kmb = r1v(C, 23824, 256, BF16).rearrange("p (h n) -> p h n", n=16)
    A.identb = r1v(C, 23952, 128, BF16)
    A.ones32 = r1v(C, 24016, 128)
    A.pastm = C.S[:, 0:2304].rearrange("p (b x) -> p b x", x=256)
    A.ownm = C.S[:, 2304:4608].rearrange("p (b x) -> p b x", x=256)
    A.r_const = Res()
    A.r_Qb = [Res(), Res()]
    A.r_Rt = [Res(), Res()]
    A.r_KT = [Res(), Res()]
    A.r_V = [Res(), Res()]
    A.r_pt = [Res() for _ in range(4)]
    A.r_ssb = [Res(), Res()]
    A.r_acc = [Res(), Res()]
    A.r_g = Res()
    A.r_rl = [Res(), Res()]
    A.r_ot = [Res(), Res()]
    w = [A.r_const]
    for bf in range(2):
        P.dve.op(lambda e, bf=bf: e.memset(A.Rt[bf][:, :, :], 0.0), w=[A.r_Rt[bf]])
    P.dve.op(lambda e: e.memset(A.ones32[:, :], 1.0), w=w)
    P.sp.dma(A.Em[0:34, :, :], D_["emat"], w=w)
    P.sp.dma(A.cmask[:, :, :], D_["cmask"].rearrange("p (t q) -> p t q", t=2), w=w)
    P.sp.dma(A.pastm[:, :, :], D_["pastm"].rearrange("p (b x) -> p b x", x=256), w=w)
    P.sp.dma(A.ownm[:, :, :], D_["ownm"].rearrange("p (b x) -> p b x", x=256), w=w)
    P.act.op(lambda e: e.activation(out=A.kmb[:, :, :], in_=C.kmT[:, :].rearrange("p (h n) -> p h n", n=16), func=AF.Identity), r=[C.kmres], w=w)
    P.act.op(lambda e: e.activation(out=A.identb[:, :], in_=C.ident[:], func=AF.Identity), r=[C.cres], w=w)
    return A


def attention_all(C, A, units, QT_d, KT_d, V_d, OT_d, qal_d):
    P = C.P
    cst = [A.r_const]
    GB = 3
    gps = C.bank[GB]
    tps = C.psum[:, GB * 512 + 256:(GB + 1) * 512].bitcast(BF16)

    def gating_steps(ui):
        i0, nqb = units[ui]
        bf = ui % 2
        NQc = nqb * 256
        q0 = i0 * 256
        Qb, Rt = A.Qb[bf], A.Rt[bf]
        steps = []

        def load():
            P.sp.dma(Qb[:, :, 0:NQc], QT_d[:, :, q0:q0 + NQc], w=[A.r_Qb[bf]])
            P.sp.dma(Rt[32:34, :, 0:NQc], qal_d[:, :, q0:q0 + NQc], w=[A.r_Rt[bf]])
        steps.append(load)
        for qt in range(NQc // 128):
            qi = i0 + qt // 2

            def stepA(qt=qt, qi=qi):
                for h in range(16):
                    P.mm(gps[:, h * 16:(h + 1) * 16], Qb[:, h, qt * 128:(qt + 1) * 128], A.kmb[:, h, :], start=True, stop=True,
                         r=[A.r_Qb[bf]] + cst, w=[C.bres[GB]], last=(h == 15))
                P.dve.op(lambda e: e.tensor_tensor(out=A.gm[:, :], in0=gps[:, 0:256], in1=A.pastm[:, qi, :], op=ALU.add),
                         r=[C.bres[GB]] + cst, w=[A.r_g])
                for h in range(16):
                    P.dve.op(lambda e, h=h: e.max(out=A.mx[:, h, :], in_=A.gm[:, h * 16:(h + 1) * 16]), r=[A.r_g], w=[A.r_g])
                P.dve.op(lambda e: e.tensor_scalar(out=A.thr[:, :], in0=A.mx[:, :, 2], scalar1=-1e29, scalar2=None, op0=ALU.max), r=[A.r_g], w=[A.r_g])
                for h in range(16):
                    P.dve.op(lambda e, h=h: e.tensor_scalar(out=A.sel[:, h * 16:(h + 1) * 16], in0=A.gm[:, h * 16:(h + 1) * 16], scalar1=A.thr[:, h:h + 1],
                                                            scalar2=None, op0=ALU.is_ge), r=[A.r_g], w=[A.r_g])
                P.dve.op(lambda e: e.tensor_tensor(out=A.sel[:, :], in0=A.sel[:, :], in1=A.ownm[:, qi, :], op=ALU.max), r=[A.r_g] + cst, w=[A.r_g])
                P.dve.op(lambda e: e.tensor_scalar(out=A.rbq[:, :], in0=A.sel[:, :], scalar1=-1.0, scalar2=30000.0, op0=ALU.add, op1=ALU.mult),
                         r=[A.r_g], w=[A.r_g])

            def stepB(qt=qt):
                for hg in range(4):
                    for hh in range(4):
                        h = hg * 4 + hh
                        P.pe.op(lambda e, h=h, hh=hh: e.transpose(tps[0:16, hh * 128:(hh + 1) * 128], A.rbq[:, h * 16:(h + 1) * 16], A.identb[:, :]),
                                r=[A.r_g] + cst, w=[C.bres[GB]])
                    P.act.op(lambda e, hg=hg: e.activation(out=Rt[0:16, hg * 4:(hg + 1) * 4, qt * 128:(qt + 1) * 128],
                                                           in_=tps[0:16, :].rearrange("p (h q) -> p h q", q=128), func=AF.Identity),
                             r=[C.bres[GB]], w=[A.r_Rt[bf]])
            steps.append(stepA)
            steps.append(stepB)
        return steps

    def unit_tiles(ui):
        i0, nqb = units[ui]
        NQc = nqb * 256
        nkb = 8 + i0 + nqb - 1
        tiles = []
        for h in range(16):
            lst = []
            for n in range(nkb):
                for jt in range(2):
                    if n <= 7 + i0:
                        lst.append((n, jt, 0, NQc))
                    else:
                        lst.append((n, jt, 256, 256))
            for ist, (n, jt, c0, ncl) in enumerate(lst):
                tiles.append((h, n, jt, c0, ncl, ist, len(lst)))
        return tiles

    cnt = {"k": 0}
    for st in gating_steps(0):
        st()
    for ui, (i0, nqb) in enumerate(units):
        bf = ui % 2
        NQc = nqb * 256
        nkb = 8 + i0 + nqb - 1
        nk = nkb * 256
        q0 = i0 * 256
        Qb, Rt = A.Qb[bf], A.Rt[bf]
        tiles = unit_tiles(ui)
        nxt = gating_steps(ui + 1) if ui + 1 < len(units) else []

        def load_head(h):
            s = h % 2
            P.sp.dma(A.KTh[s][:, 0:nk], KT_d[:, h, 0:nk], w=[A.r_KT[s]])
            P.sp.dma(A.Vh[s][:, 0:nkb * 2, :], V_d[0:nk, h * 128:(h + 1) * 128].rearrange("(c p) d -> p c d", p=128), w=[A.r_V[s]])

        def score(k, kg):
            h, n, jt, c0, ncl, ist, nst = tiles[k]
            s = h % 2
            if ist == 0:
                load_head(h)
            sb_ = C.bank[kg % 3]
            sr = C.bres[kg % 3]
            P.mm(sb_[:, 0:ncl], A.KTh[s][:, n * 256 + jt * 128:n * 256 + (jt + 1) * 128], Qb[:, h, c0:c0 + ncl], start=True, stop=False,
                 r=[A.r_KT[s], A.r_Qb[bf]], w=[sr])
            P.mm(sb_[:, 0:ncl], A.Em[0:34, n, :], Rt[0:34, h, c0:c0 + ncl], start=False, stop=True, r=[A.r_Rt[bf]] + cst, w=[sr], last=True)
            p = kg % 4
            bias = C.kalf[:, h, n * 2 + jt:n * 2 + jt + 1]
            if n >= 7 + i0:
                x = kg % 2
                P.dve.op(lambda e: e.tensor_tensor(out=A.ssb[x][:, 0:ncl], in0=sb_[:, 0:ncl], in1=A.cmask[:, jt, 0:ncl], op=ALU.add),
                         r=[sr] + cst, w=[A.r_ssb[x]])
                P.act.op(lambda e: e.activation(out=A.pt[p][:, 0:ncl], in_=A.ssb[x][:, 0:ncl], func=AF.Exp, bias=bias, scale=1.0),
                         r=[A.r_ssb[x]] + cst, w=[A.r_pt[p]])
            else:
                P.act.op(lambda e: e.activation(out=A.pt[p][:, 0:ncl], in_=sb_[:, 0:ncl], func=AF.Exp, bias=bias, scale=1.0),
                         r=[sr] + cst, w=[A.r_pt[p]])
            if ist == 0:
                P.dve.op(lambda e, NQc=NQc: e.tensor_copy(out=A.acc[s][:, 0:NQc], in_=A.pt[p][:, 0:NQc]), r=[A.r_pt[p]], w=[A.r_acc[s]])
            else:
                P.dve.op(lambda e: e.tensor_tensor(out=A.acc[s][:, c0:c0 + ncl], in0=A.acc[s][:, c0:c0 + ncl], in1=A.pt[p][:, 0:ncl], op=ALU.add),
                         r=[A.r_pt[p]], w=[A.r_acc[s]])

        def pvl(k, kg):
            h, n, jt, c0, ncl, ist, nst = tiles[k]
            s = h % 2
            p = kg % 4
            obi = 4 + 2 * s
            ob = C.bank[obi]
            lb = C.bank[obi + 1]
            lastst = (ist == nst - 1)
            P.mm(ob[:, c0:c0 + ncl], A.Vh[s][:, n * 2 + jt, :], A.pt[p][:, 0:ncl], start=(ist == 0), stop=lastst, r=[A.r_V[s], A.r_pt[p]], w=[C.bres[obi]], last=True)
            if lastst:
                P.pe.op(lambda e, NQc=NQc: e.matmul(lb[:, 0:NQc], A.ones32[:, :], A.acc[s][:, 0:NQc], start=True, stop=True),
                        r=[A.r_acc[s]] + cst, w=[C.bres[obi + 1]])
                P.dve.op(lambda e, NQc=NQc: e.reciprocal(out=A.rl[s][:, 0:NQc], in_=lb[:, 0:NQc]), r=[C.bres[obi + 1]], w=[A.r_rl[s]])
                P.dve.op(lambda e, NQc=NQc: e.tensor_tensor(out=A.ot[s][:, 0:NQc], in0=ob[:, 0:NQc], in1=A.rl[s][:, 0:NQc], op=ALU.mult),
                         r=[C.bres[obi], A.r_rl[s]], w=[A.r_ot[s]])
                P.act.dma(OT_d[:, h, q0:q0 + NQc], A.ot[s][:, 0:NQc], r=[A.r_ot[s]])

        LA = 2
        nt = len(tiles)
        sched = {}
        for si, st in enumerate(nxt):
            sched.setdefault(((si + 1) * nt) // (len(nxt) + 1), []).append(st)
        base = cnt["k"]
        for k in range(nt + LA):
            if k < nt:
                score(k, base + k)
            if k - LA >= 0:
                pvl(k - LA, base + k - LA)
            for st in sched.get(k, []):
                st()
        cnt["k"] = base + nt


LSEQ = 4096
QPRE = 1792
NQ = LSEQ - QPRE
QTILES = [(1792, 256), (2048, 1024), (3072, 1024)]


def build_F():
    P = Prog()
    C = setup_common(P)
    xT = P.dram("xT", [128, 16, HALO + LSEQ], F32, "ExternalInput")
    C.w_pw1 = P.dram("w_pw1", [D, 2 * D], F32, "ExternalInput")
    C.w_pw2 = P.dram("w_pw2", [D, D], F32, "ExternalInput")
    C.w_in = [P.dram("w_in0", [D, 2 * FF], F32, "ExternalInput"), P.dram("w_in1", [D, 2 * FF], F32, "ExternalInput")]
    C.w_out = [P.dram("w_out0", [FF, D], F32, "ExternalInput"), P.dram("w_out1", [FF, D], F32, "ExternalInput")]
    w_kv = P.dram("w_kv", [D, 2 * D], F32, "ExternalInput")
    w_q = P.dram("w_q", [D, D], F32, "ExternalInput")
    w_o = P.dram("w_o", [D, D], F32, "ExternalInput")
    D_ = {"emat": P.dram("emat", [34, 16, 128], BF16, "ExternalInput"),
          "kal": P.dram("kal", [128, 32], F32, "ExternalInput"),
          "cmask": P.dram("cmask", [128, 1024], F32, "ExternalInput"),
          "pastm": P.dram("pastm", [128, 2304], F32, "ExternalInput"),
          "ownm": P.dram("ownm", [128, 2304], F32, "ExternalInput")}
    kalf_d = P.dram("kalf", [128, 16 * 32], F32, "ExternalInput")
    qal_d = P.dram("qal", [2, 16, NQ], BF16, "ExternalInput")
    OUT = P.dram("outT", [128, 16, TCORE], F32, "ExternalOutput")
    X1A = P.dram("X1A", [128, 16, 2 + LSEQ], F32, "Internal")
    X1 = P.dram("X1", [128, 16, 2 + LSEQ], F32, "Internal")
    X2 = P.dram("X2", [128, 16, 256 + TCORE], F32, "Internal")
    KT_d = P.dram("KT_d", [128, 16, LSEQ], BF16, "Internal")
    V_d = P.dram("V_d", [LSEQ, D], BF16, "Internal")
    QT_d = P.dram("QT_d", [128, 16, NQ], BF16, "Internal")
    OT_d = P.dram("OT_d", [128, 16, NQ], BF16, "Internal")
    C.kmT = P.sb("kmT", [128, 256], F32)
    C.kmres = Res()
    kalf_t = P.sb("kalf_sb", [128, 512], F32)
    C.kalf = kalf_t[:, :].rearrange("p (h c) -> p h c", c=32)
    P.sp.dma(kalf_t[:, :], kalf_d, w=[C.cres])
    kmv = C.kmT[:, :].rearrange("p (h n) -> p h n", n=16)
    for tt in range(4):
        hf = "zero" if tt == 0 else ("hflag" if tt == 2 else None)
        phase_conformer(C, tt, xT, X1A, hf)
        phase_ffn(C, tt, 0, X1A, X1, 2, 2, hf)
    for t4 in range(4):
        load_window(C, X1, 2 + t4 * TT, TT)
        phase_rmsnorm(C, TT, "gkv")
        P.fence()
        phase_proj_hn(C, w_kv, 0, "kng", 1.0, KT_d, t4 * TT, km=kmv, km_col0=t4 * 4)
        phase_v(C, w_kv, D, V_d, t4 * TT)
        P.fence()
    for (st, n) in QTILES:
        load_window(C, X1, 2 + st, n)
        phase_rmsnorm(C, n, "gat")
        P.fence()
        phase_proj_hn(C, w_q, 0, "qng", 128.0 ** -0.5, QT_d, st - QPRE, ncols=n)
        P.fence()
    A = attention_setup(C, D_)
    P.fence()
    attention_all(C, A, [(0, 1), (1, 2), (3, 2), (5, 2), (7, 2)], QT_d, KT_d, V_d, OT_d, qal_d)
    P.fence()
    xr = [scratch(C, 0, TT), scratch(C, TT, TT)]
    xo = [scratch(C, 2 * TT, TT), scratch(C, 3 * TT, TT)]
    xrr = [Res(), Res()]
    xor_ = [Res(), Res()]
    for (st, n) in QTILES:
        regs = regions(n)
        for c in range(16):
            P.sp.dma(C.bufB[:, c, 0:n], OT_d[:, c, st - QPRE:st - QPRE + n], w=[C.B[c]])
        for oc in range(16):
            s = oc % 2
            if oc % 2 == 0:
                wv, wr = wget(C, w_o[:, oc * 128:(oc + 2) * 128], 16, 256)
            jc = (oc % 2) * 128
            bs = 2 * s
            P.sp.dma(xr[s][:, 0:n], X1[:, oc, 2 + st:2 + st + n], w=[xrr[s]])
            for kc in range(16):
                for ri, (c0, m) in enumerate(regs):
                    P.mm(C.bank[bs + ri][:, 0:m], wv[:, kc, jc:jc + 128], C.bufB[:, kc, c0:c0 + m], start=(kc == 0), stop=(kc == 15),
                         r=[wr, C.B[kc]], w=[C.bres[bs + ri]], last=(kc == 15))
            P.dve.op(lambda e, bs=bs, s=s, n=n: e.tensor_tensor(out=xo[s][:, 0:n], in0=C.psum[:, bs * 512:bs * 512 + n], in1=xr[s][:, 0:n], op=ALU.add),
                     r=[C.bres[bs + ri] for ri in range(len(regs))] + [xrr[s]], w=[xor_[s]])
            P.act.dma(X2[:, oc, st - QPRE:st - QPRE + n], xo[s][:, 0:n], r=[xor_[s]])
        P.fence()
    for t in range(NTILE):
        load_window(C, X2, 254 + t * TT, OW)
        phase_ffn(C, t, 1, X2, OUT, 256, 0, "hflag" if t == 0 else None)
    return P.finish([])


SLOPES = np.exp2(-8.0 * np.arange(1, NH + 1, dtype=np.float64) / NH)


def attn_tables(half):
    T = {}
    em = np.zeros((34, 16, 128), np.float32)
    for n in range(16):
        em[n, n, :] = 1.0
    em[32:34, :, :] = 1.0
    T["emat"] = em.astype(ml_dtypes.bfloat16)
    T["kal"] = np.zeros((128, 32), np.float32)
    pos = (np.arange(32)[None, :] * 128 + np.arange(128)[:, None]).astype(np.float64)
    T["kalf"] = (SLOPES[None, :, None] * pos[:, None, :]).astype(np.float32).reshape(128, 512)
    tq = QPRE + np.arange(NQ, dtype=np.float64)
    v = -(SLOPES[:, None] * tq[None, :])
    hi = v.astype(np.float32).astype(ml_dtypes.bfloat16)
    lo = (v - hi.astype(np.float64)).astype(np.float32).astype(ml_dtypes.bfloat16)
    T["qal"] = np.stack([hi, lo], 0)
    kp = np.arange(2)[None, :, None] * 128 + np.arange(128)[:, None, None]
    cm = np.zeros((128, 2, 512), np.float32)
    cm[:, :, 0:256] = np.where(kp > np.arange(256)[None, None, :], -30000.0, 0.0)
    T["cmask"] = cm.reshape(128, 1024)
    pm = np.full((9, 16, 16), -1e30, np.float32)
    om = np.zeros((9, 16, 16), np.float32)
    nlo = 0 if half == 1 else 8
    for i in range(9):
        cur = 7 + i
        pm[i, :, nlo:cur] = 0.0
        om[i, :, cur] = 1.0
    T["pastm"] = np.broadcast_to(pm.reshape(1, 2304), (128, 2304)).copy()
    T["ownm"] = np.broadcast_to(om.reshape(1, 2304), (128, 2304)).copy()
    return T


_CACHE = {}


def kernel(**inputs):
    inp = {k: np.asarray(v) for k, v in inputs.items()}
    cores = list(range(8))
    if "F" not in _CACHE:
        _CACHE["F"] = build_F()
    x = inp["x"].astype(np.float32, copy=False)
    ws = {"w_pw1": np.ascontiguousarray(inp["conv_w_pw1"][0]), "w_pw2": np.ascontiguousarray(inp["conv_w_pw2"][0]),
          "w_in0": np.ascontiguousarray(inp["ffn_w_in"][0]), "w_out0": np.ascontiguousarray(inp["ffn_w_out"][0]),
          "w_in1": np.ascontiguousarray(inp["ffn_w_in"][1]), "w_out1": np.ascontiguousarray(inp["ffn_w_out"][1]),
          "w_kv": np.ascontiguousarray(inp["w_kv"]), "w_q": np.ascontiguousarray(inp["w_q"][0]), "w_o": np.ascontiguousarray(inp["w_o"][0])}
    tabs = [attn_tables(0), attn_tables(1)]
    ident = np.eye(128, dtype=np.float32)
    maps = []
    for c in cores:
        b, h = c // 2, c % 2
        xt = np.ascontiguousarray(x[b].T).reshape(16, 128, 4096).transpose(1, 0, 2)
        buf = np.zeros((128, 16, HALO + LSEQ), np.float32)
        if h == 1:
            buf[:, :, HALO:] = xt
        else:
            buf[:, :, HALO + TCORE:] = xt[:, :, 0:TCORE]
        maps.append(dict(xT=buf, vecs=pack_vecs(inp, float(h)), ident=ident, **ws, **tabs[h]))
    res = run_bass_kernel_spmd(_CACHE["F"], maps, core_ids=cores)
    out = np.zeros((4, 4096, D), np.float32)
    for c in cores:
        b, h = c // 2, c % 2
        o = res.results[c]["outT"]
        out[b, h * TCORE:(h + 1) * TCORE, :] = o.transpose(2, 1, 0).reshape(TCORE, D)
    return out
```

```python
import numpy as np
import ml_dtypes
import concourse.bass as bass
import concourse.mybir as mybir
from concourse.bass_utils import run_bass_kernel_spmd
from contextlib import ExitStack

F32 = mybir.dt.float32
BF16 = mybir.dt.bfloat16
AF = mybir.ActivationFunctionType
ALU = mybir.AluOpType
AX = mybir.AxisListType

D = 2048
KC = 16
TT = 1024
NTILE = 2
TCORE = 2048
HALO = 32
WIN = TT + HALO
OW = TT + 2
FF = 5632
FC = 44
NH = 16
EPS = 1e-6
NBLK = 16
WSLOT = 44 * 128
NWSLOT = 3


class Res:
    __slots__ = ("lw", "rd")

    def __init__(self):
        self.lw = None
        self.rd = {}


class Eng:
    def __init__(self, P, name, nsem_dma=0):
        self.P = P
        self.name = name
        self.sem = P.es.enter_context(P.nc.semaphore("sem_" + name))
        self.n = 0
        self.waited = {}
        self.rec = []
        self.dsems = [P.es.enter_context(P.nc.semaphore("d%s%d" % (name, i))) for i in range(nsem_dma)]
        self.dvals = [0] * nsem_dma
        self.di = 0

    def wait(self, deps):
        for d in deps:
            if d is None:
                continue
            sem, val, key = d
            if self.waited.get(key, 0) >= val:
                continue
            self.rec.append(("w", sem, val))
            self.waited[key] = val

    @staticmethod
    def _deps(r, w):
        d = []
        for x in r:
            if x.lw is not None:
                d.append(x.lw)
        for x in w:
            if x.lw is not None:
                d.append(x.lw)
            d.extend(x.rd.values())
        return d

    @staticmethod
    def _commit(h, r, w):
        for x in r:
            x.rd[h[2]] = h
        for x in w:
            x.lw = h
            x.rd = {}

    def op(self, fn, r=(), w=(), deps=()):
        self.wait(self._deps(r, w))
        self.wait(deps)
        self.n += 1
        self.rec.append(("i", fn, self.sem, 1))
        h = (self.sem, self.n, self.name)
        self._commit(h, r, w)
        return h

    def op_noinc(self, fn, r=(), w=()):
        self.wait(self._deps(r, w))
        self.rec.append(("n", fn))

    def mark(self):
        return (self.sem, self.n, self.name) if self.n else None

    def dma(self, out, in_, r=(), w=(), deps=(), **kw):
        k = self.di
        self.di = (self.di + 1) % len(self.dsems)
        key = "%s_d%d" % (self.name, k)
        if self.dvals[k] > 0:
            self.wait([(self.dsems[k], self.dvals[k], key)])
        self.wait(self._deps(r, w))
        self.wait(deps)
        self.dvals[k] += 16
        self.rec.append(("i", (lambda e, o=out, i=in_, kw=kw: e.dma_start(out=o, in_=i, **kw)), self.dsems[k], 16))
        h = (self.dsems[k], self.dvals[k], key)
        self._commit(h, r, w)
        self.P.dma_handles[key] = h
        return h

    def replay(self, e):
        for r in self.rec:
            if r[0] == "w":
                e.wait_ge(r[1], r[2])
            elif r[0] == "n":
                r[1](e)
            else:
                r[1](e).then_inc(r[2], r[3])


class Prog:
    def __init__(self):
        self.nc = bass.Bass("TRN2", target_bir_lowering=False)
        self.es = ExitStack()
        self.dma_handles = {}
        self.pe = Eng(self, "pe")
        self.act = Eng(self, "act", 10)
        self.dve = Eng(self, "dve")
        self.pool = Eng(self, "pool", 12)
        self.sp = Eng(self, "sp", 24)
        self.engs = [self.pe, self.act, self.dve, self.pool, self.sp]
        self._pr = []
        self._pw = []

    def sb(self, name, shape, dt):
        return self.es.enter_context(self.nc.sbuf_tensor(name, list(shape), dt))

    def ps(self, name, shape, dt=F32):
        return self.es.enter_context(self.nc.psum_tensor(name, list(shape), dt))

    def dram(self, name, shape, dt, kind):
        return self.nc.dram_tensor(name, list(shape), dt, kind=kind).ap()

    def mm(self, out, lhsT, rhs, start, stop, r=(), w=(), last=False):
        pe = self.pe
        fn = (lambda e, o=out, l=lhsT, rr=rhs, s=start, t=stop: e.matmul(o, l, rr, start=s, stop=t))
        self._pr.extend(r)
        self._pw.extend(w)
        if last:
            h = pe.op(fn, r=self._pr, w=self._pw)
            self._pr = []
            self._pw = []
            return h
        pe.op_noinc(fn, r=r, w=w)
        return None

    def fence(self):
        deps = [e.mark() for e in self.engs] + list(self.dma_handles.values())
        for e in self.engs:
            e.wait(deps)

    def finish(self, final_deps):
        self.sp.wait(final_deps)
        self.fence()
        with self.nc.Block() as block:
            @block.tensor
            def _(e):
                self.pe.replay(e)

            @block.scalar
            def _(e):
                self.act.replay(e)

            @block.vector
            def _(e):
                self.dve.replay(e)

            @block.gpsimd
            def _(e):
                self.pool.replay(e)

            @block.sync
            def _(e):
                self.sp.replay(e)
        self.es.close()
        return self.nc


def regions(n):
    out = []
    c = 0
    while c < n:
        m = min(512, n - c)
        out.append((c, m))
        c += m
    return out


VOFF = {}
_o = 0
for _n, _w in [("g1", 16), ("b1", 32), ("bdw", 16), ("lng", 16), ("lnb", 16), ("b2", 16), ("wdw", 496),
               ("gf0", 16), ("gf1", 16), ("wc0", 132), ("wc1", 132), ("gkv", 16), ("gat", 16),
               ("kng", 1), ("qng", 1), ("hflag", 1), ("eps", 1), ("zero", 1)]:
    VOFF[_n] = _o
    _o += _w
NV = _o


def fm(v):
    v = np.asarray(v, np.float32).reshape(-1, 128)
    return np.ascontiguousarray(v.T)


def pack_vecs(inp, hflag):
    V = np.zeros((128, NV), np.float32)

    def put(name, arr):
        V[:, VOFF[name]:VOFF[name] + arr.shape[1]] = arr

    put("g1", fm(inp["conv_norm_g"][0]))
    put("b1", fm(inp["conv_b_pw1"][0]))
    put("bdw", fm(inp["conv_b_dw"][0]))
    put("lng", fm(inp["conv_ln_g"][0]))
    put("lnb", fm(inp["conv_ln_b"][0]))
    put("b2", fm(inp["conv_b_pw2"][0]))
    wdw = np.asarray(inp["conv_w_dw"][0], np.float32)
    put("wdw", np.ascontiguousarray(wdw.reshape(31, 16, 128).transpose(2, 1, 0)).reshape(128, 496))
    put("gf0", fm(inp["ffn_norm_g"][0]))
    put("gf1", fm(inp["ffn_norm_g"][1]))
    for l in range(2):
        wc = np.asarray(inp["ffn_w_conv"][l], np.float32)
        put("wc%d" % l, np.ascontiguousarray(wc.reshape(3, 44, 128).transpose(2, 0, 1)).reshape(128, 132))
    put("gkv", fm(inp["kv_norm_g"]))
    put("gat", fm(inp["attn_norm_g"][0]))
    put("kng", np.asarray(inp["k_norm_g"], np.float32).reshape(128, 1))
    put("qng", np.asarray(inp["q_norm_g"][0], np.float32).reshape(128, 1))
    V[:, VOFF["hflag"]] = hflag
    V[:, VOFF["eps"]] = EPS
    return V


class Ctx:
    pass


def setup_common(P):
    C = Ctx()
    C.P = P
    nc = P.nc
    C.vecs_d = P.dram("vecs", [128, NV], F32, "ExternalInput")
    C.ident_d = P.dram("ident", [128, 128], F32, "ExternalInput")
    C.R1 = P.sb("R1", [128, 16 * WIN + 8 * OW], F32)
    C.bufA = C.R1[:, 0:16 * WIN].rearrange("p (c t) -> p c t", t=WIN)
    C.bufC = C.R1[:, 16 * WIN:16 * WIN + 8 * OW].bitcast(BF16).rearrange("p (c t) -> p c t", t=OW)
    C.mbuf = C.R1[:, 0:FC * TT // 2].bitcast(BF16).rearrange("p (c t) -> p c t", t=TT)
    C.bufB_t = P.sb("bufB", [128, 16 * WIN], BF16)
    C.bufB = C.bufB_t[:, :].rearrange("p (c t) -> p c t", t=WIN)
    C.A = [Res() for _ in range(16)]
    C.B = [Res() for _ in range(16)]
    C.Cc = [Res() for _ in range(16)]
    C.M = [Res() for _ in range(FC)]
    C.wring = P.sb("wring", [128, NWSLOT * WSLOT], BF16)
    C.wres = [Res() for _ in range(NWSLOT)]
    C.wi = 0
    C.S = P.sb("S", [128, 7424], F32)
    C.vecs = P.sb("vecs_sb", [128, NV], F32)
    C.ident = P.sb("ident_sb", [128, 128], F32)
    C.ones = P.sb("ones_sb", [128, 128], BF16)
    C.psum = P.ps("psum", [128, 4096], F32)
    C.bank = [C.psum[:, i * 512:(i + 1) * 512] for i in range(8)]
    C.bres = [Res() for _ in range(8)]
    C.cres = Res()
    P.sp.dma(C.vecs[:], C.vecs_d, w=[C.cres])
    P.sp.dma(C.ident[:], C.ident_d, w=[C.cres])
    P.dve.op(lambda e: e.memset(C.ones[:], 1.0), w=[C.cres])
    P.fence()
    return C


def vcol(C, name, i=0):
    o = VOFF[name] + i
    return C.vecs[:, o:o + 1]


def wget(C, src, kcn, ncols):
    P = C.P
    k = C.wi
    C.wi = (k + 1) % NWSLOT
    view = C.wring[:, k * WSLOT:k * WSLOT + kcn * ncols].rearrange("p (k c) -> p k c", c=ncols)
    P.pool.dma(view, src.rearrange("(k p) c -> p k c", p=128), w=[C.wres[k]])
    return view, C.wres[k]


def scratch(C, off, n, dt=F32):
    if dt == F32:
        return C.S[:, off:off + n]
    return C.S[:, off:off + (n + 1) // 2].bitcast(BF16)[:, 0:n]


def phase_rmsnorm(C, W, gname):
    P = C.P
    sq = [scratch(C, 0, 512, BF16), scratch(C, 256, 512, BF16)]
    sqr = [Res(), Res()]
    rt = scratch(C, 512, W)
    rstd = scratch(C, 512 + WIN, W)
    rr = Res()
    cnt = 0
    for ri, (c0, n) in enumerate(regions(W)):
        bk = C.bank[ri]
        for c in range(16):
            s = cnt % 2
            cnt += 1
            P.act.op(lambda e, s=s, c=c, c0=c0, n=n: e.activation(out=sq[s][:, 0:n], in_=C.bufA[:, c, c0:c0 + n], func=AF.Square),
                     r=[C.A[c]], w=[sqr[s]])
            P.mm(bk[:, 0:n], C.ones[:], sq[s][:, 0:n], start=(c == 0), stop=(c == 15), r=[sqr[s], C.cres], w=[C.bres[ri]], last=True)
        P.act.op(lambda e, bk=bk, c0=c0, n=n: e.activation(out=rt[:, c0:c0 + n], in_=bk[:, 0:n], func=AF.Sqrt, bias=vcol(C, "eps"), scale=1.0 / D),
                 r=[C.bres[ri], C.cres], w=[rr])
        P.dve.op(lambda e, c0=c0, n=n: e.reciprocal(out=rstd[:, c0:c0 + n], in_=rt[:, c0:c0 + n]), r=[rr], w=[rr])
    for c in range(16):
        P.dve.op(lambda e, c=c: e.scalar_tensor_tensor(out=C.bufB[:, c, 0:W], in0=C.bufA[:, c, 0:W], scalar=vcol(C, gname, c),
                                                       in1=rstd[:, 0:W], op0=ALU.mult, op1=ALU.mult),
                 r=[C.A[c], rr, C.cres], w=[C.B[c]])


def load_window(C, src, col0, W):
    P = C.P
    for c in range(16):
        P.sp.dma(C.bufA[:, c, 0:W], src[:, c, col0:col0 + W], w=[C.A[c]])


def phase_conformer(C, tt, xT, X1A, hf):
    P = C.P
    w1 = C.w_pw1
    w2 = C.w_pw2
    col0 = tt * TT
    load_window(C, xT, col0, WIN)
    phase_rmsnorm(C, WIN, "g1")
    P.fence()
    sg = scratch(C, 0, WIN)
    u = [scratch(C, 1056, WIN, BF16), scratch(C, 1056 + 528, WIN, BF16)]
    dg = [scratch(C, 2112, 31 * 128, BF16).rearrange("p (k c) -> p k c", c=128),
          scratch(C, 2112 + 1984, 31 * 128, BF16).rearrange("p (k c) -> p k c", c=128)]
    vh = scratch(C, 6080, 4)
    junk = scratch(C, 6088, 32)
    sgr = Res()
    ur = [Res(), Res()]
    dgr = [Res(), Res()]
    vhr = Res()
    regs = regions(WIN)
    wdwv = C.vecs[:, VOFF["wdw"]:VOFF["wdw"] + 496].rearrange("p (j k) -> p j k", k=31)

    TD = 12
    cacc = scratch(C, 6120, TT)
    caccr = Res()

    def conv(j):
        s = j % 2
        for ro in range(2):
            bk = C.bank[6 + ro]
            for k in range(TD, 31):
                o0 = 2 + ro * 512 + k
                P.mm(bk[:, :], dg[s][:, k, :], u[s][:, o0:o0 + 512], start=(k == TD), stop=(k == 30),
                     r=[dgr[s], ur[s]], w=[C.bres[6 + ro]], last=(k == 30))
        for k in range(TD):
            if k == 0:
                P.dve.op(lambda e, s=s, j=j, k=k: e.tensor_scalar(out=cacc[:, :], in0=u[s][:, 2 + k:2 + k + TT], scalar1=vcol(C, "wdw", j * 31 + k),
                                                                scalar2=None, op0=ALU.mult), r=[ur[s], C.cres], w=[caccr])
            else:
                P.dve.op(lambda e, s=s, j=j, k=k: e.scalar_tensor_tensor(out=cacc[:, :], in0=u[s][:, 2 + k:2 + k + TT], scalar=vcol(C, "wdw", j * 31 + k),
                                                                       in1=cacc[:, :], op0=ALU.mult, op1=ALU.add), r=[ur[s], C.cres], w=[caccr])
        P.dve.op(lambda e, j=j: e.scalar_tensor_tensor(out=C.bufA[:, j, 2:2 + TT], in0=C.psum[:, 6 * 512:8 * 512], scalar=vcol(C, "bdw", j), in1=cacc[:, :],
                                                      op0=ALU.add, op1=ALU.add), r=[C.bres[6], C.bres[7], caccr, C.cres], w=[C.A[j]])
        for o in range(2):
            P.dve.op(lambda e, o=o, s=s, j=j: e.tensor_tensor(out=junk[:, 0:31], in0=u[s][:, o:o + 31], in1=wdwv[:, j, :], op=ALU.mult),
                     r=[ur[s], C.cres], w=[vhr])
            P.dve.op(lambda e, o=o: e.reduce_sum(out=vh[:, o:o + 1], in_=junk[:, 0:31], axis=AX.X), r=[vhr], w=[vhr])
        P.dve.op(lambda e, j=j: e.tensor_scalar(out=C.bufA[:, j, 0:2], in0=vh[:, 0:2], scalar1=vcol(C, "bdw", j), scalar2=None, op0=ALU.add),
                 r=[vhr, C.cres], w=[C.A[j]])

    for j in range(16):
        s = j % 2
        if j % 2 == 0:
            wa, war = wget(C, w1[:, j * 128:(j + 2) * 128], 16, 256)
            wg, wgr = wget(C, w1[:, D + j * 128:D + (j + 2) * 128], 16, 256)
        jc = (j % 2) * 128
        for kc in range(16):
            for ri, (c0, n) in enumerate(regs):
                P.mm(C.bank[ri][:, 0:n], wg[:, kc, jc:jc + 128], C.bufB[:, kc, c0:c0 + n], start=(kc == 0), stop=(kc == 15),
                     r=[wgr, C.B[kc]], w=[C.bres[ri]], last=(kc == 15))
        for ri, (c0, n) in enumerate(regs):
            P.act.op(lambda e, ri=ri, c0=c0, n=n, j=j: e.activation(out=sg[:, c0:c0 + n], in_=C.bank[ri][:, 0:n], func=AF.Sigmoid,
                                                                  bias=vcol(C, "b1", 16 + j), scale=1.0),
                     r=[C.bres[ri], C.cres], w=[sgr])
        for kc in range(16):
            for ri, (c0, n) in enumerate(regs):
                P.mm(C.bank[3 + ri][:, 0:n], wa[:, kc, jc:jc + 128], C.bufB[:, kc, c0:c0 + n], start=(kc == 0), stop=(kc == 15),
                     r=[war, C.B[kc]], w=[C.bres[3 + ri]], last=(kc == 15))
        for ri, (c0, n) in enumerate(regs):
            P.dve.op(lambda e, ri=ri, c0=c0, n=n, j=j, s=s: e.scalar_tensor_tensor(out=u[s][:, c0:c0 + n], in0=C.bank[3 + ri][:, 0:n],
                                                                               scalar=vcol(C, "b1", j), in1=sg[:, c0:c0 + n],
                                                                               op0=ALU.add, op1=ALU.mult),
                     r=[C.bres[3 + ri], sgr, C.cres], w=[ur[s]])
        if hf is not None:
            P.dve.op(lambda e, s=s: e.tensor_scalar(out=u[s][:, 0:HALO], in0=u[s][:, 0:HALO], scalar1=vcol(C, hf), scalar2=None, op0=ALU.mult),
                     r=[C.cres], w=[ur[s]])
        for k in range(TD, 31):
            P.act.op(lambda e, s=s, k=k, j=j: e.activation(out=dg[s][:, k, :], in_=C.ident[:], func=AF.Identity, scale=vcol(C, "wdw", j * 31 + k)),
                     r=[C.cres], w=[dgr[s]])
        if j >= 1:
            conv(j - 1)
    conv(15)
    P.fence()
    sq = [scratch(C, 0, OW, BF16), scratch(C, 520, OW, BF16)]
    vb = [scratch(C, 1040, OW, BF16), scratch(C, 1560, OW, BF16)]
    mean = scratch(C, 2080, OW)
    rstd = scratch(C, 2080 + OW, OW)
    t1 = [scratch(C, 2080 + 2 * OW, OW), scratch(C, 2080 + 3 * OW, OW)]
    sqr = [Res(), Res()]
    vbr = [Res(), Res()]
    mr = Res()
    t1r = [Res(), Res()]
    oregs = regions(OW)
    for c in range(16):
        s = c % 2
        P.act.op(lambda e, s=s, c=c: e.activation(out=sq[s][:, 0:OW], in_=C.bufA[:, c, 0:OW], func=AF.Square), r=[C.A[c]], w=[sqr[s]])
        P.dve.op(lambda e, s=s, c=c: e.tensor_copy(out=vb[s][:, 0:OW], in_=C.bufA[:, c, 0:OW]), r=[C.A[c]], w=[vbr[s]])
        for ri, (c0, n) in enumerate(oregs):
            P.mm(C.bank[ri][:, 0:n], C.ones[:], vb[s][:, c0:c0 + n], start=(c == 0), stop=(c == 15), r=[vbr[s], C.cres], w=[C.bres[ri]], last=True)
            P.mm(C.bank[3 + ri][:, 0:n], C.ones[:], sq[s][:, c0:c0 + n], start=(c == 0), stop=(c == 15), r=[sqr[s], C.cres], w=[C.bres[3 + ri]], last=True)
    for ri, (c0, n) in enumerate(oregs):
        P.act.op(lambda e, ri=ri, c0=c0, n=n: e.activation(out=mean[:, c0:c0 + n], in_=C.bank[ri][:, 0:n], func=AF.Identity, scale=1.0 / D),
                 r=[C.bres[ri]], w=[mr])
        P.dve.op(lambda e, c0=c0, n=n: e.tensor_tensor(out=t1[0][:, c0:c0 + n], in0=mean[:, c0:c0 + n], in1=mean[:, c0:c0 + n], op=ALU.mult),
                 r=[mr], w=[t1r[0]])
        P.dve.op(lambda e, ri=ri, c0=c0, n=n: e.scalar_tensor_tensor(out=t1[1][:, c0:c0 + n], in0=C.bank[3 + ri][:, 0:n], scalar=1.0 / D,
                                                                    in1=t1[0][:, c0:c0 + n], op0=ALU.mult, op1=ALU.subtract),
                 r=[C.bres[3 + ri], t1r[0]], w=[t1r[1]])
        P.act.op(lambda e, c0=c0, n=n: e.activation(out=t1[0][:, c0:c0 + n], in_=t1[1][:, c0:c0 + n], func=AF.Sqrt, bias=vcol(C, "eps"), scale=1.0),
                 r=[t1r[1], C.cres], w=[t1r[0]])
        P.dve.op(lambda e, c0=c0, n=n: e.reciprocal(out=rstd[:, c0:c0 + n], in_=t1[0][:, c0:c0 + n]), r=[t1r[0]], w=[mr])
    for c in range(16):
        s = c % 2
        P.dve.op(lambda e, s=s, c=c: e.tensor_tensor(out=t1[s][:, 0:OW], in0=C.bufA[:, c, 0:OW], in1=mean[:, 0:OW], op=ALU.subtract),
                 r=[C.A[c], mr], w=[t1r[s]])
        P.dve.op(lambda e, s=s: e.tensor_tensor(out=t1[s][:, 0:OW], in0=t1[s][:, 0:OW], in1=rstd[:, 0:OW], op=ALU.mult),
                 r=[mr], w=[t1r[s]])
        P.act.op(lambda e, s=s, c=c: e.activation(out=C.bufC[:, c, 0:OW], in_=t1[s][:, 0:OW], func=AF.Silu, bias=vcol(C, "lnb", c), scale=vcol(C, "lng", c)),
                 r=[t1r[s], C.cres], w=[C.Cc[c]])
    P.fence()
    xr = [scratch(C, 0, OW), scratch(C, OW, OW)]
    xrr = [Res(), Res()]
    for oc in range(16):
        s = oc % 2
        if oc % 2 == 0:
            wv, wr = wget(C, w2[:, oc * 128:(oc + 2) * 128], 16, 256)
        jc = (oc % 2) * 128
        bs = 3 * s
        P.sp.dma(xr[s][:, 0:OW], xT[:, oc, col0 + 30:col0 + 30 + OW], w=[xrr[s]])
        for kc in range(16):
            for ri, (c0, n) in enumerate(oregs):
                P.mm(C.bank[bs + ri][:, 0:n], wv[:, kc, jc:jc + 128], C.bufC[:, kc, c0:c0 + n], start=(kc == 0), stop=(kc == 15),
                     r=[wr, C.Cc[kc]], w=[C.bres[bs + ri]], last=(kc == 15))
        for ri, (c0, n) in enumerate(oregs):
            P.dve.op(lambda e, bs=bs, ri=ri, c0=c0, n=n, oc=oc, s=s: e.scalar_tensor_tensor(out=C.bufA[:, oc, c0:c0 + n], in0=C.bank[bs + ri][:, 0:n],
                                                                                       scalar=vcol(C, "b2", oc), in1=xr[s][:, c0:c0 + n],
                                                                                       op0=ALU.add, op1=ALU.add),
                     r=[C.bres[bs + ri], xrr[s], C.cres], w=[C.A[oc]])
        if tt == 0:
            P.act.dma(X1A[:, oc, 0:OW], C.bufA[:, oc, 0:OW], r=[C.A[oc]])
        else:
            P.act.dma(X1A[:, oc, 2 + tt * TT:2 + (tt + 1) * TT], C.bufA[:, oc, 2:OW], r=[C.A[oc]])
    P.fence()


def phase_ffn(C, tt, l, XRES, XOUT, res_halo, out_halo, hf):
    P = C.P
    w_in = C.w_in[l]
    w_out = C.w_out[l]
    phase_rmsnorm(C, OW, "gf%d" % l)
    P.fence()
    y = [scratch(C, 0, TT), scratch(C, TT, TT)]
    sl = [scratch(C, 2 * TT, TT), scratch(C, 3 * TT, TT)]
    yr = [Res(), Res()]
    sr = [Res(), Res()]
    wcn = "wc%d" % l
    for c in range(FC):
        s = c % 2
        if c % 2 == 0:
            wg, wgr = wget(C, w_in[:, c * 128:(c + 2) * 128], 16, 256)
            wu, wur = wget(C, w_in[:, FF + c * 128:FF + (c + 2) * 128], 16, 256)
        jc = (c % 2) * 128
        gb = 0 if s == 0 else 5
        gps = C.psum[:, gb * 512:gb * 512 + OW]
        for kc in range(16):
            for ri, (c0, n) in enumerate(regions(OW)):
                P.mm(C.bank[gb + ri][:, 0:n], wg[:, kc, jc:jc + 128], C.bufB[:, kc, c0:c0 + n], start=(kc == 0), stop=(kc == 15),
                     r=[wgr, C.B[kc]], w=[C.bres[gb + ri]], last=(kc == 15))
        gres = [C.bres[gb], C.bres[gb + 1], C.bres[gb + 2]]
        if hf is not None:
            P.dve.op(lambda e, gps=gps: e.tensor_scalar(out=gps[:, 0:2], in0=gps[:, 0:2], scalar1=vcol(C, hf), scalar2=None, op0=ALU.mult),
                     r=[C.cres], w=gres)
        P.act.op(lambda e, gps=gps, s=s, c=c: e.activation(out=y[s][:, :], in_=gps[:, 2:OW], func=AF.Identity, scale=vcol(C, wcn, 2 * FC + c)),
                 r=gres + [C.cres], w=[yr[s]])
        P.dve.op(lambda e, gps=gps, s=s, c=c: e.scalar_tensor_tensor(out=y[s][:, :], in0=gps[:, 1:OW - 1], scalar=vcol(C, wcn, FC + c), in1=y[s][:, :],
                                                                  op0=ALU.mult, op1=ALU.add), r=gres + [C.cres], w=[yr[s]])
        P.dve.op(lambda e, gps=gps, s=s, c=c: e.scalar_tensor_tensor(out=y[s][:, :], in0=gps[:, 0:OW - 2], scalar=vcol(C, wcn, c), in1=y[s][:, :],
                                                                  op0=ALU.mult, op1=ALU.add), r=gres + [C.cres], w=[yr[s]])
        P.act.op(lambda e, s=s: e.activation(out=sl[s][:, :], in_=y[s][:, :], func=AF.Silu), r=[yr[s]], w=[sr[s]])
        for kc in range(16):
            for ri in range(2):
                P.mm(C.bank[3 + ri][:, :], wu[:, kc, jc:jc + 128], C.bufB[:, kc, 2 + ri * 512:2 + ri * 512 + 512], start=(kc == 0), stop=(kc == 15),
                     r=[wur, C.B[kc]], w=[C.bres[3 + ri]], last=(kc == 15))
        P.dve.op(lambda e, s=s, c=c: e.tensor_tensor(out=C.mbuf[:, c, :], in0=C.psum[:, 3 * 512:5 * 512], in1=sl[s][:, :], op=ALU.mult),
                 r=[C.bres[3], C.bres[4], sr[s]], w=[C.M[c]])
    P.fence()
    xr = [scratch(C, 0, TT), scratch(C, TT, TT)]
    xo = [scratch(C, 2 * TT, TT), scratch(C, 3 * TT, TT)]
    xrr = [Res(), Res()]
    xor_ = [Res(), Res()]
    for oc in range(16):
        s = oc % 2
        wv, wr = wget(C, w_out[:, oc * 128:(oc + 1) * 128], FC, 128)
        P.sp.dma(xr[s][:, :], XRES[:, oc, res_halo + tt * TT:res_halo + (tt + 1) * TT], w=[xrr[s]])
        bs = 2 * s
        for c in range(FC):
            for ri in range(2):
                P.mm(C.bank[bs + ri][:, :], wv[:, c, :], C.mbuf[:, c, ri * 512:(ri + 1) * 512], start=(c == 0), stop=(c == FC - 1),
                     r=[wr, C.M[c]], w=[C.bres[bs + ri]], last=(c == FC - 1))
        P.dve.op(lambda e, bs=bs, s=s: e.tensor_tensor(out=xo[s][:, :], in0=C.psum[:, bs * 512:(bs + 2) * 512], in1=xr[s][:, :], op=ALU.add),
                 r=[C.bres[bs], C.bres[bs + 1], xrr[s]], w=[xor_[s]])
        P.act.dma(XOUT[:, oc, out_halo + tt * TT:out_halo + (tt + 1) * TT], xo[s][:, :], r=[xor_[s]])
    P.fence()


_CACHE = {}


def _ident():
    return np.eye(128, dtype=np.float32)


def phase_proj_hn(C, w, colbase, gname, scale, dst, dst_col0, km=None, km_col0=0, ncols=TT):
    P = C.P
    sqk = [scratch(C, 0, TT, BF16), scratch(C, 5300, TT, BF16)]
    rt = scratch(C, 512, TT)
    rs = scratch(C, 1536, TT)
    kf = scratch(C, 2560, TT)
    kb = [scratch(C, 3584, TT, BF16), scratch(C, 4096, TT, BF16)]
    kms = scratch(C, 4608, 4)
    r_sq = [Res(), Res()]
    r_rt, r_kf, r_km = Res(), Res(), Res()
    r_kb = [Res(), Res()]
    regs = regions(ncols)
    nr = len(regs)
    wcur = {}

    def proj(hc):
        s = hc % 2
        if hc % 2 == 0:
            wcur["w"] = wget(C, w[:, colbase + hc * 128:colbase + (hc + 2) * 128], 16, 256)
        wv, wr = wcur["w"]
        jc = (hc % 2) * 128
        bs = (0, 2, 6)[hc % 3]
        for kc in range(16):
            for ri, (c0, n) in enumerate(regs):
                P.mm(C.bank[bs + ri][:, 0:n], wv[:, kc, jc:jc + 128], C.bufB[:, kc, c0:c0 + n], start=(kc == 0), stop=(kc == 15),
                     r=[wr, C.B[kc]], w=[C.bres[bs + ri]], last=(kc == 15))
        yps = C.psum[:, bs * 512:bs * 512 + ncols]
        yres = [C.bres[bs + ri] for ri in range(nr)]
        P.act.op(lambda e, yps=yps: e.activation(out=sqk[s][:, 0:ncols], in_=yps, func=AF.Square), r=yres, w=[r_sq[s]])

    def epi(hc):
        s = hc % 2
        bs = (0, 2, 6)[hc % 3]
        yps = C.psum[:, bs * 512:bs * 512 + ncols]
        yres = [C.bres[bs + ri] for ri in range(nr)]
        for ri, (c0, n) in enumerate(regs):
            P.mm(C.bank[4 + ri][:, 0:n], C.ones[:], sqk[s][:, c0:c0 + n], start=True, stop=True, r=[r_sq[s], C.cres], w=[C.bres[4 + ri]], last=True)
        P.act.op(lambda e: e.activation(out=rt[:, 0:ncols], in_=C.psum[:, 4 * 512:4 * 512 + ncols], func=AF.Sqrt, bias=vcol(C, "eps"), scale=1.0 / 128),
                 r=[C.bres[4 + ri] for ri in range(nr)] + [C.cres], w=[r_rt])
        P.dve.op(lambda e: e.reciprocal(out=rs[:, 0:ncols], in_=rt[:, 0:ncols]), r=[r_rt], w=[r_rt])
        P.dve.op(lambda e, yps=yps: e.scalar_tensor_tensor(out=kf[:, 0:ncols], in0=yps, scalar=vcol(C, gname), in1=rs[:, 0:ncols], op0=ALU.mult, op1=ALU.mult),
                 r=yres + [r_rt, C.cres], w=[r_kf])
        P.act.op(lambda e, s=s: e.activation(out=kb[s][:, 0:ncols], in_=kf[:, 0:ncols], func=AF.Identity, scale=float(scale)), r=[r_kf], w=[r_kb[s]])
        P.act.dma(dst[:, hc, dst_col0:dst_col0 + ncols], kb[s][:, 0:ncols], r=[r_kb[s]])
        if km is not None:
            P.dve.op(lambda e: e.tensor_reduce(out=kms[:, 0:4], in_=kf[:, :].rearrange("p (n j) -> p n j", j=256), axis=AX.X, op=ALU.add),
                     r=[r_kf], w=[r_km])
            P.act.op(lambda e, hc=hc: e.activation(out=km[:, hc, km_col0:km_col0 + 4], in_=kms[:, 0:4], func=AF.Identity, scale=1.0 / 256),
                     r=[r_km], w=[C.kmres])

    for hc in range(16):
        proj(hc)
        if hc >= 1:
            epi(hc - 1)
    epi(15)


def phase_v(C, w, colbase, V_d, tok0):
    P = C.P
    vst = [scratch(C, 5812, 512, BF16), scratch(C, 6068, 512, BF16)]
    vr = [Res(), Res()]
    base = 16 * WIN
    vw = [C.R1[:, base + i * 4096:base + (i + 1) * 4096].bitcast(BF16).rearrange("p (k c) -> p k c", c=512) for i in range(2)]
    if not hasattr(C, "vwr"):
        C.vwr = [Res(), Res()]
        C.vcnt = 0
    for vb in range(4):
        i = vb % 2
        P.pool.dma(vw[i], w[:, colbase + vb * 512:colbase + (vb + 1) * 512].rearrange("(k p) c -> p k c", p=128), w=[C.vwr[i]])
        for tk in range(8):
            s = C.vcnt % 2
            C.vcnt += 1
            bk = 6 + s
            for kc in range(16):
                P.mm(C.bank[bk][:, :], C.bufB[:, kc, tk * 128:(tk + 1) * 128], vw[i][:, kc, :], start=(kc == 0), stop=(kc == 15),
                     r=[C.vwr[i], C.B[kc]], w=[C.bres[bk]], last=(kc == 15))
            P.act.op(lambda e, s=s, bk=bk: e.activation(out=vst[s][:, :], in_=C.bank[bk][:, :], func=AF.Identity), r=[C.bres[bk]], w=[vr[s]])
            P.act.dma(V_d[tok0 + tk * 128:tok0 + (tk + 1) * 128, vb * 512:(vb + 1) * 512], vst[s][:, :], r=[vr[s]])


def r1v(C, off, n, dt=F32):
    if dt == F32:
        return C.R1[:, off:off + n]
    return C.R1[:, off:off + (n + 1) // 2].bitcast(BF16)[:, 0:n]


def attention_setup(C, D_):
    P = C.P
    A = Ctx()
    A.Qb = [r1v(C, 0, 8192, BF16).rearrange("p (h q) -> p h q", q=512), C.bufB_t[:, 0:8192].rearrange("p (h q) -> p h q", q=512)]
    A.KTh = [r1v(C, 4096, 4096, BF16), r1v(C, 6144, 4096, BF16)]
    A.Vh = [r1v(C, 8192, 4096, BF16).rearrange("p (c d) -> p c d", d=128), r1v(C, 10240, 4096, BF16).rearrange("p (c d) -> p c d", d=128)]
    A.Rt = [r1v(C, 12288, 8192, BF16).rearrange("p (h q) -> p h q", q=512), C.bufB_t[:, 8192:16384].rearrange("p (h q) -> p h q", q=512)]
    A.pt = [r1v(C, 16384 + i * 256, 512, BF16) for i in range(4)]
    A.ssb = [r1v(C, 17408, 512), r1v(C, 17920, 512)]
    A.acc = [r1v(C, 18432, 512), r1v(C, 18944, 512)]
    A.rl = [r1v(C, 19456, 512), r1v(C, 19968, 512)]
    A.ot = [r1v(C, 20480, 512, BF16), r1v(C, 20736, 512, BF16)]
    A.gm = r1v(C, 20992, 256)
    A.sel = r1v(C, 21248, 256)
    A.rbq = r1v(C, 21504, 256, BF16)
    A.mx = r1v(C, 21632, 128).rearrange("p (h k) -> p h k", k=8)
    A.thr = r1v(C, 21760, 16)
    A.Em = r1v(C, 21776, 2048, BF16).rearrange("p (n j) -> p n j", j=128)
    A.cmask = r1v(C, 22800, 1024).rearrange("p (t q) -> p t q", t=2)
    A.kmb = r1v(C, 23824, 256, BF16).rearrange("p (h n) -> p h n", n=16)
    A.identb = r1v(C, 23952, 128, BF16)
    A.ones32 = r1v(C, 24016, 128)
    A.pastm = C.S[:, 0:2304].rearrange("p (b x) -> p b x", x=256)
    A.ownm = C.S[:, 2304:4608].rearrange("p (b x) -> p b x", x=256)
    A.r_const = Res()
    A.r_Qb = [Res(), Res()]
    A.r_Rt = [Res(), Res()]
    A.r_KT = [Res(), Res()]
    A.r_V = [Res(), Res()]
    A.r_pt = [Res() for _ in range(4)]
    A.r_ssb = [Res(), Res()]
    A.r_acc = [Res(), Res()]
    A.r_g = Res()
    A.r_rl = [Res(), Res()]
    A.r_ot = [Res(), Res()]
    w = [A.r_const]
    for bf in range(2):
        P.dve.op(lambda e, bf=bf: e.memset(A.Rt[bf][:, :, :], 0.0), w=[A.r_Rt[bf]])
    P.dve.op(lambda e: e.memset(A.ones32[:, :], 1.0), w=w)
    P.sp.dma(A.Em[0:34, :, :], D_["emat"], w=w)
    P.sp.dma(A.cmask[:, :, :], D_["cmask"].rearrange("p (t q) -> p t q", t=2), w=w)
    P.sp.dma(A.pastm[:, :, :], D_["pastm"].rearrange("p (b x) -> p b x", x=256), w=w)
    P.sp.dma(A.ownm[:, :, :], D_["ownm"].rearrange("p (b x) -> p b x", x=256), w=w)
    P.act.op(lambda e: e.activation(out=A.kmb[:, :, :], in_=C.kmT[:, :].rearrange("p (h n) -> p h n", n=16), func=AF.Identity), r=[C.kmres], w=w)
    P.act.op(lambda e: e.activation(out=A.identb[:, :], in_=C.ident[:], func=AF.Identity), r=[C.cres], w=w)
    return A


def attention_all(C, A, units, QT_d, KT_d, V_d, OT_d, qal_d):
    P = C.P
    cst = [A.r_const]
    GB = 3
    gps = C.bank[GB]
    tps = C.psum[:, GB * 512 + 256:(GB + 1) * 512].bitcast(BF16)

    def gating_steps(ui):
        i0, nqb = units[ui]
        bf = ui % 2
        NQc = nqb * 256
        q0 = i0 * 256
        Qb, Rt = A.Qb[bf], A.Rt[bf]
        steps = []

        def load():
            P.sp.dma(Qb[:, :, 0:NQc], QT_d[:, :, q0:q0 + NQc], w=[A.r_Qb[bf]])
            P.sp.dma(Rt[32:34, :, 0:NQc], qal_d[:, :, q0:q0 + NQc], w=[A.r_Rt[bf]])
        steps.append(load)
        for qt in range(NQc // 128):
            qi = i0 + qt // 2

            def stepA(qt=qt, qi=qi):
                for h in range(16):
                    P.mm(gps[:, h * 16:(h + 1) * 16], Qb[:, h, qt * 128:(qt + 1) * 128], A.kmb[:, h, :], start=True, stop=True,
                         r=[A.r_Qb[bf]] + cst, w=[C.bres[GB]], last=(h == 15))
                P.dve.op(lambda e: e.tensor_tensor(out=A.gm[:, :], in0=gps[:, 0:256], in1=A.pastm[:, qi, :], op=ALU.add),
                         r=[C.bres[GB]] + cst, w=[A.r_g])
                for h in range(16):
                    P.dve.op(lambda e, h=h: e.max(out=A.mx[:, h, :], in_=A.gm[:, h * 16:(h + 1) * 16]), r=[A.r_g], w=[A.r_g])
                P.dve.op(lambda e: e.tensor_scalar(out=A.thr[:, :], in0=A.mx[:, :, 2], scalar1=-1e29, scalar2=None, op0=ALU.max), r=[A.r_g], w=[A.r_g])
                for h in range(16):
                    P.dve.op(lambda e, h=h: e.tensor_scalar(out=A.sel[:, h * 16:(h + 1) * 16], in0=A.gm[:, h * 16:(h + 1) * 16], scalar1=A.thr[:, h:h + 1],
                                                            scalar2=None, op0=ALU.is_ge), r=[A.r_g], w=[A.r_g])
                P.dve.op(lambda e: e.tensor_tensor(out=A.sel[:, :], in0=A.sel[:, :], in1=A.ownm[:, qi, :], op=ALU.max), r=[A.r_g] + cst, w=[A.r_g])
                P.dve.op(lambda e: e.tensor_scalar(out=A.rbq[:, :], in0=A.sel[:, :], scalar1=-1.0, scalar2=30000.0, op0=ALU.add, op1=ALU.mult),
                         r=[A.r_g], w=[A.r_g])

            def stepB(qt=qt):
                for hg in range(4):
                    for hh in range(4):
                        h = hg * 4 + hh
                        P.pe.op(lambda e, h=h, hh=hh: e.transpose(tps[0:16, hh * 128:(hh + 1) * 128], A.rbq[:, h * 16:(h + 1) * 16], A.identb[:, :]),
                                r=[A.r_g] + cst, w=[C.bres[GB]])
                    P.act.op(lambda e, hg=hg: e.activation(out=Rt[0:16, hg * 4:(hg + 1) * 4, qt * 128:(qt + 1) * 128],
                                                           in_=tps[0:16, :].rearrange("p (h q) -> p h q", q=128), func=AF.Identity),
                             r=[C.bres[GB]], w=[A.r_Rt[bf]])
            steps.append(stepA)
            steps.append(stepB)
        return steps

    def unit_tiles(ui):
        i0, nqb = units[ui]
        NQc = nqb * 256
        nkb = 8 + i0 + nqb - 1
        tiles = []
        for h in range(16):
            lst = []
            for n in range(nkb):
                for jt in range(2):
                    if n <= 7 + i0:
                        lst.append((n, jt, 0, NQc))
                    else:
                        lst.append((n, jt, 256, 256))
            for ist, (n, jt, c0, ncl) in enumerate(lst):
                tiles.append((h, n, jt, c0, ncl, ist, len(lst)))
        return tiles

    cnt = {"k": 0}
    for st in gating_steps(0):
        st()
    for ui, (i0, nqb) in enumerate(units):
        bf = ui % 2
        NQc = nqb * 256
        nkb = 8 + i0 + nqb - 1
        nk = nkb * 256
        q0 = i0 * 256
        Qb, Rt = A.Qb[bf], A.Rt[bf]
        tiles = unit_tiles(ui)
        nxt = gating_steps(ui + 1) if ui + 1 < len(units) else []

        def load_head(h):
            s = h % 2
            P.sp.dma(A.KTh[s][:, 0:nk], KT_d[:, h, 0:nk], w=[A.r_KT[s]])
            P.sp.dma(A.Vh[s][:, 0:nkb * 2, :], V_d[0:nk, h * 128:(h + 1) * 128].rearrange("(c p) d -> p c d", p=128), w=[A.r_V[s]])

        def score(k, kg):
            h, n, jt, c0, ncl, ist, nst = tiles[k]
            s = h % 2
            if ist == 0:
                load_head(h)
            sb_ = C.bank[kg % 3]
            sr = C.bres[kg % 3]
            P.mm(sb_[:, 0:ncl], A.KTh[s][:, n * 256 + jt * 128:n * 256 + (jt + 1) * 128], Qb[:, h, c0:c0 + ncl], start=True, stop=False,
                 r=[A.r_KT[s], A.r_Qb[bf]], w=[sr])
            P.mm(sb_[:, 0:ncl], A.Em[0:34, n, :], Rt[0:34, h, c0:c0 + ncl], start=False, stop=True, r=[A.r_Rt[bf]] + cst, w=[sr], last=True)
            p = kg % 4
            bias = C.kalf[:, h, n * 2 + jt:n * 2 + jt + 1]
            if n >= 7 + i0:
                x = kg % 2
                P.dve.op(lambda e: e.tensor_tensor(out=A.ssb[x][:, 0:ncl], in0=sb_[:, 0:ncl], in1=A.cmask[:, jt, 0:ncl], op=ALU.add),
                         r=[sr] + cst, w=[A.r_ssb[x]])
                P.act.op(lambda e: e.activation(out=A.pt[p][:, 0:ncl], in_=A.ssb[x][:, 0:ncl], func=AF.Exp, bias=bias, scale=1.0),
                         r=[A.r_ssb[x]] + cst, w=[A.r_pt[p]])
            else:
                P.act.op(lambda e: e.activation(out=A.pt[p][:, 0:ncl], in_=sb_[:, 0:ncl], func=AF.Exp, bias=bias, scale=1.0),
                         r=[sr] + cst, w=[A.r_pt[p]])
            if ist == 0:
                P.dve.op(lambda e, NQc=NQc: e.tensor_copy(out=A.acc[s][:, 0:NQc], in_=A.pt[p][:, 0:NQc]), r=[A.r_pt[p]], w=[A.r_acc[s]])
            else:
                P.dve.op(lambda e: e.tensor_tensor(out=A.acc[s][:, c0:c0 + ncl], in0=A.acc[s][:, c0:c0 + ncl], in1=A.pt[p][:, 0:ncl], op=ALU.add),
                         r=[A.r_pt[p]], w=[A.r_acc[s]])

        def pvl(k, kg):
            h, n, jt, c0, ncl, ist, nst = tiles[k]
            s = h % 2
            p = kg % 4
            obi = 4 + 2 * s
            ob = C.bank[obi]
            lb = C.bank[obi + 1]
            lastst = (ist == nst - 1)
            P.mm(ob[:, c0:c0 + ncl], A.Vh[s][:, n * 2 + jt, :], A.pt[p][:, 0:ncl], start=(ist == 0), stop=lastst, r=[A.r_V[s], A.r_pt[p]], w=[C.bres[obi]], last=True)
            if lastst:
                P.pe.op(lambda e, NQc=NQc: e.matmul(lb[:, 0:NQc], A.ones32[:, :], A.acc[s][:, 0:NQc], start=True, stop=True),
                        r=[A.r_acc[s]] + cst, w=[C.bres[obi + 1]])
                P.dve.op(lambda e, NQc=NQc: e.reciprocal(out=A.rl[s][:, 0:NQc], in_=lb[:, 0:NQc]), r=[C.bres[obi + 1]], w=[A.r_rl[s]])
                P.dve.op(lambda e, NQc=NQc: e.tensor_tensor(out=A.ot[s][:, 0:NQc], in0=ob[:, 0:NQc], in1=A.rl[s][:, 0:NQc], op=ALU.mult),
                         r=[C.bres[obi], A.r_rl[s]], w=[A.r_ot[s]])
                P.act.dma(OT_d[:, h, q0:q0 + NQc], A.ot[s][:, 0:NQc], r=[A.r_ot[s]])

        LA = 2
        nt = len(tiles)
        sched = {}
        for si, st in enumerate(nxt):
            sched.setdefault(((si + 1) * nt) // (len(nxt) + 1), []).append(st)
        base = cnt["k"]
        for k in range(nt + LA):
            if k < nt:
                score(k, base + k)
            if k - LA >= 0:
                pvl(k - LA, base + k - LA)
            for st in sched.get(k, []):
                st()
        cnt["k"] = base + nt


LSEQ = 4096
QPRE = 1792
NQ = LSEQ - QPRE
QTILES = [(1792, 256), (2048, 1024), (3072, 1024)]


def build_F():
    P = Prog()
    C = setup_common(P)
    xT = P.dram("xT", [128, 16, HALO + LSEQ], F32, "ExternalInput")
    C.w_pw1 = P.dram("w_pw1", [D, 2 * D], F32, "ExternalInput")
    C.w_pw2 = P.dram("w_pw2", [D, D], F32, "ExternalInput")
    C.w_in = [P.dram("w_in0", [D, 2 * FF], F32, "ExternalInput"), P.dram("w_in1", [D, 2 * FF], F32, "ExternalInput")]
    C.w_out = [P.dram("w_out0", [FF, D], F32, "ExternalInput"), P.dram("w_out1", [FF, D], F32, "ExternalInput")]
    w_kv = P.dram("w_kv", [D, 2 * D], F32, "ExternalInput")
    w_q = P.dram("w_q", [D, D], F32, "ExternalInput")
    w_o = P.dram("w_o", [D, D], F32, "ExternalInput")
    D_ = {"emat": P.dram("emat", [34, 16, 128], BF16, "ExternalInput"),
          "kal": P.dram("kal", [128, 32], F32, "ExternalInput"),
          "cmask": P.dram("cmask", [128, 1024], F32, "ExternalInput"),
          "pastm": P.dram("pastm", [128, 2304], F32, "ExternalInput"),
          "ownm": P.dram("ownm", [128, 2304], F32, "ExternalInput")}
    kalf_d = P.dram("kalf", [128, 16 * 32], F32, "ExternalInput")
    qal_d = P.dram("qal", [2, 16, NQ], BF16, "ExternalInput")
    OUT = P.dram("outT", [128, 16, TCORE], F32, "ExternalOutput")
    X1A = P.dram("X1A", [128, 16, 2 + LSEQ], F32, "Internal")
    X1 = P.dram("X1", [128, 16, 2 + LSEQ], F32, "Internal")
    X2 = P.dram("X2", [128, 16, 256 + TCORE], F32, "Internal")
    KT_d = P.dram("KT_d", [128, 16, LSEQ], BF16, "Internal")
    V_d = P.dram("V_d", [LSEQ, D], BF16, "Internal")
    QT_d = P.dram("QT_d", [128, 16, NQ], BF16, "Internal")
    OT_d = P.dram("OT_d", [128, 16, NQ], BF16, "Internal")
    C.kmT = P.sb("kmT", [128, 256], F32)
    C.kmres = Res()
    kalf_t = P.sb("kalf_sb", [128, 512], F32)
    C.kalf = kalf_t[:, :].rearrange("p (h c) -> p h c", c=32)
    P.sp.dma(kalf_t[:, :], kalf_d, w=[C.cres])
    kmv = C.kmT[:, :].rearrange("p (h n) -> p h n", n=16)
    for tt in range(4):
        hf = "zero" if tt == 0 else ("hflag" if tt == 2 else None)
        phase_conformer(C, tt, xT, X1A, hf)
        phase_ffn(C, tt, 0, X1A, X1, 2, 2, hf)
    for t4 in range(4):
        load_window(C, X1, 2 + t4 * TT, TT)
        phase_rmsnorm(C, TT, "gkv")
        P.fence()
        phase_proj_hn(C, w_kv, 0, "kng", 1.0, KT_d, t4 * TT, km=kmv, km_col0=t4 * 4)
        phase_v(C, w_kv, D, V_d, t4 * TT)
        P.fence()
    for (st, n) in QTILES:
        load_window(C, X1, 2 + st, n)
        phase_rmsnorm(C, n, "gat")
        P.fence()
        phase_proj_hn(C, w_q, 0, "qng", 128.0 ** -0.5, QT_d, st - QPRE, ncols=n)
        P.fence()
    A = attention_setup(C, D_)
    P.fence()
    attention_all(C, A, [(0, 1), (1, 2), (3, 2), (5, 2), (7, 2)], QT_d, KT_d, V_d, OT_d, qal_d)
    P.fence()
    xr = [scratch(C, 0, TT), scratch(C, TT, TT)]
    xo = [scratch(C, 2 * TT, TT), scratch(C, 3 * TT, TT)]
    xrr = [Res(), Res()]
    xor_ = [Res(), Res()]
    for (st, n) in QTILES:
        regs = regions(n)
        for c in range(16):
            P.sp.dma(C.bufB[:, c, 0:n], OT_d[:, c, st - QPRE:st - QPRE + n], w=[C.B[c]])
        for oc in range(16):
            s = oc % 2
            if oc % 2 == 0:
                wv, wr = wget(C, w_o[:, oc * 128:(oc + 2) * 128], 16, 256)
            jc = (oc % 2) * 128
            bs = 2 * s
            P.sp.dma(xr[s][:, 0:n], X1[:, oc, 2 + st:2 + st + n], w=[xrr[s]])
            for kc in range(16):
                for ri, (c0, m) in enumerate(regs):
                    P.mm(C.bank[bs + ri][:, 0:m], wv[:, kc, jc:jc + 128], C.bufB[:, kc, c0:c0 + m], start=(kc == 0), stop=(kc == 15),
                         r=[wr, C.B[kc]], w=[C.bres[bs + ri]], last=(kc == 15))
            P.dve.op(lambda e, bs=bs, s=s, n=n: e.tensor_tensor(out=xo[s][:, 0:n], in0=C.psum[:, bs * 512:bs * 512 + n], in1=xr[s][:, 0:n], op=ALU.add),
                     r=[C.bres[bs + ri] for ri in range(len(regs))] + [xrr[s]], w=[xor_[s]])
            P.act.dma(X2[:, oc, st - QPRE:st - QPRE + n], xo[s][:, 0:n], r=[xor_[s]])
        P.fence()
    for t in range(NTILE):
        load_window(C, X2, 254 + t * TT, OW)
        phase_ffn(C, t, 1, X2, OUT, 256, 0, "hflag" if t == 0 else None)
    return P.finish([])


SLOPES = np.exp2(-8.0 * np.arange(1, NH + 1, dtype=np.float64) / NH)


def attn_tables(half):
    T = {}
    em = np.zeros((34, 16, 128), np.float32)
    for n in range(16):
        em[n, n, :] = 1.0
    em[32:34, :, :] = 1.0
    T["emat"] = em.astype(ml_dtypes.bfloat16)
    T["kal"] = np.zeros((128, 32), np.float32)
    pos = (np.arange(32)[None, :] * 128 + np.arange(128)[:, None]).astype(np.float64)
    T["kalf"] = (SLOPES[None, :, None] * pos[:, None, :]).astype(np.float32).reshape(128, 512)
    tq = QPRE + np.arange(NQ, dtype=np.float64)
    v = -(SLOPES[:, None] * tq[None, :])
    hi = v.astype(np.float32).astype(ml_dtypes.bfloat16)
    lo = (v - hi.astype(np.float64)).astype(np.float32).astype(ml_dtypes.bfloat16)
    T["qal"] = np.stack([hi, lo], 0)
    kp = np.arange(2)[None, :, None] * 128 + np.arange(128)[:, None, None]
    cm = np.zeros((128, 2, 512), np.float32)
    cm[:, :, 0:256] = np.where(kp > np.arange(256)[None, None, :], -30000.0, 0.0)
    T["cmask"] = cm.reshape(128, 1024)
    pm = np.full((9, 16, 16), -1e30, np.float32)
    om = np.zeros((9, 16, 16), np.float32)
    nlo = 0 if half == 1 else 8
    for i in range(9):
        cur = 7 + i
        pm[i, :, nlo:cur] = 0.0
        om[i, :, cur] = 1.0
    T["pastm"] = np.broadcast_to(pm.reshape(1, 2304), (128, 2304)).copy()
    T["ownm"] = np.broadcast_to(om.reshape(1, 2304), (128, 2304)).copy()
    return T


_CACHE = {}


def kernel(**inputs):
    inp = {k: np.asarray(v) for k, v in inputs.items()}
    cores = list(range(8))
    if "F" not in _CACHE:
        _CACHE["F"] = build_F()
    x = inp["x"].astype(np.float32, copy=False)
    ws = {"w_pw1": np.ascontiguousarray(inp["conv_w_pw1"][0]), "w_pw2": np.ascontiguousarray(inp["conv_w_pw2"][0]),
          "w_in0": np.ascontiguousarray(inp["ffn_w_in"][0]), "w_out0": np.ascontiguousarray(inp["ffn_w_out"][0]),
          "w_in1": np.ascontiguousarray(inp["ffn_w_in"][1]), "w_out1": np.ascontiguousarray(inp["ffn_w_out"][1]),
          "w_kv": np.ascontiguousarray(inp["w_kv"]), "w_q": np.ascontiguousarray(inp["w_q"][0]), "w_o": np.ascontiguousarray(inp["w_o"][0])}
    tabs = [attn_tables(0), attn_tables(1)]
    ident = np.eye(128, dtype=np.float32)
    maps = []
    for c in cores:
        b, h = c // 2, c % 2
        xt = np.ascontiguousarray(x[b].T).reshape(16, 128, 4096).transpose(1, 0, 2)
        buf = np.zeros((128, 16, HALO + LSEQ), np.float32)
        if h == 1:
            buf[:, :, HALO:] = xt
        else:
            buf[:, :, HALO + TCORE:] = xt[:, :, 0:TCORE]
        maps.append(dict(xT=buf, vecs=pack_vecs(inp, float(h)), ident=ident, **ws, **tabs[h]))
    res = run_bass_kernel_spmd(_CACHE["F"], maps, core_ids=cores)
    out = np.zeros((4, 4096, D), np.float32)
    for c in cores:
        b, h = c // 2, c % 2
        o = res.results[c]["outT"]
        out[b, h * TCORE:(h + 1) * TCORE, :] = o.transpose(2, 1, 0).reshape(TCORE, D)
    return out
```
